# Optimizing a Trainium2 kernel written in Bass

```python
import jax, jax.numpy as jnp
from jax import lax
import numpy as np

D_MODEL = 1024
BATCH = 32
SEQ = 2048
DEPTH = 2

RET_HEADS = 4
RET_DK = 128
RET_DV = 256
RET_QK = RET_HEADS * RET_DK
RET_V = RET_HEADS * RET_DV
RET_CHUNK = 128
ROPE_BASE = 10000.0
SB_HEADS = 8
SB_DH = 64
SB_W = SB_HEADS * SB_DH
SB_BLOCK = 128
FF = ((8 * D_MODEL + 3 * 256 - 1) // (3 * 256)) * 256
NORM_EPS = 1e-6
IN_SPLITS = (RET_QK, RET_QK, RET_V, RET_V, SB_W, SB_W, SB_W, D_MODEL, D_MODEL)
IN_COLS = sum(IN_SPLITS)

kernel_name = "hybrid_retention_stickbreaking_gated"


def _rmsnorm(x, g):
    x32 = x.astype(jnp.float32)
    y = x32 * lax.rsqrt(jnp.mean(x32 * x32, axis=-1, keepdims=True) + NORM_EPS)
    return (y * g.astype(jnp.float32)).astype(x.dtype)


def _split_cols(p):
    offs = np.cumsum(IN_SPLITS)[:-1].tolist()
    return jnp.split(p, offs, axis=-1)


def _rope(x):
    S, d = x.shape[1], x.shape[-1]
    half = d // 2
    pos = jnp.arange(S, dtype=jnp.float32)
    inv = 1.0 / (ROPE_BASE ** (jnp.arange(half, dtype=jnp.float32) / half))
    ang = pos[:, None] * inv[None, :]
    cos = jnp.cos(ang)[None, :, None, :]
    sin = jnp.sin(ang)[None, :, None, :]
    x32 = x.astype(jnp.float32)
    x1, x2 = x32[..., :half], x32[..., half:]
    return jnp.concatenate([x1 * cos - x2 * sin, x1 * sin + x2 * cos], axis=-1)


def _retention_chunkwise(q, k, v):
    B, S, H, DK = q.shape
    DV = v.shape[-1]
    C = RET_CHUNK
    N = S // C
    log_g = jnp.log1p(-jnp.exp2(-5.0 - jnp.arange(H, dtype=jnp.float32)))
    q = q.reshape(B, N, C, H, DK)
    k = k.reshape(B, N, C, H, DK)
    v = v.reshape(B, N, C, H, DV)
    i = jnp.arange(C, dtype=jnp.float32)
    diff = i[:, None] - i[None, :]
    dec = jnp.where(diff[None] >= 0,
                    jnp.exp(log_g[:, None, None] * jnp.maximum(diff, 0.0)[None]), 0.0)
    scores = jnp.einsum('bnihd,bnjhd->bnhij', q, k) * dec[None, None]
    o_intra = jnp.einsum('bnhij,bnjhe->bnihe', scores, v)
    k_dec = jnp.exp(log_g[:, None] * (C - 1.0 - i)[None, :])
    kv = jnp.einsum('bnjhd,hj,bnjhe->nbhde', k, k_dec, v)
    chunk_dec = jnp.exp(log_g * C)[None, :, None, None]

    def step(R, kv_n):
        return chunk_dec * R + kv_n, R

    _, R_prev = lax.scan(step, jnp.zeros((B, H, DK, DV), jnp.float32), kv)
    q_dec = jnp.exp(log_g[:, None] * (i + 1.0)[None, :])
    o_cross = jnp.einsum('bnihd,nbhde,hi->bnihe', q, R_prev, q_dec)
    return (o_intra + o_cross).reshape(B, S, H, DV)


def _head_norm(y):
    mu = jnp.mean(y, axis=-1, keepdims=True)
    var = jnp.mean(jnp.square(y - mu), axis=-1, keepdims=True)
    return (y - mu) * lax.rsqrt(var + NORM_EPS)


def _stick_breaking(q, k, v):
    B, S, H, d = q.shape
    scale = d ** -0.5
    outs = []
    for blk in range(S // SB_BLOCK):
        start, end = blk * SB_BLOCK, (blk + 1) * SB_BLOCK
        qb = q[:, start:end]
        kk = k[:, :end]
        vv = v[:, :end]
        z = jnp.einsum('bqhd,bkhd->bhqk', qb, kk).astype(jnp.float32) * scale
        q_pos = start + jnp.arange(SB_BLOCK)
        k_pos = jnp.arange(end)
        mask = (k_pos[None, :] < q_pos[:, None])[None, None]
        log_beta = jax.nn.log_sigmoid(z)
        log_one_minus = jnp.where(mask, jax.nn.log_sigmoid(-z), 0.0)
        suffix = lax.cumsum(log_one_minus, axis=3, reverse=True) - log_one_minus
        A = jnp.where(mask, jnp.exp(log_beta + suffix), 0.0)
        outs.append(jnp.einsum('bhqk,bkhd->bqhd', A.astype(v.dtype), vv))
    return jnp.concatenate(outs, axis=1)


def _mixer(h, w_in, w_ret_o, w_sb_o, w_out):
    B, S, _ = h.shape
    proj = jnp.einsum('bsd,df->bsf', h, w_in)
    rq, rk, rv, rg, sq, sk, sv, gr, gs = _split_cols(proj)
    rq = _rope(rq.reshape(B, S, RET_HEADS, RET_DK))
    rk = _rope(rk.reshape(B, S, RET_HEADS, RET_DK)) * (RET_DK ** -0.5)
    rv = rv.reshape(B, S, RET_HEADS, RET_DV).astype(jnp.float32)
    ret = _head_norm(_retention_chunkwise(rq, rk, rv)).reshape(B, S, RET_V).astype(h.dtype)
    ret = ret * jax.nn.silu(rg)
    ret = jnp.einsum('bsv,vd->bsd', ret, w_ret_o)
    sb = _stick_breaking(sq.reshape(B, S, SB_HEADS, SB_DH),
                         sk.reshape(B, S, SB_HEADS, SB_DH),
                         sv.reshape(B, S, SB_HEADS, SB_DH)).reshape(B, S, SB_W)
    sb = jnp.einsum('bsv,vd->bsd', sb, w_sb_o)
    merged = jax.nn.sigmoid(gr) * ret + jax.nn.sigmoid(gs) * sb
    return jnp.einsum('bsd,de->bse', merged, w_out)


def _swiglu(h, w_gate_up, w_down):
    gu = jnp.einsum('bsd,df->bsf', h, w_gate_up)
    g, u = gu[..., :FF], gu[..., FF:]
    return jnp.einsum('bsf,fd->bsd', jax.nn.silu(g) * u, w_down)


def setup_inputs(seed: int = 0) -> dict:
    key = jax.random.key(seed)
    ks = jax.random.split(key, 10)
    f32 = jnp.float32

    def w(k, shape, fan_in):
        return jax.random.normal(k, shape, f32) * (fan_in ** -0.5)

    return {
        "x": jax.random.normal(ks[0], (BATCH, SEQ, D_MODEL), f32),
        "norm_mix": 1.0 + 0.02 * jax.random.normal(ks[1], (DEPTH, D_MODEL), f32),
        "w_in": w(ks[2], (DEPTH, D_MODEL, IN_COLS), D_MODEL),
        "w_ret_o": w(ks[3], (DEPTH, RET_V, D_MODEL), RET_V),
        "w_sb_o": w(ks[4], (DEPTH, SB_W, D_MODEL), SB_W),
        "w_out": w(ks[5], (DEPTH, D_MODEL, D_MODEL), D_MODEL),
        "norm_ffn": 1.0 + 0.02 * jax.random.normal(ks[6], (DEPTH, D_MODEL), f32),
        "w_gate_up": w(ks[7], (DEPTH, D_MODEL, 2 * FF), D_MODEL),
        "w_down": w(ks[8], (DEPTH, FF, D_MODEL), FF),
        "norm_final": 1.0 + 0.02 * jax.random.normal(ks[9], (D_MODEL,), f32),
    }


def reference(x, norm_mix, w_in, w_ret_o, w_sb_o, w_out, norm_ffn, w_gate_up, w_down, norm_final):
    h = x
    for layer in range(DEPTH):
        hn = _rmsnorm(h, norm_mix[layer])
        h = h + _mixer(hn, w_in[layer], w_ret_o[layer], w_sb_o[layer], w_out[layer])
        hn = _rmsnorm(h, norm_ffn[layer])
        h = h + _swiglu(hn, w_gate_up[layer], w_down[layer])
    return _rmsnorm(h, norm_final)
```

```python
import numpy as np
from contextlib import ExitStack
import concourse.bass as bass
import concourse.mybir as mybir
from concourse.bass_utils import run_bass_kernel_spmd

F32, BF16 = mybir.dt.float32, mybir.dt.bfloat16
AF = mybir.ActivationFunctionType
ALU = mybir.AluOpType
AX = mybir.AxisListType

D = 1024
NC8 = 8
RH, RDK, RDV = 4, 128, 256
SBH, SBD = 8, 64
FF = 2816
NFC = FF // 128
INC = 6656
EPS = 1e-6
O_RQ, O_RK, O_RV, O_RG, O_SQ, O_SK, O_SV, O_GR, O_GS = 0, 512, 1024, 2048, 3072, 3584, 4096, 4608, 5632


class Buf:
    __slots__ = ("w", "r", "excl")

    def __init__(self, excl=False):
        self.w = {}
        self.r = {}
        self.excl = excl


class KB:
    def __init__(self, nc, ctx):
        self.nc, self.ctx = nc, ctx
        self.engs = {"pe": nc.tensor, "act": nc.scalar, "dve": nc.vector, "pool": nc.gpsimd, "sp": nc.sync}
        self.sems = []
        self.esem = {}
        self.seen = {e: {} for e in self.engs}
        for e in self.engs:
            self.new_eng_sem(e)
        self.dpool = {}
        for q in ("sp", "pool"):
            self.dpool[q] = [[self._new_sem("d%s%d" % (q, i)), 0] for i in range(12)]
        self.dnext = {"sp": 0, "pool": 0}
        self.dsids = set(sl[0] for q in self.dpool for sl in self.dpool[q])

    def _new_sem(self, name):
        h = self.ctx.enter_context(self.nc.semaphore(name))
        self.sems.append(h)
        return len(self.sems) - 1

    def new_eng_sem(self, e):
        self.esem[e] = [self._new_sem("e%s%d" % (e, len(self.sems))), 0]

    def new_epoch(self):
        for e in ("pe", "act", "dve", "pool"):
            if self.esem[e][1] > 12000:
                self.new_eng_sem(e)

    def _waits(self, e, reads, writes):
        need = {}
        mysid0 = self.esem[e][0]
        for b in reads:
            for sid, v in b.w.items():
                need[sid] = max(need.get(sid, 0), v)
            if b.excl:
                for sid, v in b.r.items():
                    if sid != mysid0:
                        need[sid] = max(need.get(sid, 0), v)
        for b in writes:
            for sid, v in b.w.items():
                need[sid] = max(need.get(sid, 0), v)
            for sid, v in b.r.items():
                need[sid] = max(need.get(sid, 0), v)
        mysid = self.esem[e][0]
        for sid, v in need.items():
            if sid == mysid and e == "pe":
                continue
            if self.seen[e].get(sid, 0) >= v:
                continue
            self.engs[e].wait_ge(self.sems[sid], v)
            self.seen[e][sid] = v

    def op(self, e, fn, reads=(), writes=(), inc=True):
        self._waits(e, reads, writes)
        ins = fn(self.engs[e])
        mysid = self.esem[e][0]
        tgt = self.esem[e][1] + 1
        if inc:
            ins.then_inc(self.sems[mysid], 1)
            self.esem[e][1] = tgt
        for b in reads:
            b.r[mysid] = max(b.r.get(mysid, 0), tgt)
        for b in writes:
            b.w = {mysid: tgt}
            b.r = {}
        return ins

    def dma(self, q, out, in_, reads=(), writes=()):
        self._waits(q, reads, writes)
        pool = self.dpool[q]
        slot = pool[self.dnext[q] % len(pool)]
        self.dnext[q] += 1
        sid = slot[0]
        if slot[1] > 0 and self.seen[q].get(sid, 0) < slot[1] * 16:
            self.engs[q].wait_ge(self.sems[sid], slot[1] * 16)
            self.seen[q][sid] = slot[1] * 16
        ins = self.engs[q].dma_start(out=out, in_=in_)
        slot[1] += 1
        v = slot[1] * 16
        ins.then_inc(self.sems[sid], 16)
        for b in reads:
            b.r[sid] = max(b.r.get(sid, 0), v)
        for b in writes:
            b.w = {ks: kv for ks, kv in b.w.items() if ks in self.dsids}
            b.w[sid] = v
            b.r = {}

    def barrier(self):
        tg = {}
        for e in ("pe", "act", "dve", "pool"):
            sid, c = self.esem[e]
            if c > 0:
                tg[sid] = c
        for q in ("sp", "pool"):
            for sid, c in self.dpool[q]:
                if c > 0:
                    tg[sid] = c * 16
        for e in self.engs:
            for sid, v in tg.items():
                if self.seen[e].get(sid, 0) >= v:
                    continue
                self.engs[e].wait_ge(self.sems[sid], v)
                self.seen[e][sid] = v

    def mm(self, ps, psb, pairs, reads, first=True, last=True):
        n = len(pairs)
        for i, (l, r) in enumerate(pairs):
            st = first and i == 0
            sp = last and i == n - 1
            self.op("pe", lambda t, l=l, r=r, st=st, sp=sp: t.matmul(ps, l, r, start=st, stop=sp),
                    reads=reads if i == 0 else (), writes=(psb,), inc=(i == n - 1))


def host_consts(S):
    c = {}
    ar = np.arange(128)
    c["ident"] = np.eye(128, dtype=np.float32)
    c["negmask"] = np.where(ar[:, None] >= ar[None, :], -30000.0, 0.0).astype(np.float32)
    c["negu"] = np.where(ar[:, None] >= ar[None, :], -1.0, 0.0).astype(np.float32)
    prot = np.zeros((128, 128), np.float32)
    for m in range(64):
        prot[m + 64, m] = 1.0
        prot[m, m + 64] = 1.0
    c["prot"] = prot
    c["onesm"] = np.full((128, 128), 1.0 / 1024, np.float32)
    sel = np.zeros((128, 16, 48), np.float32)
    for kb in range(16):
        sel[:, kb, kb] = 1.0
        sel[:, kb, 32 + kb] = 1.0
    c["sel"] = sel.reshape(128, 16 * 48)
    ns = np.zeros((128, 16, 128), np.float32)
    for kb in range(16):
        for r in range(16):
            if r > kb:
                ns[r, kb, :] = -1.0
                ns[32 + r, kb, :] = -1.0
    c["negstep"] = ns.reshape(128, 16 * 128)
    half = 64
    inv = (1.0 / (np.float32(10000.0) ** (np.arange(half, dtype=np.float32) / np.float32(half)))).astype(np.float32)
    pos = np.arange(S, dtype=np.float32)
    ang = (pos[:, None] * inv[None, :]).astype(np.float32)
    cos = np.cos(ang).astype(np.float32).T
    sin = np.sin(ang).astype(np.float32).T
    cosf = np.concatenate([cos, cos], 0)
    sinf = np.concatenate([-sin, sin], 0)
    ksc = np.float32(RDK ** -0.5)
    c["rope"] = np.stack([cosf, sinf, cosf * ksc, sinf * ksc], 1).astype(np.float32).reshape(128, 4 * S)
    lg = np.log1p(-np.exp2(-5.0 - np.arange(RH, dtype=np.float32))).astype(np.float32)
    i = np.arange(128, dtype=np.float32)
    diff = i[None, :] - i[:, None]
    dect = np.where(diff[None] >= 0, np.exp(lg[:, None, None] * np.maximum(diff, 0.0)[None]), 0.0)
    c["dect"] = np.ascontiguousarray(dect.transpose(1, 0, 2)).astype(np.float32).reshape(128, RH * 128)
    c["kdec"] = np.exp(lg[None, :] * (127.0 - i)[:, None]).astype(np.float32)
    qdec = np.exp(lg[:, None] * (i + 1.0)[None, :]).astype(np.float32)
    c["qdec"] = np.broadcast_to(qdec[None], (128, RH, 128)).astype(np.float32).reshape(128, RH * 128).copy()
    cd = np.exp(lg * 128.0).astype(np.float32)
    return c, [float(x) for x in cd]


def build_program(NSEQ, S, DEPTH, debug=False):
    assert S % 512 == 0
    NTG = S // 512
    NTB = S // 128
    consts, CD = host_consts(S)
    nc = bass.Bass("TRN2", target_bir_lowering=False)
    ctx = ExitStack()

    def din(name, shape, dt=F32):
        return nc.dram_tensor(name, list(shape), dt, kind="ExternalInput").ap()

    def dscr(name, shape, dt=BF16):
        return nc.dram_tensor(name, list(shape), dt, kind=("ExternalOutput" if debug else "Internal")).ap()

    xT = din("xT", [NSEQ, D, S])
    outT = nc.dram_tensor("outT", [NSEQ, D, S], F32, kind="ExternalOutput").ap()
    w_in = din("w_in", [DEPTH, D, INC])
    w_ret_o = din("w_ret_o", [DEPTH, D, D])
    w_sb_o = din("w_sb_o", [DEPTH, 512, D])
    w_out = din("w_out", [DEPTH, D, D])
    w_gu = din("w_gate_up", [DEPTH, D, 2 * FF])
    w_dn = din("w_down", [DEPTH, FF, D])
    gvec = din("gvec", [128, (2 * DEPTH + 1) * 8])
    cin = {k: din("c_" + k, v.shape) for k, v in consts.items()}
    wb_in = dscr("wb_in", [DEPTH, D, INC])
    wb_ret_o = dscr("wb_ret_o", [DEPTH, D, D])
    wb_sb_o = dscr("wb_sb_o", [DEPTH, 512, D])
    wb_out = dscr("wb_out", [DEPTH, D, D])
    wb_gu = dscr("wb_gu", [DEPTH, D, 2 * FF])
    wb_dn = dscr("wb_dn", [DEPTH, FF, D])
    s_rqt = dscr("s_rqt", [4, 128, S])
    s_rkt = dscr("s_rkt", [4, 128, S])
    s_rv = dscr("s_rv", [S, 1024])
    s_rg = dscr("s_rg", [S, 1024])
    s_sqt = dscr("s_sqt", [4, 128, S])
    s_skt = dscr("s_skt", [4, 128, S])
    s_sv = dscr("s_sv", [S, 512])
    s_grt = dscr("s_grt", [8, 128, S])
    s_gst = dscr("s_gst", [8, 128, S])
    s_sbt = dscr("s_sbt", [4, 128, S])
    s_retgt = dscr("s_retgt", [8, 128, S])
    B_scr = {n: [Buf() for _ in range(16)] for n in
             ("rqt", "rkt", "rv", "rg", "sqt", "skt", "sv", "grt", "gst", "sbt", "retgt")}
    B_w = {n: Buf() for n in ("in", "ret_o", "sb_o", "out", "gu", "dn")}

    def sb(name, shape, dt):
        return ctx.enter_context(nc.sbuf_tensor(name, list(shape), dt))

    k = KB(nc, ctx)
    UID = [0]
    hT = sb("hT", [128, 8, S], F32)
    B_h = [[Buf() for _ in range(NTG)] for _ in range(8)]
    identb = sb("identb", [128, 128], BF16)
    negmaskb = sb("negmaskb", [128, 128], BF16)
    negub = sb("negub", [128, 128], BF16)
    protb = sb("protb", [128, 128], BF16)
    onesmb = sb("onesmb", [128, 128], BF16)
    selb = sb("selb", [128, 16, 48], BF16)
    negstepb = sb("negstepb", [128, 16, 128], BF16)
    dect = sb("dect", [128, RH, 128], F32)
    kdec = sb("kdec", [128, RH], F32)
    qdec = sb("qdec", [128, RH, 128], F32)
    gv = sb("gv", [128, 2 * DEPTH + 1, 8], F32)
    epsb = sb("epsb", [128, 1], F32)
    B_const = Buf()
    ps = [ctx.enter_context(nc.psum_tensor("ps%d" % i, [128, 512], F32)) for i in range(7)]
    pT = ctx.enter_context(nc.psum_tensor("pT", [128, 1024], BF16))
    B_ps = [Buf(excl=True) for _ in range(7)]
    B_pT = Buf(excl=True)
    rr = {"evac": 0}

    def evac_eng():
        rr["evac"] += 1
        return "act" if rr["evac"] % 2 else "dve"

    def copy_op(e, out, in_, reads, writes):
        if e == "act":
            k.op("act", lambda t: t.activation(out=out, in_=in_, func=AF.Copy), reads=reads, writes=writes)
        else:
            k.op(e, lambda t: t.tensor_copy(out=out, in_=in_), reads=reads, writes=writes)

    with ExitStack() as pctx:
        stg = [pctx.enter_context(nc.sbuf_tensor("stg%d" % i, [128, 2048], F32)) for i in range(2)]
        stb = [pctx.enter_context(nc.sbuf_tensor("stb%d" % i, [128, 2048], BF16)) for i in range(2)]
        B_stg = [Buf(), Buf()]
        B_stb = [Buf(), Buf()]
        cnt = [0]

        def conv(dst_sb_ap, src_dram, ncol, npart=128):
            i = cnt[0] % 2
            cnt[0] += 1
            k.dma("sp", stg[i][0:npart, 0:ncol], src_dram, writes=(B_stg[i],))
            copy_op("dve", dst_sb_ap, stg[i][0:npart, 0:ncol], (B_stg[i],), (B_const,))

        conv(identb[:], cin["ident"], 128)
        conv(negmaskb[:], cin["negmask"], 128)
        conv(negub[:], cin["negu"], 128)
        conv(protb[:], cin["prot"], 128)
        conv(onesmb[:], cin["onesm"], 128)
        conv(selb[:].rearrange("p a b -> p (a b)"), cin["sel"], 16 * 48)
        conv(negstepb[:].rearrange("p a b -> p (a b)"), cin["negstep"], 2048)
        k.dma("sp", dect[:].rearrange("p a b -> p (a b)"), cin["dect"], writes=(B_const,))
        k.dma("sp", kdec[:], cin["kdec"], writes=(B_const,))
        k.dma("sp", qdec[:].rearrange("p a b -> p (a b)"), cin["qdec"], writes=(B_const,))
        k.dma("sp", gv[:].rearrange("p a b -> p (a b)"), gvec, writes=(B_const,))
        k.op("dve", lambda t: t.memset(epsb[:], EPS), writes=(B_const,))

        def conv_w(dst, src, R, C):
            for r0 in range(0, R, 128):
                for c0 in range(0, C, 2048):
                    cw = min(2048, C - c0)
                    i = cnt[0] % 2
                    e = ("dve", "act", "pool")[cnt[0] % 3]
                    cnt[0] += 1
                    k.dma("sp", stg[i][:, 0:cw], src[r0:r0 + 128, c0:c0 + cw], writes=(B_stg[i],))
                    copy_op(e, stb[i][:, 0:cw], stg[i][:, 0:cw], (B_stg[i],), (B_stb[i],))
                    k.dma("pool", dst[r0:r0 + 128, c0:c0 + cw], stb[i][:, 0:cw], reads=(B_stb[i],),
                          writes=(B_w["in"],))

        for l in range(DEPTH):
            conv_w(wb_in[l], w_in[l], D, INC)
            conv_w(wb_ret_o[l], w_ret_o[l], D, D)
            conv_w(wb_sb_o[l], w_sb_o[l], 512, D)
            conv_w(wb_out[l], w_out[l], D, D)
            conv_w(wb_gu[l], w_gu[l], D, 2 * FF)
            conv_w(wb_dn[l], w_dn[l], FF, D)
        k.barrier()

    if debug == "p0":
        return nc, ctx, consts
    def rmsnorm_tg(tg, gidx, sq, B_sq, rstd, B_rstd, out_fn):
        cols = slice(tg * 512, (tg + 1) * 512)
        k.op("act", lambda t: t.activation(out=sq[:], in_=hT[:, :, cols], func=AF.Square),
             reads=[B_h[c][tg] for c in range(8)], writes=(B_sq,))
        k.mm(ps[0][:], B_ps[0], [(onesmb[:], sq[:, c, :]) for c in range(8)], reads=(B_sq, B_const))
        k.op("act", lambda t: t.activation(out=rstd[:], in_=ps[0][:], func=AF.Ln, bias=epsb[:]),
             reads=(B_ps[0], B_const), writes=(B_rstd,))
        k.op("act", lambda t: t.activation(out=rstd[:], in_=rstd[:], func=AF.Exp, scale=-0.5),
             reads=(B_rstd,), writes=(B_rstd,))
        for c in range(8):
            o, ob = out_fn(c)
            k.op("dve", lambda t, o=o, c=c: t.scalar_tensor_tensor(
                out=o, in0=hT[:, c, cols], scalar=gv[:, gidx, c:c + 1], in1=rstd[:], op0=ALU.mult, op1=ALU.mult),
                reads=(B_h[c][tg], B_rstd, B_const), writes=ob)

    def layer(l):
        with ExitStack() as p:
            def al(name, shape, dt):
                UID[0] += 1
                return p.enter_context(nc.sbuf_tensor("%s_%d" % (name, UID[0]), list(shape), dt))
            hnT = al("hnT", [128, 8, S], BF16)
            B_hn = [Buf() for _ in range(NTG)]
            sq = al("sq", [128, 8, 512], BF16)
            rstd = al("rstd", [128, 512], F32)
            B_sq, B_rstd = Buf(), Buf()
            for tg in range(NTG):
                rmsnorm_tg(tg, l, sq, B_sq, rstd, B_rstd,
                           lambda c, tg=tg: (hnT[:, c, tg * 512:(tg + 1) * 512], (B_hn[tg],)))
            wt = [al("wt%d" % i, [128, 8, 512], BF16) for i in range(2)]
            B_wt = [Buf(), Buf()]
            ost = [al("ost%d" % i, [128, max(S, 2048)], BF16) for i in range(2)]
            B_ost = [Buf(), Buf()]
            rot = al("rot", [128, 4, 512], F32)
            B_rot = Buf()
            xb = al("xb", [128, 512], BF16)
            B_xb = Buf()
            t1 = al("t1", [128, 512], F32)
            t2 = al("t2", [128, 512], F32)
            B_t1, B_t2 = Buf(), Buf()
            st = {"ps": 1, "ost": 0}

            def next_ps():
                st["ps"] = 1 + (st["ps"] % 5)
                return st["ps"]

            def load_w(gi):
                i = gi % 2
                k.dma("sp", wt[i][:], wb_in[l][:, gi * 512:(gi + 1) * 512].rearrange("(c p) n -> p c n", p=128),
                      reads=(B_w["in"],), writes=(B_wt[i],))

            NG = INC // 512
            load_w(0)
            rope_v = cin["rope"].rearrange("p (a s) -> p a s", a=4)
            for gi in range(NG):
                if isinstance(debug, str) and debug.startswith("g") and gi >= int(debug[1:2]):
                    break
                stage = debug[2:] if isinstance(debug, str) and debug.startswith("g") else ""

                if gi + 1 < NG:
                    load_w(gi + 1)
                w = wt[gi % 2]
                Bw = B_wt[gi % 2]
                col0 = gi * 512
                if col0 in (O_RQ, O_RK):
                    isk = col0 == O_RK
                    dst = s_rkt if isk else s_rqt
                    Bd = B_scr["rkt" if isk else "rqt"]
                    for tg in range(NTG):
                        k.dma("sp", rot[:], rope_v[:, :, tg * 512:(tg + 1) * 512], writes=(B_rot,))
                        for h in range(4):
                            pi = next_ps()
                            k.mm(ps[pi][:], B_ps[pi],
                                 [(w[:, c, h * 128:(h + 1) * 128], hnT[:, c, tg * 512:(tg + 1) * 512]) for c in range(8)],
                                 reads=(Bw, B_hn[tg]))
                            if stage == "a":
                                continue
                            copy_op("act", xb[:], ps[pi][:], (B_ps[pi],), (B_xb,))
                            if stage == "b":
                                continue
                            k.mm(ps[6][:], B_ps[6], [(protb[:], xb[:])], reads=(B_xb, B_const))
                            if stage == "c":
                                continue
                            tb = 2 if isk else 0
                            k.op("dve", lambda t, pi=pi, tb=tb: t.tensor_tensor(
                                out=t1[:], in0=ps[pi][:], in1=rot[:, tb, :], op=ALU.mult),
                                reads=(B_ps[pi], B_rot), writes=(B_t1,))
                            k.op("dve", lambda t, tb=tb: t.tensor_tensor(
                                out=t2[:], in0=ps[6][:], in1=rot[:, tb + 1, :], op=ALU.mult),
                                reads=(B_ps[6], B_rot), writes=(B_t2,))
                            if stage == "d":
                                continue
                            o = ost[h % 2]
                            k.op("dve", lambda t, o=o, tg=tg: t.tensor_tensor(
                                out=o[:, tg * 512:(tg + 1) * 512], in0=t1[:], in1=t2[:], op=ALU.add),
                                reads=(B_t1, B_t2), writes=(B_ost[h % 2],))
                            if stage == "e":
                                continue
                            k.dma("pool", dst[h][:, tg * 512:(tg + 1) * 512], o[:, tg * 512:(tg + 1) * 512],
                                  reads=(B_ost[h % 2],), writes=(Bd[h],))
                elif col0 in (O_SQ, O_SK, O_GR, O_GR + 512, O_GS, O_GS + 512):
                    if col0 == O_SQ:
                        dst, Bd, base, fn = s_sqt, B_scr["sqt"], 0, AF.Identity
                    elif col0 == O_SK:
                        dst, Bd, base, fn = s_skt, B_scr["skt"], 0, AF.Copy
                    elif col0 >= O_GS:
                        dst, Bd, base, fn = s_gst, B_scr["gst"], (col0 - O_GS) // 128, AF.Sigmoid
                    else:
                        dst, Bd, base, fn = s_grt, B_scr["grt"], (col0 - O_GR) // 128, AF.Sigmoid
                    for fc in range(4):
                        oi = st["ost"] % 2
                        st["ost"] += 1
                        o = ost[oi]
                        for tg in range(NTG):
                            pi = next_ps()
                            k.mm(ps[pi][:], B_ps[pi],
                                 [(w[:, c, fc * 128:(fc + 1) * 128], hnT[:, c, tg * 512:(tg + 1) * 512]) for c in range(8)],
                                 reads=(Bw, B_hn[tg]))
                            if fn == AF.Copy:
                                copy_op(evac_eng(), o[:, tg * 512:(tg + 1) * 512], ps[pi][:], (B_ps[pi],), (B_ost[oi],))
                            elif fn == AF.Identity:
                                k.op("dve", lambda t, o=o, tg=tg, pi=pi: t.tensor_scalar(
                                    out=o[:, tg * 512:(tg + 1) * 512], in0=ps[pi][:], scalar1=0.125, scalar2=0.0,
                                    op0=ALU.mult, op1=ALU.add),
                                    reads=(B_ps[pi],), writes=(B_ost[oi],))
                            else:
                                k.op("act", lambda t, o=o, tg=tg, pi=pi: t.activation(
                                    out=o[:, tg * 512:(tg + 1) * 512], in_=ps[pi][:], func=AF.Sigmoid),
                                    reads=(B_ps[pi],), writes=(B_ost[oi],))
                        k.dma("pool", dst[base + fc], o[:, 0:S], reads=(B_ost[oi],), writes=(Bd[base + fc],))
                else:
                    if col0 >= O_SV:
                        dst, Bd, cb, fn = s_sv, B_scr["sv"], 0, AF.Copy
                    elif col0 >= O_RG:
                        dst, Bd, cb, fn = s_rg, B_scr["rg"], col0 - O_RG, AF.Silu
                    else:
                        dst, Bd, cb, fn = s_rv, B_scr["rv"], col0 - O_RV, AF.Copy
                    for tq in range(NTB // 4):
                        oi = st["ost"] % 2
                        st["ost"] += 1
                        o = ost[oi]
                        for tb4 in range(4):
                            tb = tq * 4 + tb4
                            pi = next_ps()
                            k.mm(ps[pi][:], B_ps[pi],
                                 [(hnT[:, c, tb * 128:(tb + 1) * 128], w[:, c, :]) for c in range(8)],
                                 reads=(Bw, B_hn[tb // 4]))
                            if fn == AF.Copy:
                                copy_op(evac_eng(), o[:, tb4 * 512:(tb4 + 1) * 512], ps[pi][:], (B_ps[pi],), (B_ost[oi],))
                            else:
                                k.op("act", lambda t, o=o, tb4=tb4, pi=pi: t.activation(
                                    out=o[:, tb4 * 512:(tb4 + 1) * 512], in_=ps[pi][:], func=AF.Silu),
                                    reads=(B_ps[pi],), writes=(B_ost[oi],))
                        k.dma("pool",
                              dst[tq * 512:(tq + 1) * 512, cb:cb + 512].rearrange("(t p) n -> p t n", p=128),
                              o[:, 0:2048].rearrange("p (t n) -> p t n", n=512),
                              reads=(B_ost[oi],), writes=(Bd[tq],))
            k.barrier()
        if debug == "p1" or (isinstance(debug, str) and debug.startswith("g")):
            return
        sb_phase(l)
        if debug == "p2":
            return
        ret_phase(l)
        if debug == "p3":
            return
        post_phase(l)
        if debug == "p4":
            return
        ffn_phase(l)

    def sb_phase(l):
        with ExitStack() as p:
            def al(name, shape, dt):
                UID[0] += 1
                return p.enter_context(nc.sbuf_tensor("%s_%d" % (name, UID[0]), list(shape), dt))
            qa = al("qa", [128, S], BF16)
            qb = al("qb", [128, S], BF16)
            kt = al("kt", [128, S], BF16)
            sv = al("sv", [128, NTB, 128], BF16)
            B_q, B_k, B_v = Buf(), Buf(), Buf()
            spb = [al("spb%d" % i, [128, NTB, 512], BF16) for i in range(2)]
            B_sp = [Buf(), Buf()]
            et = [al("et%d" % i, [128, 512], F32) for i in range(2)]
            B_et = [Buf(), Buf()]
            at = [al("at%d" % i, [128, 512], BF16) for i in range(3)]
            B_at = [Buf(), Buf(), Buf()]
            hl = [al("hl%d" % i, [128, 512], BF16) for i in range(2)]
            B_hl = [Buf(), Buf()]
            osb = al("osb", [128, S], BF16)
            B_osb = Buf()
            for i in range(2):
                k.op("dve", lambda t, i=i: t.memset(hl[i][:], 0.0), writes=(B_hl[i],))
            k.op("dve", lambda t: t.memset(qa[64:128, :], 0.0), writes=(B_q,))
            k.op("dve", lambda t: t.memset(qb[0:64, :], 0.0), writes=(B_q,))
            cnt = {"z": 0, "t": 0, "e": 0, "a": 0}
            for c in range(4):
                k.dma("sp", qa[0:64, :], s_sqt[c][0:64, :], reads=(B_scr["sqt"][c],), writes=(B_q,))
                k.dma("sp", qb[64:128, :], s_sqt[c][64:128, :], reads=(B_scr["sqt"][c],), writes=(B_q,))
                k.dma("sp", kt[:], s_skt[c], reads=(B_scr["skt"][c],), writes=(B_k,))
                k.dma("sp", sv[:], s_sv[:, c * 128:(c + 1) * 128].rearrange("(t p) n -> p t n", p=128),
                      reads=[B_scr["sv"][i] for i in range(NTB // 4)], writes=(B_v,))
                units = [(g, hh) for g in range(NTG) for hh in range(2)]

                def geom(g, kb):
                    lo = max(0, 128 * kb - 512 * g)
                    return lo, kb >= 4 * g

                def p1_block(u, kb):
                    g, hh = units[u]
                    q = qa if hh == 0 else qb
                    spt, Bs = spb[u % 2], B_sp[u % 2]
                    lo, diag = geom(g, kb)
                    zi = cnt["z"] % 2
                    cnt["z"] += 1
                    z, Bz = ps[zi], B_ps[zi]
                    ks = kt[:, kb * 128:(kb + 1) * 128]
                    q0 = g * 512
                    if diag:
                        k.mm(z[:, lo:lo + 128], Bz, [(identb[:], negmaskb[:]), (ks, q[:, q0 + lo:q0 + lo + 128])],
                             reads=(B_const, B_k, B_q))
                        if lo + 128 < 512:
                            k.mm(z[:, lo + 128:512], Bz, [(ks, q[:, q0 + lo + 128:q0 + 512])], reads=(B_k, B_q))
                    else:
                        k.mm(z[:, lo:512], Bz, [(ks, q[:, q0 + lo:q0 + 512])], reads=(B_k, B_q))
                    ei = cnt["e"] % 2
                    cnt["e"] += 1
                    k.op("act", lambda t: t.activation(out=et[ei][:, lo:512], in_=z[:, lo:512], func=AF.Exp),
                         reads=(Bz,), writes=(B_et[ei],))
                    k.op("act", lambda t: t.activation(out=spt[:, kb, lo:512], in_=et[ei][:, lo:512], func=AF.Ln, bias=1.0),
                         reads=(B_et[ei],), writes=(Bs,))
                    nkb = 4 * g + 4
                    k.op("pe", lambda t: t.matmul(ps[4][0:48, lo:512], selb[:, kb, :], spt[:, kb, lo:512],
                                                   start=(kb == 0), stop=(kb == nkb - 1)),
                         reads=(Bs, B_const), writes=(B_ps[4],))

                def p1_fin(u):
                    h_, Bh = hl[u % 2], B_hl[u % 2]
                    k.op("dve", lambda t: t.tensor_copy(out=h_[0:48, :], in_=ps[4][0:48, :]),
                         reads=(B_ps[4],), writes=(Bh,))
                    k.op("dve", lambda t: t.tensor_tensor(out=h_[32:48, :], in0=ps[4][32:48, :], in1=h_[32:48, :],
                                                          op=ALU.subtract),
                         reads=(B_ps[4], Bh), writes=(Bh,))

                def p2_block(u, kb):
                    g, hh = units[u]
                    q = qa if hh == 0 else qb
                    spt, Bs = spb[u % 2], B_sp[u % 2]
                    h_, Bh = hl[u % 2], B_hl[u % 2]
                    lo, diag = geom(g, kb)
                    ti = 2 + cnt["t"] % 2
                    cnt["t"] += 1
                    T, Bt = ps[ti], B_ps[ti]
                    ks = kt[:, kb * 128:(kb + 1) * 128]
                    q0 = g * 512
                    rd = (B_const, B_k, B_q, Bs, Bh)

                    def grp(a, b, withmask):
                        prs = []
                        if withmask:
                            prs.append((identb[:], negmaskb[:]))
                        prs.append((ks, q[:, q0 + a:q0 + b]))
                        prs.append((negub[:], spt[:, kb, a:b]))
                        prs.append((negstepb[:, kb, :], h_[:, a:b]))
                        k.mm(T[:, a:b], Bt, prs, reads=rd)
                    if diag:
                        grp(lo, lo + 128, True)
                        if lo + 128 < 512:
                            grp(lo + 128, 512, False)
                    else:
                        grp(lo, 512, False)
                    ai = cnt["a"] % 3
                    cnt["a"] += 1
                    k.op("act", lambda t: t.activation(out=at[ai][:, lo:512], in_=T[:, lo:512], func=AF.Exp),
                         reads=(Bt,), writes=(B_at[ai],))
                    nkb = 4 * g + 4
                    k.op("pe", lambda t: t.matmul(ps[5][hh * 64:(hh + 1) * 64, lo:512], sv[:, kb, hh * 64:(hh + 1) * 64],
                                                   at[ai][:, lo:512], start=(kb == 0), stop=(kb == nkb - 1)),
                         reads=(B_at[ai], B_v), writes=(B_ps[5],))

                def p2_fin(u):
                    g, hh = units[u]
                    if hh == 1:
                        copy_op("dve", osb[:, g * 512:(g + 1) * 512], ps[5][:], (B_ps[5],), (B_osb,))

                nu = len(units)
                for u in range(nu + 1):
                    n1 = 4 * units[u][0] + 4 if u < nu else 0
                    n2 = 4 * units[u - 1][0] + 4 if u >= 1 else 0
                    for i in range(max(n1, n2)):
                        if i < n1:
                            p1_block(u, i)
                        if i < n2:
                            p2_block(u - 1, i)
                    if u < nu:
                        p1_fin(u)
                    if u >= 1:
                        p2_fin(u - 1)
                k.dma("pool", s_sbt[c], osb[:], reads=(B_osb,), writes=(B_scr["sbt"][c],))
            k.barrier()

    def ret_phase(l):
        with ExitStack() as p:
            def al(name, shape, dt):
                UID[0] += 1
                return p.enter_context(nc.sbuf_tensor("%s_%d" % (name, UID[0]), list(shape), dt))
            rq = al("rq", [128, S], BF16)
            rk = al("rk", [128, S], BF16)
            rv = al("rv", [128, NTB, 256], BF16)
            rg = al("rg", [128, NTB, 256], BF16)
            B_in = Buf()
            obuf = al("obuf", [128, NTB, 256], F32)
            sqb = al("sqb", [128, NTB, 256], F32)
            yb = al("yb", [128, NTB, 256], BF16)
            B_ob, B_sqb, B_yb = Buf(), Buf(), Buf()
            stt = [al("stt%d" % i, [128, 128], BF16) for i in range(2)]
            B_stt = [Buf(), Buf()]
            qd = [al("qd%d" % i, [128, 128], BF16) for i in range(2)]
            B_qd = [Buf(), Buf()]
            kd = [al("kd%d" % i, [128, 128], BF16) for i in range(2)]
            B_kd = [Buf(), Buf()]
            Rf = al("Rf", [128, 256], F32)
            Rb = [al("Rb%d" % i, [128, 256], BF16) for i in range(2)]
            B_Rf = Buf()
            B_Rb = [Buf(), Buf()]
            stats = al("stats", [128, 6, NTB], F32)
            B_stats = Buf()
            ogt = al("ogt", [128, 2, S], BF16)
            B_ogt = Buf()
            for h in range(RH):
                k.dma("sp", rq[:], s_rqt[h], reads=(B_scr["rqt"][h],), writes=(B_in,))
                k.dma("sp", rk[:], s_rkt[h], reads=(B_scr["rkt"][h],), writes=(B_in,))
                k.dma("sp", rv[:], s_rv[:, h * 256:(h + 1) * 256].rearrange("(t p) n -> p t n", p=128),
                      reads=[B_scr["rv"][i] for i in range(NTB // 4)], writes=(B_in,))
                k.dma("sp", rg[:], s_rg[:, h * 256:(h + 1) * 256].rearrange("(t p) n -> p t n", p=128),
                      reads=[B_scr["rg"][i] for i in range(NTB // 4)], writes=(B_in,))
                for n in range(NTB):
                    cs = slice(n * 128, (n + 1) * 128)
                    i2 = n % 2
                    k.mm(ps[i2][:, 0:128], B_ps[i2], [(rk[:, cs], rq[:, cs])], reads=(B_in,))
                    k.op("dve", lambda t, i2=i2: t.tensor_tensor(out=stt[i2][:], in0=ps[i2][:, 0:128], in1=dect[:, h, :],
                                                                 op=ALU.mult),
                         reads=(B_ps[i2], B_const), writes=(B_stt[i2],))
                    oi = 2 + i2
                    if n > 0:
                        k.op("dve", lambda t, i2=i2, cs=cs: t.tensor_tensor(out=qd[i2][:], in0=rq[:, cs], in1=qdec[:, h, :],
                                                                            op=ALU.mult),
                             reads=(B_in, B_const), writes=(B_qd[i2],))
                        k.mm(ps[oi][:, 0:256], B_ps[oi], [(stt[i2][:], rv[:, n, :]), (qd[i2][:], Rb[(n - 1) % 2][:])],
                             reads=(B_stt[i2], B_in, B_qd[i2], B_Rb[(n - 1) % 2]))
                    else:
                        k.mm(ps[oi][:, 0:256], B_ps[oi], [(stt[i2][:], rv[:, n, :])], reads=(B_stt[i2], B_in))
                    copy_op("act", obuf[:, n, :], ps[oi][:, 0:256], (B_ps[oi],), (B_ob,))
                    if n < NTB - 1:
                        k.op("pe", lambda t, cs=cs: t.transpose(out=pT[:, 0:128], in_=rk[:, cs], identity=identb[:]),
                             reads=(B_in, B_const), writes=(B_pT,))
                        k.op("dve", lambda t, i2=i2: t.tensor_scalar(out=kd[i2][:], in0=pT[:, 0:128],
                                                                     scalar1=kdec[:, h:h + 1], scalar2=0.0, op0=ALU.mult, op1=ALU.add),
                             reads=(B_pT, B_const), writes=(B_kd[i2],))
                        k.mm(ps[4][:, 0:256], B_ps[4], [(kd[i2][:], rv[:, n, :])], reads=(B_kd[i2], B_in))
                        if n == 0:
                            copy_op("dve", Rf[:], ps[4][:, 0:256], (B_ps[4],), (B_Rf,))
                        else:
                            k.op("dve", lambda t: t.scalar_tensor_tensor(out=Rf[:], in0=Rf[:], scalar=CD[h],
                                                                         in1=ps[4][:, 0:256], op0=ALU.mult, op1=ALU.add),
                                 reads=(B_ps[4], B_Rf), writes=(B_Rf,))
                        copy_op("act", Rb[n % 2][:], Rf[:], (B_Rf,), (B_Rb[n % 2],))
                k.op("dve", lambda t: t.tensor_reduce(out=stats[:, 0, :], in_=obuf[:], axis=AX.X, op=ALU.add),
                     reads=(B_ob,), writes=(B_stats,))
                k.op("act", lambda t: t.activation(out=sqb[:], in_=obuf[:], func=AF.Square), reads=(B_ob,), writes=(B_sqb,))
                k.op("dve", lambda t: t.tensor_reduce(out=stats[:, 1, :], in_=sqb[:], axis=AX.X, op=ALU.add),
                     reads=(B_sqb,), writes=(B_stats,))
                k.op("dve", lambda t: t.tensor_scalar(out=stats[:, 2, :], in0=stats[:, 0, :], scalar1=1.0 / 256, scalar2=0.0,
                                                      op0=ALU.mult, op1=ALU.add), reads=(B_stats,), writes=(B_stats,))
                k.op("dve", lambda t: t.tensor_tensor(out=stats[:, 3, :], in0=stats[:, 2, :], in1=stats[:, 2, :], op=ALU.mult),
                     reads=(B_stats,), writes=(B_stats,))
                k.op("dve", lambda t: t.scalar_tensor_tensor(out=stats[:, 3, :], in0=stats[:, 1, :], scalar=1.0 / 256,
                                                             in1=stats[:, 3, :], op0=ALU.mult, op1=ALU.subtract),
                     reads=(B_stats,), writes=(B_stats,))
                k.op("act", lambda t: t.activation(out=stats[:, 4, :], in_=stats[:, 3, :], func=AF.Ln, bias=epsb[:]),
                     reads=(B_stats, B_const), writes=(B_stats,))
                k.op("act", lambda t: t.activation(out=stats[:, 4, :], in_=stats[:, 4, :], func=AF.Exp, scale=-0.5),
                     reads=(B_stats,), writes=(B_stats,))
                k.op("dve", lambda t: t.scalar_tensor_tensor(out=stats[:, 5, :], in0=stats[:, 2, :], scalar=-1.0,
                                                             in1=stats[:, 4, :], op0=ALU.mult, op1=ALU.mult),
                     reads=(B_stats,), writes=(B_stats,))
                for n in range(NTB):
                    e = "act" if n % 2 else "dve"
                    if e == "act":
                        k.op("act", lambda t, n=n: t.activation(out=sqb[:, n, :], in_=obuf[:, n, :], func=AF.Identity,
                                                                scale=stats[:, 4, n:n + 1], bias=stats[:, 5, n:n + 1]),
                             reads=(B_ob, B_stats), writes=(B_sqb,))
                    else:
                        k.op("dve", lambda t, n=n: t.tensor_scalar(out=sqb[:, n, :], in0=obuf[:, n, :],
                                                                   scalar1=stats[:, 4, n:n + 1], scalar2=stats[:, 5, n:n + 1],
                                                                   op0=ALU.mult, op1=ALU.add),
                             reads=(B_ob, B_stats), writes=(B_sqb,))
                k.op("dve", lambda t: t.tensor_tensor(out=yb[:], in0=sqb[:], in1=rg[:], op=ALU.mult),
                     reads=(B_sqb, B_in), writes=(B_yb,))
                for ec in range(2):
                    for n0 in range(0, NTB, 8):
                        nn = min(8, NTB - n0)
                        for j in range(nn):
                            k.op("pe", lambda t, j=j, n0=n0: t.transpose(out=pT[:, j * 128:(j + 1) * 128],
                                                                       in_=yb[:, n0 + j, ec * 128:(ec + 1) * 128],
                                                                       identity=identb[:]),
                                 reads=(B_yb, B_const), writes=(B_pT,), inc=(j == nn - 1))
                        copy_op(evac_eng(), ogt[:, ec, n0 * 128:(n0 + nn) * 128], pT[:, 0:nn * 128], (B_pT,), (B_ogt,))
                for ec in range(2):
                    k.dma("pool", s_retgt[h * 2 + ec], ogt[:, ec, :], reads=(B_ogt,), writes=(B_scr["retgt"][h * 2 + ec],))
            k.barrier()

    def post_phase(l):
        with ExitStack() as p:
            def al(name, shape, dt):
                UID[0] += 1
                return p.enter_context(nc.sbuf_tensor("%s_%d" % (name, UID[0]), list(shape), dt))
            wsbo = al("wsbo", [128, 4, D], BF16)
            wreto = al("wreto", [128, 8, D], BF16)
            wout = al("wout", [128, 8, D], BF16)
            B_wp = Buf()
            k.dma("sp", wsbo[:], wb_sb_o[l].rearrange("(c p) n -> p c n", p=128), reads=(B_w["in"],), writes=(B_wp,))
            k.dma("sp", wreto[:], wb_ret_o[l].rearrange("(c p) n -> p c n", p=128), reads=(B_w["in"],), writes=(B_wp,))
            k.dma("sp", wout[:], wb_out[l].rearrange("(c p) n -> p c n", p=128), reads=(B_w["in"],), writes=(B_wp,))
            sbt = al("sbt", [128, 4, 512], BF16)
            rgt = al("rgt", [128, 8, 512], BF16)
            grt = al("grt", [128, 8, 512], BF16)
            gst = al("gst", [128, 8, 512], BF16)
            B_ld = Buf()
            t1 = [al("pt1%d" % i, [128, 512], F32) for i in range(2)]
            t2 = [al("pt2%d" % i, [128, 512], F32) for i in range(2)]
            B_t1 = [Buf(), Buf()]
            B_t2 = [Buf(), Buf()]
            mt = al("mt", [128, 8, 512], BF16)
            B_mt = Buf()
            pc = [0]
            for tg in range(NTG):
                cols = slice(tg * 512, (tg + 1) * 512)
                k.dma("sp", sbt[:], s_sbt[:, :, cols].rearrange("c p n -> p c n"),
                      reads=B_scr["sbt"][0:4], writes=(B_ld,))
                k.dma("sp", rgt[:], s_retgt[:, :, cols].rearrange("c p n -> p c n"),
                      reads=B_scr["retgt"][0:8], writes=(B_ld,))
                k.dma("sp", grt[:], s_grt[:, :, cols].rearrange("c p n -> p c n"),
                      reads=B_scr["grt"][0:8], writes=(B_ld,))
                k.dma("sp", gst[:], s_gst[:, :, cols].rearrange("c p n -> p c n"),
                      reads=B_scr["gst"][0:8], writes=(B_ld,))
                for ec in range(8):
                    i2 = ec % 2
                    p1, p2 = i2 * 2, i2 * 2 + 1
                    es = slice(ec * 128, (ec + 1) * 128)
                    k.mm(ps[p1][:], B_ps[p1], [(wsbo[:, c, es], sbt[:, c, :]) for c in range(4)], reads=(B_wp, B_ld))
                    k.mm(ps[p2][:], B_ps[p2], [(wreto[:, c, es], rgt[:, c, :]) for c in range(8)], reads=(B_wp, B_ld))
                    k.op("dve", lambda t, p1=p1, i2=i2, ec=ec: t.tensor_tensor(out=t1[i2][:], in0=ps[p1][:], in1=gst[:, ec, :],
                                                                               op=ALU.mult),
                         reads=(B_ps[p1], B_ld), writes=(B_t1[i2],))
                    k.op("dve", lambda t, p2=p2, i2=i2, ec=ec: t.tensor_tensor(out=t2[i2][:], in0=ps[p2][:], in1=grt[:, ec, :],
                                                                               op=ALU.mult),
                         reads=(B_ps[p2], B_ld), writes=(B_t2[i2],))
                    k.op("pool", lambda t, i2=i2, ec=ec: t.tensor_tensor(out=mt[:, ec, :], in0=t1[i2][:], in1=t2[i2][:],
                                                                         op=ALU.add),
                         reads=(B_t1[i2], B_t2[i2]), writes=(B_mt,))
                for dc in range(8):
                    pi = 4 + dc % 2
                    k.mm(ps[pi][:], B_ps[pi], [(wout[:, c, dc * 128:(dc + 1) * 128], mt[:, c, :]) for c in range(8)],
                         reads=(B_wp, B_mt))
                    k.op("dve", lambda t, pi=pi, dc=dc: t.tensor_tensor(out=hT[:, dc, cols], in0=ps[pi][:], in1=hT[:, dc, cols],
                                                                        op=ALU.add),
                         reads=(B_ps[pi], B_h[dc][tg]), writes=(B_h[dc][tg],))
            k.barrier()

    def ffn_phase(l):
        with ExitStack() as p:
            def al(name, shape, dt):
                UID[0] += 1
                return p.enter_context(nc.sbuf_tensor("%s_%d" % (name, UID[0]), list(shape), dt))
            hn = al("hn2", [128, 8, 512], BF16)
            B_hn = Buf()
            sq = al("sq2", [128, 8, 512], BF16)
            rstd = al("rstd2", [128, 512], F32)
            B_sq, B_rstd = Buf(), Buf()
            wgu = [al("wgu%d" % i, [128, 8, 512], BF16) for i in range(2)]
            B_wgu = [Buf(), Buf()]
            wd = [al("wd%d" % i, [128, NFC, 256], BF16) for i in range(2)]
            B_wd = [Buf(), Buf()]
            actT = al("actT", [128, NFC, 512], BF16)
            B_act = Buf()
            sg = [al("sg%d" % i, [128, 512], F32) for i in range(2)]
            B_sg = [Buf(), Buf()]
            wc = [0, 0]
            for tg in range(NTG):
                cols = slice(tg * 512, (tg + 1) * 512)
                rmsnorm_tg(tg, DEPTH + l, sq, B_sq, rstd, B_rstd, lambda c: (hn[:, c, :], (B_hn,)))

                def load_gu(fg):
                    i = wc[0] % 2
                    wc[0] += 1
                    k.dma("sp", wgu[i][:, :, 0:256],
                          wb_gu[l][:, fg * 256:(fg + 1) * 256].rearrange("(c p) n -> p c n", p=128),
                          reads=(B_w["in"],), writes=(B_wgu[i],))
                    k.dma("sp", wgu[i][:, :, 256:512],
                          wb_gu[l][:, FF + fg * 256:FF + (fg + 1) * 256].rearrange("(c p) n -> p c n", p=128),
                          reads=(B_w["in"],), writes=(B_wgu[i],))
                    return i
                nfg = NFC // 2
                cur = load_gu(0)
                for fg in range(nfg):
                    nxt = load_gu(fg + 1) if fg + 1 < nfg else None
                    w, Bw = wgu[cur], B_wgu[cur]
                    for j in range(2):
                        fcn = fg * 2 + j
                        i2 = fcn % 2
                        pg, pu = 1 + i2 * 2, 2 + i2 * 2
                        k.mm(ps[pg][:], B_ps[pg], [(w[:, c, j * 128:(j + 1) * 128], hn[:, c, :]) for c in range(8)],
                             reads=(Bw, B_hn))
                        k.mm(ps[pu][:], B_ps[pu], [(w[:, c, 256 + j * 128:256 + (j + 1) * 128], hn[:, c, :]) for c in range(8)],
                             reads=(Bw, B_hn))
                        k.op("act", lambda t, i2=i2, pg=pg: t.activation(out=sg[i2][:], in_=ps[pg][:], func=AF.Silu),
                             reads=(B_ps[pg],), writes=(B_sg[i2],))
                        k.op("dve", lambda t, i2=i2, pu=pu, fcn=fcn: t.tensor_tensor(out=actT[:, fcn, :], in0=ps[pu][:],
                                                                                     in1=sg[i2][:], op=ALU.mult),
                             reads=(B_ps[pu], B_sg[i2]), writes=(B_act,))
                    cur = nxt

                def load_wd(dg):
                    i = wc[1] % 2
                    wc[1] += 1
                    k.dma("sp", wd[i][:], wb_dn[l][:, dg * 256:(dg + 1) * 256].rearrange("(c p) n -> p c n", p=128),
                          reads=(B_w["in"],), writes=(B_wd[i],))
                    return i
                cur = load_wd(0)
                for dg in range(4):
                    nxt = load_wd(dg + 1) if dg + 1 < 4 else None
                    for j in range(2):
                        dc = dg * 2 + j
                        pi = 5 + dc % 2
                        k.mm(ps[pi][:], B_ps[pi], [(wd[cur][:, f, j * 128:(j + 1) * 128], actT[:, f, :]) for f in range(NFC)],
                             reads=(B_wd[cur], B_act))
                        k.op("dve", lambda t, pi=pi, dc=dc: t.tensor_tensor(out=hT[:, dc, cols], in0=ps[pi][:],
                                                                            in1=hT[:, dc, cols], op=ALU.add),
                             reads=(B_ps[pi], B_h[dc][tg]), writes=(B_h[dc][tg],))
                    cur = nxt
            k.barrier()

    for s in range(NSEQ):
        for c in range(8):
            k.dma("sp", hT[:, c, :], xT[s, c * 128:(c + 1) * 128, :], writes=B_h[c])
        for l in range(DEPTH):
            k.new_epoch()
            layer(l)
        if debug in ("p1", "p2", "p3") or (isinstance(debug, str) and debug.startswith("g")):
            break
        with ExitStack() as p:
            sq = p.enter_context(nc.sbuf_tensor("sqf_%d" % s, [128, 8, 512], BF16))
            rstd = p.enter_context(nc.sbuf_tensor("rstdf_%d" % s, [128, 512], F32))
            of = [p.enter_context(nc.sbuf_tensor("of%d_%d" % (i, s), [128, 8, 512], F32)) for i in range(2)]
            B_sq, B_rstd = Buf(), Buf()
            B_of = [Buf(), Buf()]
            for tg in range(NTG):
                o, Bo = of[tg % 2], B_of[tg % 2]
                rmsnorm_tg(tg, 2 * DEPTH, sq, B_sq, rstd, B_rstd, lambda c, o=o, Bo=Bo: (o[:, c, :], (Bo,)))
                k.dma("pool", outT[s][:, tg * 512:(tg + 1) * 512].rearrange("(c p) n -> p c n", p=128), o[:],
                      reads=(Bo,))
            k.barrier()
    k.barrier()
    return nc, ctx, consts


_CACHE = {}


def _prep_inputs(inputs, S, DEPTH):
    consts, _ = host_consts(S)
    gl = [inputs["norm_mix"][l] for l in range(DEPTH)] + [inputs["norm_ffn"][l] for l in range(DEPTH)] + \
         [inputs["norm_final"]]
    gvec = np.stack([np.asarray(g, np.float32).reshape(8, 128).T for g in gl], 1)
    shared = {
        "w_in": np.ascontiguousarray(inputs["w_in"], np.float32),
        "w_ret_o": np.ascontiguousarray(inputs["w_ret_o"], np.float32),
        "w_sb_o": np.ascontiguousarray(inputs["w_sb_o"], np.float32),
        "w_out": np.ascontiguousarray(inputs["w_out"], np.float32),
        "w_gate_up": np.ascontiguousarray(inputs["w_gate_up"], np.float32),
        "w_down": np.ascontiguousarray(inputs["w_down"], np.float32),
        "gvec": np.ascontiguousarray(gvec.reshape(128, -1), np.float32),
    }
    for kk, v in consts.items():
        shared["c_" + kk] = np.ascontiguousarray(v, np.float32)
    return shared


def kernel(x, norm_mix, w_in, w_ret_o, w_sb_o, w_out, norm_ffn, w_gate_up, w_down, norm_final):
    x = np.asarray(x, np.float32)
    B, S, _ = x.shape
    DEPTH = np.asarray(w_in).shape[0]
    ncores = 8
    NSEQ = B // ncores
    inputs = dict(norm_mix=np.asarray(norm_mix), w_in=np.asarray(w_in), w_ret_o=np.asarray(w_ret_o),
                  w_sb_o=np.asarray(w_sb_o), w_out=np.asarray(w_out), norm_ffn=np.asarray(norm_ffn),
                  w_gate_up=np.asarray(w_gate_up), w_down=np.asarray(w_down), norm_final=np.asarray(norm_final))
    shared = _prep_inputs(inputs, S, DEPTH)
    nc, ctx, _ = build_program(NSEQ, S, DEPTH)
    in_maps = []
    for c in range(ncores):
        m = dict(shared)
        m["xT"] = np.ascontiguousarray(x[c * NSEQ:(c + 1) * NSEQ].transpose(0, 2, 1))
        in_maps.append(m)
    res = run_bass_kernel_spmd(nc, in_maps, core_ids=list(range(ncores)))
    out = np.empty((B, S, D), np.float32)
    for c in range(ncores):
        out[c * NSEQ:(c + 1) * NSEQ] = res.results[c]["outT"].transpose(0, 2, 1)
    return out
```

```python
import numpy as np
from contextlib import ExitStack
import concourse.bass as bass
import concourse.mybir as mybir
from concourse.bass_utils import run_bass_kernel_spmd

F32, BF16 = mybir.dt.float32, mybir.dt.bfloat16
AF = mybir.ActivationFunctionType
ALU = mybir.AluOpType
AX = mybir.AxisListType

D = 1024
NC8 = 8
RH, RDK, RDV = 4, 128, 256
SBH, SBD = 8, 64
FF = 2816
NFC = FF // 128
INC = 6656
EPS = 1e-6
O_RQ, O_RK, O_RV, O_RG, O_SQ, O_SK, O_SV, O_GR, O_GS = 0, 512, 1024, 2048, 3072, 3584, 4096, 4608, 5632


class Buf:
    __slots__ = ("w", "r", "excl")

    def __init__(self, excl=False):
        self.w = {}
        self.r = {}
        self.excl = excl


class KB:
    def __init__(self, nc, ctx):
        self.nc, self.ctx = nc, ctx
        self.engs = {"pe": nc.tensor, "act": nc.scalar, "dve": nc.vector, "pool": nc.gpsimd, "sp": nc.sync}
        self.sems = []
        self.esem = {}
        self.seen = {e: {} for e in self.engs}
        for e in self.engs:
            self.new_eng_sem(e)
        self.dpool = {}
        for q in ("sp", "pool"):
            self.dpool[q] = [[self._new_sem("d%s%d" % (q, i)), 0] for i in range(12)]
        self.dnext = {"sp": 0, "pool": 0}
        self.dsids = set(sl[0] for q in self.dpool for sl in self.dpool[q])

    def _new_sem(self, name):
        h = self.ctx.enter_context(self.nc.semaphore(name))
        self.sems.append(h)
        return len(self.sems) - 1

    def new_eng_sem(self, e):
        self.esem[e] = [self._new_sem("e%s%d" % (e, len(self.sems))), 0]

    def new_epoch(self):
        for e in ("pe", "act", "dve", "pool"):
            if self.esem[e][1] > 12000:
                self.new_eng_sem(e)

    def _waits(self, e, reads, writes):
        need = {}
        mysid0 = self.esem[e][0]
        for b in reads:
            for sid, v in b.w.items():
                need[sid] = max(need.get(sid, 0), v)
            if b.excl:
                for sid, v in b.r.items():
                    if sid != mysid0:
                        need[sid] = max(need.get(sid, 0), v)
        for b in writes:
            for sid, v in b.w.items():
                need[sid] = max(need.get(sid, 0), v)
            for sid, v in b.r.items():
                need[sid] = max(need.get(sid, 0), v)
        mysid = self.esem[e][0]
        for sid, v in need.items():
            if sid == mysid and e == "pe":
                continue
            if self.seen[e].get(sid, 0) >= v:
                continue
            self.engs[e].wait_ge(self.sems[sid], v)
            self.seen[e][sid] = v

    def op(self, e, fn, reads=(), writes=(), inc=True):
        self._waits(e, reads, writes)
        ins = fn(self.engs[e])
        mysid = self.esem[e][0]
        tgt = self.esem[e][1] + 1
        if inc:
            ins.then_inc(self.sems[mysid], 1)
            self.esem[e][1] = tgt
        for b in reads:
            b.r[mysid] = max(b.r.get(mysid, 0), tgt)
        for b in writes:
            b.w = {mysid: tgt}
            b.r = {}
        return ins

    def dma(self, q, out, in_, reads=(), writes=()):
        self._waits(q, reads, writes)
        pool = self.dpool[q]
        slot = pool[self.dnext[q] % len(pool)]
        self.dnext[q] += 1
        sid = slot[0]
        if slot[1] > 0 and self.seen[q].get(sid, 0) < slot[1] * 16:
            self.engs[q].wait_ge(self.sems[sid], slot[1] * 16)
            self.seen[q][sid] = slot[1] * 16
        ins = self.engs[q].dma_start(out=out, in_=in_)
        slot[1] += 1
        v = slot[1] * 16
        ins.then_inc(self.sems[sid], 16)
        for b in reads:
            b.r[sid] = max(b.r.get(sid, 0), v)
        for b in writes:
            b.w = {ks: kv for ks, kv in b.w.items() if ks in self.dsids}
            b.w[sid] = v
            b.r = {}

    def barrier(self):
        tg = {}
        for e in ("pe", "act", "dve", "pool"):
            sid, c = self.esem[e]
            if c > 0:
                tg[sid] = c
        for q in ("sp", "pool"):
            for sid, c in self.dpool[q]:
                if c > 0:
                    tg[sid] = c * 16
        for e in self.engs:
            for sid, v in tg.items():
                if self.seen[e].get(sid, 0) >= v:
                    continue
                self.engs[e].wait_ge(self.sems[sid], v)
                self.seen[e][sid] = v

    def mm(self, ps, psb, pairs, reads, first=True, last=True):
        n = len(pairs)
        for i, (l, r) in enumerate(pairs):
            st = first and i == 0
            sp = last and i == n - 1
            self.op("pe", lambda t, l=l, r=r, st=st, sp=sp: t.matmul(ps, l, r, start=st, stop=sp),
                    reads=reads if i == 0 else (), writes=(psb,), inc=(i == n - 1))


def host_consts(S):
    c = {}
    ar = np.arange(128)
    c["ident"] = np.eye(128, dtype=np.float32)
    c["negmask"] = np.where(ar[:, None] >= ar[None, :], -30000.0, 0.0).astype(np.float32)
    c["negu"] = np.where(ar[:, None] >= ar[None, :], -1.0, 0.0).astype(np.float32)
    prot = np.zeros((128, 128), np.float32)
    for m in range(64):
        prot[m + 64, m] = 1.0
        prot[m, m + 64] = 1.0
    c["prot"] = prot
    c["onesm"] = np.full((128, 128), 1.0 / 1024, np.float32)
    sel = np.zeros((128, 16, 48), np.float32)
    for kb in range(16):
        sel[:, kb, kb] = 1.0
        sel[:, kb, 32 + kb] = 1.0
    c["sel"] = sel.reshape(128, 16 * 48)
    ns = np.zeros((128, 16, 128), np.float32)
    for kb in range(16):
        for r in range(16):
            if r > kb:
                ns[r, kb, :] = -1.0
                ns[32 + r, kb, :] = -1.0
    c["negstep"] = ns.reshape(128, 16 * 128)
    half = 64
    inv = (1.0 / (np.float32(10000.0) ** (np.arange(half, dtype=np.float32) / np.float32(half)))).astype(np.float32)
    pos = np.arange(S, dtype=np.float32)
    ang = (pos[:, None] * inv[None, :]).astype(np.float32)
    cos = np.cos(ang).astype(np.float32).T
    sin = np.sin(ang).astype(np.float32).T
    cosf = np.concatenate([cos, cos], 0)
    sinf = np.concatenate([-sin, sin], 0)
    ksc = np.float32(RDK ** -0.5)
    c["rope"] = np.stack([cosf, sinf, cosf * ksc, sinf * ksc], 1).astype(np.float32).reshape(128, 4 * S)
    lg = np.log1p(-np.exp2(-5.0 - np.arange(RH, dtype=np.float32))).astype(np.float32)
    i = np.arange(128, dtype=np.float32)
    diff = i[None, :] - i[:, None]
    dect = np.where(diff[None] >= 0, np.exp(lg[:, None, None] * np.maximum(diff, 0.0)[None]), 0.0)
    c["dect"] = np.ascontiguousarray(dect.transpose(1, 0, 2)).astype(np.float32).reshape(128, RH * 128)
    c["kdec"] = np.exp(lg[None, :] * (127.0 - i)[:, None]).astype(np.float32)
    qdec = np.exp(lg[:, None] * (i + 1.0)[None, :]).astype(np.float32)
    c["qdec"] = np.broadcast_to(qdec[None], (128, RH, 128)).astype(np.float32).reshape(128, RH * 128).copy()
    cd = np.exp(lg * 128.0).astype(np.float32)
    return c, [float(x) for x in cd]


def build_program(NSEQ, S, DEPTH, debug=False):
    assert S % 512 == 0
    NTG = S // 512
    NTB = S // 128
    consts, CD = host_consts(S)
    nc = bass.Bass("TRN2", target_bir_lowering=False)
    ctx = ExitStack()

    def din(name, shape, dt=F32):
        return nc.dram_tensor(name, list(shape), dt, kind="ExternalInput").ap()

    def dscr(name, shape, dt=BF16):
        return nc.dram_tensor(name, list(shape), dt, kind=("ExternalOutput" if debug else "Internal")).ap()

    xT = din("xT", [NSEQ, D, S])
    outT = nc.dram_tensor("outT", [NSEQ, D, S], F32, kind="ExternalOutput").ap()
    w_in = din("w_in", [DEPTH, D, INC])
    w_ret_o = din("w_ret_o", [DEPTH, D, D])
    w_sb_o = din("w_sb_o", [DEPTH, 512, D])
    w_out = din("w_out", [DEPTH, D, D])
    w_gu = din("w_gate_up", [DEPTH, D, 2 * FF])
    w_dn = din("w_down", [DEPTH, FF, D])
    gvec = din("gvec", [128, (2 * DEPTH + 1) * 8])
    cin = {k: din("c_" + k, v.shape) for k, v in consts.items()}
    wb_in = dscr("wb_in", [DEPTH, D, INC])
    wb_ret_o = dscr("wb_ret_o", [DEPTH, D, D])
    wb_sb_o = dscr("wb_sb_o", [DEPTH, 512, D])
    wb_out = dscr("wb_out", [DEPTH, D, D])
    wb_gu = dscr("wb_gu", [DEPTH, D, 2 * FF])
    wb_dn = dscr("wb_dn", [DEPTH, FF, D])
    s_rqt = dscr("s_rqt", [4, 128, S])
    s_rkt = dscr("s_rkt", [4, 128, S])
    s_rv = dscr("s_rv", [S, 1024])
    s_rg = dscr("s_rg", [S, 1024])
    s_sqt = dscr("s_sqt", [4, 128, S])
    s_skt = dscr("s_skt", [4, 128, S])
    s_sv = dscr("s_sv", [S, 512])
    s_grt = dscr("s_grt", [8, 128, S])
    s_gst = dscr("s_gst", [8, 128, S])
    s_sbt = dscr("s_sbt", [4, 128, S])
    s_retgt = dscr("s_retgt", [8, 128, S])
    B_scr = {n: [Buf() for _ in range(16)] for n in
             ("rqt", "rkt", "rv", "rg", "sqt", "skt", "sv", "grt", "gst", "sbt", "retgt")}
    B_w = {n: Buf() for n in ("in", "ret_o", "sb_o", "out", "gu", "dn")}

    def sb(name, shape, dt):
        return ctx.enter_context(nc.sbuf_tensor(name, list(shape), dt))

    k = KB(nc, ctx)
    UID = [0]
    hT = sb("hT", [128, 8, S], F32)
    B_h = [[Buf() for _ in range(NTG)] for _ in range(8)]
    identb = sb("identb", [128, 128], BF16)
    negmaskb = sb("negmaskb", [128, 128], BF16)
    negub = sb("negub", [128, 128], BF16)
    protb = sb("protb", [128, 128], BF16)
    onesmb = sb("onesmb", [128, 128], BF16)
    selb = sb("selb", [128, 16, 48], BF16)
    negstepb = sb("negstepb", [128, 16, 128], BF16)
    dect = sb("dect", [128, RH, 128], F32)
    kdec = sb("kdec", [128, RH], F32)
    qdec = sb("qdec", [128, RH, 128], F32)
    gv = sb("gv", [128, 2 * DEPTH + 1, 8], F32)
    epsb = sb("epsb", [128, 1], F32)
    B_const = Buf()
    ps = [ctx.enter_context(nc.psum_tensor("ps%d" % i, [128, 512], F32)) for i in range(7)]
    pT = ctx.enter_context(nc.psum_tensor("pT", [128, 1024], BF16))
    B_ps = [Buf(excl=True) for _ in range(7)]
    B_pT = Buf(excl=True)
    rr = {"evac": 0}

    def evac_eng():
        rr["evac"] += 1
        return "act" if rr["evac"] % 2 else "dve"

    def copy_op(e, out, in_, reads, writes):
        if e == "act":
            k.op("act", lambda t: t.activation(out=out, in_=in_, func=AF.Copy), reads=reads, writes=writes)
        else:
            k.op(e, lambda t: t.tensor_copy(out=out, in_=in_), reads=reads, writes=writes)

    with ExitStack() as pctx:
        stg = [pctx.enter_context(nc.sbuf_tensor("stg%d" % i, [128, 2048], F32)) for i in range(2)]
        stb = [pctx.enter_context(nc.sbuf_tensor("stb%d" % i, [128, 2048], BF16)) for i in range(2)]
        B_stg = [Buf(), Buf()]
        B_stb = [Buf(), Buf()]
        cnt = [0]

        def conv(dst_sb_ap, src_dram, ncol, npart=128):
            i = cnt[0] % 2
            cnt[0] += 1
            k.dma("sp", stg[i][0:npart, 0:ncol], src_dram, writes=(B_stg[i],))
            copy_op("dve", dst_sb_ap, stg[i][0:npart, 0:ncol], (B_stg[i],), (B_const,))

        conv(identb[:], cin["ident"], 128)
        conv(negmaskb[:], cin["negmask"], 128)
        conv(negub[:], cin["negu"], 128)
        conv(protb[:], cin["prot"], 128)
        conv(onesmb[:], cin["onesm"], 128)
        conv(selb[:].rearrange("p a b -> p (a b)"), cin["sel"], 16 * 48)
        conv(negstepb[:].rearrange("p a b -> p (a b)"), cin["negstep"], 2048)
        k.dma("sp", dect[:].rearrange("p a b -> p (a b)"), cin["dect"], writes=(B_const,))
        k.dma("sp", kdec[:], cin["kdec"], writes=(B_const,))
        k.dma("sp", qdec[:].rearrange("p a b -> p (a b)"), cin["qdec"], writes=(B_const,))
        k.dma("sp", gv[:].rearrange("p a b -> p (a b)"), gvec, writes=(B_const,))
        k.op("dve", lambda t: t.memset(epsb[:], EPS), writes=(B_const,))

        def conv_w(dst, src, R, C):
            for r0 in range(0, R, 128):
                for c0 in range(0, C, 2048):
                    cw = min(2048, C - c0)
                    i = cnt[0] % 2
                    e = ("dve", "act", "pool")[cnt[0] % 3]
                    cnt[0] += 1
                    k.dma("sp", stg[i][:, 0:cw], src[r0:r0 + 128, c0:c0 + cw], writes=(B_stg[i],))
                    copy_op(e, stb[i][:, 0:cw], stg[i][:, 0:cw], (B_stg[i],), (B_stb[i],))
                    k.dma("pool", dst[r0:r0 + 128, c0:c0 + cw], stb[i][:, 0:cw], reads=(B_stb[i],),
                          writes=(B_w["in"],))

        for l in range(DEPTH):
            conv_w(wb_in[l], w_in[l], D, INC)
            conv_w(wb_ret_o[l], w_ret_o[l], D, D)
            conv_w(wb_sb_o[l], w_sb_o[l], 512, D)
            conv_w(wb_out[l], w_out[l], D, D)
            conv_w(wb_gu[l], w_gu[l], D, 2 * FF)
            conv_w(wb_dn[l], w_dn[l], FF, D)
        k.barrier()

    if debug == "p0":
        return nc, ctx, consts
    def rmsnorm_tg(tg, gidx, sq, B_sq, rstd, B_rstd, out_fn):
        cols = slice(tg * 512, (tg + 1) * 512)
        k.op("act", lambda t: t.activation(out=sq[:], in_=hT[:, :, cols], func=AF.Square),
             reads=[B_h[c][tg] for c in range(8)], writes=(B_sq,))
        k.mm(ps[0][:], B_ps[0], [(onesmb[:], sq[:, c, :]) for c in range(8)], reads=(B_sq, B_const))
        k.op("act", lambda t: t.activation(out=rstd[:], in_=ps[0][:], func=AF.Ln, bias=epsb[:]),
             reads=(B_ps[0], B_const), writes=(B_rstd,))
        k.op("act", lambda t: t.activation(out=rstd[:], in_=rstd[:], func=AF.Exp, scale=-0.5),
             reads=(B_rstd,), writes=(B_rstd,))
        for c in range(8):
            o, ob = out_fn(c)
            k.op("dve", lambda t, o=o, c=c: t.scalar_tensor_tensor(
                out=o, in0=hT[:, c, cols], scalar=gv[:, gidx, c:c + 1], in1=rstd[:], op0=ALU.mult, op1=ALU.mult),
                reads=(B_h[c][tg], B_rstd, B_const), writes=ob)

    def layer(l):
        with ExitStack() as p:
            def al(name, shape, dt):
                UID[0] += 1
                return p.enter_context(nc.sbuf_tensor("%s_%d" % (name, UID[0]), list(shape), dt))
            hnT = al("hnT", [128, 8, S], BF16)
            B_hn = [Buf() for _ in range(NTG)]
            sq = al("sq", [128, 8, 512], BF16)
            rstd = al("rstd", [128, 512], F32)
            B_sq, B_rstd = Buf(), Buf()
            for tg in range(NTG):
                rmsnorm_tg(tg, l, sq, B_sq, rstd, B_rstd,
                           lambda c, tg=tg: (hnT[:, c, tg * 512:(tg + 1) * 512], (B_hn[tg],)))
            wt = [al("wt%d" % i, [128, 8, 512], BF16) for i in range(2)]
            B_wt = [Buf(), Buf()]
            ost = [al("ost%d" % i, [128, max(S, 2048)], BF16) for i in range(2)]
            B_ost = [Buf(), Buf()]
            rot = al("rot", [128, 4, 512], F32)
            B_rot = Buf()
            xb = al("xb", [128, 512], BF16)
            B_xb = Buf()
            t1 = al("t1", [128, 512], F32)
            t2 = al("t2", [128, 512], F32)
            B_t1, B_t2 = Buf(), Buf()
            st = {"ps": 1, "ost": 0}

            def next_ps():
                st["ps"] = 1 + (st["ps"] % 5)
                return st["ps"]

            def load_w(gi):
                i = gi % 2
                k.dma("sp", wt[i][:], wb_in[l][:, gi * 512:(gi + 1) * 512].rearrange("(c p) n -> p c n", p=128),
                      reads=(B_w["in"],), writes=(B_wt[i],))

            NG = INC // 512
            load_w(0)
            rope_v = cin["rope"].rearrange("p (a s) -> p a s", a=4)
            for gi in range(NG):
                if isinstance(debug, str) and debug.startswith("g") and gi >= int(debug[1:2]):
                    break
                stage = debug[2:] if isinstance(debug, str) and debug.startswith("g") else ""

                if gi + 1 < NG:
                    load_w(gi + 1)
                w = wt[gi % 2]
                Bw = B_wt[gi % 2]
                col0 = gi * 512
                if col0 in (O_RQ, O_RK):
                    isk = col0 == O_RK
                    dst = s_rkt if isk else s_rqt
                    Bd = B_scr["rkt" if isk else "rqt"]
                    for tg in range(NTG):
                        k.dma("sp", rot[:], rope_v[:, :, tg * 512:(tg + 1) * 512], writes=(B_rot,))
                        for h in range(4):
                            pi = next_ps()
                            k.mm(ps[pi][:], B_ps[pi],
                                 [(w[:, c, h * 128:(h + 1) * 128], hnT[:, c, tg * 512:(tg + 1) * 512]) for c in range(8)],
                                 reads=(Bw, B_hn[tg]))
                            if stage == "a":
                                continue
                            copy_op("act", xb[:], ps[pi][:], (B_ps[pi],), (B_xb,))
                            if stage == "b":
                                continue
                            k.mm(ps[6][:], B_ps[6], [(protb[:], xb[:])], reads=(B_xb, B_const))
                            if stage == "c":
                                continue
                            tb = 2 if isk else 0
                            k.op("dve", lambda t, pi=pi, tb=tb: t.tensor_tensor(
                                out=t1[:], in0=ps[pi][:], in1=rot[:, tb, :], op=ALU.mult),
                                reads=(B_ps[pi], B_rot), writes=(B_t1,))
                            k.op("dve", lambda t, tb=tb: t.tensor_tensor(
                                out=t2[:], in0=ps[6][:], in1=rot[:, tb + 1, :], op=ALU.mult),
                                reads=(B_ps[6], B_rot), writes=(B_t2,))
                            if stage == "d":
                                continue
                            o = ost[h % 2]
                            k.op("dve", lambda t, o=o, tg=tg: t.tensor_tensor(
                                out=o[:, tg * 512:(tg + 1) * 512], in0=t1[:], in1=t2[:], op=ALU.add),
                                reads=(B_t1, B_t2), writes=(B_ost[h % 2],))
                            if stage == "e":
                                continue
                            k.dma("pool", dst[h][:, tg * 512:(tg + 1) * 512], o[:, tg * 512:(tg + 1) * 512],
                                  reads=(B_ost[h % 2],), writes=(Bd[h],))
                elif col0 in (O_SQ, O_SK, O_GR, O_GR + 512, O_GS, O_GS + 512):
                    if col0 == O_SQ:
                        dst, Bd, base, fn = s_sqt, B_scr["sqt"], 0, AF.Identity
                    elif col0 == O_SK:
                        dst, Bd, base, fn = s_skt, B_scr["skt"], 0, AF.Copy
                    elif col0 >= O_GS:
                        dst, Bd, base, fn = s_gst, B_scr["gst"], (col0 - O_GS) // 128, AF.Sigmoid
                    else:
                        dst, Bd, base, fn = s_grt, B_scr["grt"], (col0 - O_GR) // 128, AF.Sigmoid
                    for fc in range(4):
                        oi = st["ost"] % 2
                        st["ost"] += 1
                        o = ost[oi]
                        for tg in range(NTG):
                            pi = next_ps()
                            k.mm(ps[pi][:], B_ps[pi],
                                 [(w[:, c, fc * 128:(fc + 1) * 128], hnT[:, c, tg * 512:(tg + 1) * 512]) for c in range(8)],
                                 reads=(Bw, B_hn[tg]))
                            if fn == AF.Copy:
                                copy_op(evac_eng(), o[:, tg * 512:(tg + 1) * 512], ps[pi][:], (B_ps[pi],), (B_ost[oi],))
                            elif fn == AF.Identity:
                                k.op("dve", lambda t, o=o, tg=tg, pi=pi: t.tensor_scalar(
                                    out=o[:, tg * 512:(tg + 1) * 512], in0=ps[pi][:], scalar1=0.125, scalar2=0.0,
                                    op0=ALU.mult, op1=ALU.add),
                                    reads=(B_ps[pi],), writes=(B_ost[oi],))
                            else:
                                k.op("act", lambda t, o=o, tg=tg, pi=pi: t.activation(
                                    out=o[:, tg * 512:(tg + 1) * 512], in_=ps[pi][:], func=AF.Sigmoid),
                                    reads=(B_ps[pi],), writes=(B_ost[oi],))
                        k.dma("pool", dst[base + fc], o[:, 0:S], reads=(B_ost[oi],), writes=(Bd[base + fc],))
                else:
                    if col0 >= O_SV:
                        dst, Bd, cb, fn = s_sv, B_scr["sv"], 0, AF.Copy
                    elif col0 >= O_RG:
                        dst, Bd, cb, fn = s_rg, B_scr["rg"], col0 - O_RG, AF.Silu
                    else:
                        dst, Bd, cb, fn = s_rv, B_scr["rv"], col0 - O_RV, AF.Copy
                    for tq in range(NTB // 4):
                        oi = st["ost"] % 2
                        st["ost"] += 1
                        o = ost[oi]
                        for tb4 in range(4):
                            tb = tq * 4 + tb4
                            pi = next_ps()
                            k.mm(ps[pi][:], B_ps[pi],
                                 [(hnT[:, c, tb * 128:(tb + 1) * 128], w[:, c, :]) for c in range(8)],
                                 reads=(Bw, B_hn[tb // 4]))
                            if fn == AF.Copy:
                                copy_op(evac_eng(), o[:, tb4 * 512:(tb4 + 1) * 512], ps[pi][:], (B_ps[pi],), (B_ost[oi],))
                            else:
                                k.op("act", lambda t, o=o, tb4=tb4, pi=pi: t.activation(
                                    out=o[:, tb4 * 512:(tb4 + 1) * 512], in_=ps[pi][:], func=AF.Silu),
                                    reads=(B_ps[pi],), writes=(B_ost[oi],))
                        k.dma("pool",
                              dst[tq * 512:(tq + 1) * 512, cb:cb + 512].rearrange("(t p) n -> p t n", p=128),
                              o[:, 0:2048].rearrange("p (t n) -> p t n", n=512),
                              reads=(B_ost[oi],), writes=(Bd[tq],))
            k.barrier()
        if debug == "p1" or (isinstance(debug, str) and debug.startswith("g")):
            return
        sb_phase(l)
        if debug == "p2":
            return
        ret_phase(l)
        if debug == "p3":
            return
        post_phase(l)
        if debug == "p4":
            return
        ffn_phase(l)

    def sb_phase(l):
        with ExitStack() as p:
            def al(name, shape, dt):
                UID[0] += 1
                return p.enter_context(nc.sbuf_tensor("%s_%d" % (name, UID[0]), list(shape), dt))
            qa = al("qa", [128, S], BF16)
            qb = al("qb", [128, S], BF16)
            kt = al("kt", [128, S], BF16)
            sv = al("sv", [128, NTB, 128], BF16)
            B_q, B_k, B_v = Buf(), Buf(), Buf()
            spb = [al("spb%d" % i, [128, NTB, 512], BF16) for i in range(2)]
            B_sp = [Buf(), Buf()]
            et = [al("et%d" % i, [128, 512], F32) for i in range(2)]
            B_et = [Buf(), Buf()]
            at = [al("at%d" % i, [128, 512], BF16) for i in range(4)]
            B_at = [Buf() for _ in range(4)]
            hl = [al("hl%d" % i, [128, 512], BF16) for i in range(2)]
            B_hl = [Buf(), Buf()]
            osb = al("osb", [128, S], BF16)
            B_osb = Buf()
            for i in range(2):
                k.op("dve", lambda t, i=i: t.memset(hl[i][:], 0.0), writes=(B_hl[i],))
            k.op("dve", lambda t: t.memset(qa[64:128, :], 0.0), writes=(B_q,))
            k.op("dve", lambda t: t.memset(qb[0:64, :], 0.0), writes=(B_q,))
            cnt = {"z": 0, "t": 0, "e": 0, "a": 0}
            tst = {}
            for c in range(4):
                k.dma("sp", qa[0:64, :], s_sqt[c][0:64, :], reads=(B_scr["sqt"][c],), writes=(B_q,))
                k.dma("sp", qb[64:128, :], s_sqt[c][64:128, :], reads=(B_scr["sqt"][c],), writes=(B_q,))
                k.dma("sp", kt[:], s_skt[c], reads=(B_scr["skt"][c],), writes=(B_k,))
                k.dma("sp", sv[:], s_sv[:, c * 128:(c + 1) * 128].rearrange("(t p) n -> p t n", p=128),
                      reads=[B_scr["sv"][i] for i in range(NTB // 4)], writes=(B_v,))
                units = [(g, hh) for g in range(NTG) for hh in range(2)]

                def geom(g, kb):
                    lo = max(0, 128 * kb - 512 * g)
                    return lo, kb >= 4 * g

                def p1_block(u, kb):
                    g, hh = units[u]
                    q = qa if hh == 0 else qb
                    spt, Bs = spb[u % 2], B_sp[u % 2]
                    lo, diag = geom(g, kb)
                    zi = cnt["z"] % 2
                    cnt["z"] += 1
                    z, Bz = ps[zi], B_ps[zi]
                    ks = kt[:, kb * 128:(kb + 1) * 128]
                    q0 = g * 512
                    if diag:
                        k.mm(z[:, lo:lo + 128], Bz, [(identb[:], negmaskb[:]), (ks, q[:, q0 + lo:q0 + lo + 128])],
                             reads=(B_const, B_k, B_q))
                        if lo + 128 < 512:
                            k.mm(z[:, lo + 128:512], Bz, [(ks, q[:, q0 + lo + 128:q0 + 512])], reads=(B_k, B_q))
                    else:
                        k.mm(z[:, lo:512], Bz, [(ks, q[:, q0 + lo:q0 + 512])], reads=(B_k, B_q))
                    ei = cnt["e"] % 2
                    cnt["e"] += 1
                    k.op("act", lambda t: t.activation(out=et[ei][:, lo:512], in_=z[:, lo:512], func=AF.Exp),
                         reads=(Bz,), writes=(B_et[ei],))
                    tst[("p1", u, kb)] = ei

                def p1_mid(u, kb):
                    g, hh = units[u]
                    spt, Bs = spb[u % 2], B_sp[u % 2]
                    lo, diag = geom(g, kb)
                    ei = tst[("p1", u, kb)]
                    k.op("act", lambda t: t.activation(out=spt[:, kb, lo:512], in_=et[ei][:, lo:512], func=AF.Ln, bias=1.0),
                         reads=(B_et[ei],), writes=(Bs,))

                def p1_back(u, kb):
                    g, hh = units[u]
                    spt, Bs = spb[u % 2], B_sp[u % 2]
                    lo, diag = geom(g, kb)
                    nkb = 4 * g + 4
                    k.op("pe", lambda t: t.matmul(ps[4][0:48, lo:512], selb[:, kb, :], spt[:, kb, lo:512],
                                                   start=(kb == 0), stop=(kb == nkb - 1)),
                         reads=(Bs, B_const), writes=(B_ps[4],))

                def p1_fin(u):
                    h_, Bh = hl[u % 2], B_hl[u % 2]
                    k.op("dve", lambda t: t.tensor_copy(out=h_[0:48, :], in_=ps[4][0:48, :]),
                         reads=(B_ps[4],), writes=(Bh,))
                    k.op("dve", lambda t: t.tensor_tensor(out=h_[32:48, :], in0=ps[4][32:48, :], in1=h_[32:48, :],
                                                          op=ALU.subtract),
                         reads=(B_ps[4], Bh), writes=(Bh,))

                def p2_block(u, kb):
                    g, hh = units[u]
                    q = qa if hh == 0 else qb
                    spt, Bs = spb[u % 2], B_sp[u % 2]
                    h_, Bh = hl[u % 2], B_hl[u % 2]
                    lo, diag = geom(g, kb)
                    ti = 2 + cnt["t"] % 2
                    cnt["t"] += 1
                    T, Bt = ps[ti], B_ps[ti]
                    ks = kt[:, kb * 128:(kb + 1) * 128]
                    q0 = g * 512
                    rd = (B_const, B_k, B_q, Bs, Bh)

                    def grp(a, b, withmask):
                        prs = []
                        if withmask:
                            prs.append((identb[:], negmaskb[:]))
                        prs.append((ks, q[:, q0 + a:q0 + b]))
                        prs.append((negub[:], spt[:, kb, a:b]))
                        prs.append((negstepb[:, kb, :], h_[:, a:b]))
                        k.mm(T[:, a:b], Bt, prs, reads=rd)
                    if diag:
                        grp(lo, lo + 128, True)
                        if lo + 128 < 512:
                            grp(lo + 128, 512, False)
                    else:
                        grp(lo, 512, False)
                    ai = cnt["a"] % 4
                    cnt["a"] += 1
                    k.op("act", lambda t: t.activation(out=at[ai][:, lo:512], in_=T[:, lo:512], func=AF.Exp),
                         reads=(Bt,), writes=(B_at[ai],))
                    tst[("p2", u, kb)] = ai

                def p2_back(u, kb):
                    g, hh = units[u]
                    lo, diag = geom(g, kb)
                    ai = tst[("p2", u, kb)]
                    nkb = 4 * g + 4
                    k.op("pe", lambda t: t.matmul(ps[5][hh * 64:(hh + 1) * 64, lo:512], sv[:, kb, hh * 64:(hh + 1) * 64],
                                                   at[ai][:, lo:512], start=(kb == 0), stop=(kb == nkb - 1)),
                         reads=(B_at[ai], B_v), writes=(B_ps[5],))

                def p2_fin(u):
                    g, hh = units[u]
                    if hh == 1:
                        copy_op("dve", osb[:, g * 512:(g + 1) * 512], ps[5][:], (B_ps[5],), (B_osb,))

                nu = len(units)
                q_mid, q_back = [], []

                def do_mid(t):
                    if t[0] == "p1":
                        p1_mid(t[1], t[2])

                def do_back(t):
                    if t[0] == "p1":
                        p1_back(t[1], t[2])
                    else:
                        p2_back(t[1], t[2])

                def flush():
                    for t in q_mid:
                        do_mid(t)
                    del q_mid[:]
                    for t in q_back:
                        do_back(t)
                    del q_back[:]

                def step(t):
                    if t[0] == "p1":
                        p1_block(t[1], t[2])
                    else:
                        p2_block(t[1], t[2])
                    for x in q_mid:
                        do_mid(x)
                    del q_mid[:]
                    q_mid.append(t)
                    q_back.append(t)
                    if len(q_back) > 2:
                        do_back(q_back.pop(0))

                for u in range(nu + 1):
                    n1 = 4 * units[u][0] + 4 if u < nu else 0
                    n2 = 4 * units[u - 1][0] + 4 if u >= 1 else 0
                    for i in range(max(n1, n2)):
                        if i < n1:
                            step(("p1", u, i))
                        if i < n2:
                            step(("p2", u - 1, i))
                    flush()
                    if u < nu:
                        p1_fin(u)
                    if u >= 1:
                        p2_fin(u - 1)
                k.dma("pool", s_sbt[c], osb[:], reads=(B_osb,), writes=(B_scr["sbt"][c],))
            k.barrier()

    def ret_phase(l):
        with ExitStack() as p:
            def al(name, shape, dt):
                UID[0] += 1
                return p.enter_context(nc.sbuf_tensor("%s_%d" % (name, UID[0]), list(shape), dt))
            rq = al("rq", [128, S], BF16)
            rk = al("rk", [128, S], BF16)
            rv = al("rv", [128, NTB, 256], BF16)
            rg = al("rg", [128, NTB, 256], BF16)
            B_in = Buf()
            obuf = al("obuf", [128, NTB, 256], F32)
            sqb = al("sqb", [128, NTB, 256], F32)
            yb = al("yb", [128, NTB, 256], BF16)
            B_ob, B_sqb, B_yb = Buf(), Buf(), Buf()
            stt = [al("stt%d" % i, [128, 128], BF16) for i in range(2)]
            B_stt = [Buf(), Buf()]
            qd = [al("qd%d" % i, [128, 128], BF16) for i in range(2)]
            B_qd = [Buf(), Buf()]
            kd = [al("kd%d" % i, [128, 128], BF16) for i in range(2)]
            B_kd = [Buf(), Buf()]
            Rf = al("Rf", [128, 256], F32)
            Rb = [al("Rb%d" % i, [128, 256], BF16) for i in range(2)]
            B_Rf = Buf()
            B_Rb = [Buf(), Buf()]
            stats = al("stats", [128, 6, NTB], F32)
            B_stats = Buf()
            ogt = al("ogt", [128, 2, S], BF16)
            B_ogt = Buf()
            for h in range(RH):
                k.dma("sp", rq[:], s_rqt[h], reads=(B_scr["rqt"][h],), writes=(B_in,))
                k.dma("sp", rk[:], s_rkt[h], reads=(B_scr["rkt"][h],), writes=(B_in,))
                k.dma("sp", rv[:], s_rv[:, h * 256:(h + 1) * 256].rearrange("(t p) n -> p t n", p=128),
                      reads=[B_scr["rv"][i] for i in range(NTB // 4)], writes=(B_in,))
                k.dma("sp", rg[:], s_rg[:, h * 256:(h + 1) * 256].rearrange("(t p) n -> p t n", p=128),
                      reads=[B_scr["rg"][i] for i in range(NTB // 4)], writes=(B_in,))
                for n in range(NTB):
                    cs = slice(n * 128, (n + 1) * 128)
                    i2 = n % 2
                    k.mm(ps[i2][:, 0:128], B_ps[i2], [(rk[:, cs], rq[:, cs])], reads=(B_in,))
                    k.op("dve", lambda t, i2=i2: t.tensor_tensor(out=stt[i2][:], in0=ps[i2][:, 0:128], in1=dect[:, h, :],
                                                                 op=ALU.mult),
                         reads=(B_ps[i2], B_const), writes=(B_stt[i2],))
                    oi = 2 + i2
                    if n > 0:
                        k.op("dve", lambda t, i2=i2, cs=cs: t.tensor_tensor(out=qd[i2][:], in0=rq[:, cs], in1=qdec[:, h, :],
                                                                            op=ALU.mult),
                             reads=(B_in, B_const), writes=(B_qd[i2],))
                        k.mm(ps[oi][:, 0:256], B_ps[oi], [(stt[i2][:], rv[:, n, :]), (qd[i2][:], Rb[(n - 1) % 2][:])],
                             reads=(B_stt[i2], B_in, B_qd[i2], B_Rb[(n - 1) % 2]))
                    else:
                        k.mm(ps[oi][:, 0:256], B_ps[oi], [(stt[i2][:], rv[:, n, :])], reads=(B_stt[i2], B_in))
                    copy_op("act", obuf[:, n, :], ps[oi][:, 0:256], (B_ps[oi],), (B_ob,))
                    if n < NTB - 1:
                        k.op("pe", lambda t, cs=cs: t.transpose(out=pT[:, 0:128], in_=rk[:, cs], identity=identb[:]),
                             reads=(B_in, B_const), writes=(B_pT,))
                        k.op("dve", lambda t, i2=i2: t.tensor_scalar(out=kd[i2][:], in0=pT[:, 0:128],
                                                                     scalar1=kdec[:, h:h + 1], scalar2=0.0, op0=ALU.mult, op1=ALU.add),
                             reads=(B_pT, B_const), writes=(B_kd[i2],))
                        k.mm(ps[4][:, 0:256], B_ps[4], [(kd[i2][:], rv[:, n, :])], reads=(B_kd[i2], B_in))
                        if n == 0:
                            copy_op("dve", Rf[:], ps[4][:, 0:256], (B_ps[4],), (B_Rf,))
                        else:
                            k.op("dve", lambda t: t.scalar_tensor_tensor(out=Rf[:], in0=Rf[:], scalar=CD[h],
                                                                         in1=ps[4][:, 0:256], op0=ALU.mult, op1=ALU.add),
                                 reads=(B_ps[4], B_Rf), writes=(B_Rf,))
                        copy_op("act", Rb[n % 2][:], Rf[:], (B_Rf,), (B_Rb[n % 2],))
                k.op("dve", lambda t: t.tensor_reduce(out=stats[:, 0, :], in_=obuf[:], axis=AX.X, op=ALU.add),
                     reads=(B_ob,), writes=(B_stats,))
                k.op("act", lambda t: t.activation(out=sqb[:], in_=obuf[:], func=AF.Square), reads=(B_ob,), writes=(B_sqb,))
                k.op("dve", lambda t: t.tensor_reduce(out=stats[:, 1, :], in_=sqb[:], axis=AX.X, op=ALU.add),
                     reads=(B_sqb,), writes=(B_stats,))
                k.op("dve", lambda t: t.tensor_scalar(out=stats[:, 2, :], in0=stats[:, 0, :], scalar1=1.0 / 256, scalar2=0.0,
                                                      op0=ALU.mult, op1=ALU.add), reads=(B_stats,), writes=(B_stats,))
                k.op("dve", lambda t: t.tensor_tensor(out=stats[:, 3, :], in0=stats[:, 2, :], in1=stats[:, 2, :], op=ALU.mult),
                     reads=(B_stats,), writes=(B_stats,))
                k.op("dve", lambda t: t.scalar_tensor_tensor(out=stats[:, 3, :], in0=stats[:, 1, :], scalar=1.0 / 256,
                                                             in1=stats[:, 3, :], op0=ALU.mult, op1=ALU.subtract),
                     reads=(B_stats,), writes=(B_stats,))
                k.op("act", lambda t: t.activation(out=stats[:, 4, :], in_=stats[:, 3, :], func=AF.Ln, bias=epsb[:]),
                     reads=(B_stats, B_const), writes=(B_stats,))
                k.op("act", lambda t: t.activation(out=stats[:, 4, :], in_=stats[:, 4, :], func=AF.Exp, scale=-0.5),
                     reads=(B_stats,), writes=(B_stats,))
                k.op("dve", lambda t: t.scalar_tensor_tensor(out=stats[:, 5, :], in0=stats[:, 2, :], scalar=-1.0,
                                                             in1=stats[:, 4, :], op0=ALU.mult, op1=ALU.mult),
                     reads=(B_stats,), writes=(B_stats,))
                for n in range(NTB):
                    e = "act" if n % 2 else "dve"
                    if e == "act":
                        k.op("act", lambda t, n=n: t.activation(out=sqb[:, n, :], in_=obuf[:, n, :], func=AF.Identity,
                                                                scale=stats[:, 4, n:n + 1], bias=stats[:, 5, n:n + 1]),
                             reads=(B_ob, B_stats), writes=(B_sqb,))
                    else:
                        k.op("dve", lambda t, n=n: t.tensor_scalar(out=sqb[:, n, :], in0=obuf[:, n, :],
                                                                   scalar1=stats[:, 4, n:n + 1], scalar2=stats[:, 5, n:n + 1],
                                                                   op0=ALU.mult, op1=ALU.add),
                             reads=(B_ob, B_stats), writes=(B_sqb,))
                k.op("dve", lambda t: t.tensor_tensor(out=yb[:], in0=sqb[:], in1=rg[:], op=ALU.mult),
                     reads=(B_sqb, B_in), writes=(B_yb,))
                for ec in range(2):
                    for n0 in range(0, NTB, 8):
                        nn = min(8, NTB - n0)
                        for j in range(nn):
                            k.op("pe", lambda t, j=j, n0=n0: t.transpose(out=pT[:, j * 128:(j + 1) * 128],
                                                                       in_=yb[:, n0 + j, ec * 128:(ec + 1) * 128],
                                                                       identity=identb[:]),
                                 reads=(B_yb, B_const), writes=(B_pT,), inc=(j == nn - 1))
                        copy_op(evac_eng(), ogt[:, ec, n0 * 128:(n0 + nn) * 128], pT[:, 0:nn * 128], (B_pT,), (B_ogt,))
                for ec in range(2):
                    k.dma("pool", s_retgt[h * 2 + ec], ogt[:, ec, :], reads=(B_ogt,), writes=(B_scr["retgt"][h * 2 + ec],))
            k.barrier()

    def post_phase(l):
        with ExitStack() as p:
            def al(name, shape, dt):
                UID[0] += 1
                return p.enter_context(nc.sbuf_tensor("%s_%d" % (name, UID[0]), list(shape), dt))
            wsbo = al("wsbo", [128, 4, D], BF16)
            wreto = al("wreto", [128, 8, D], BF16)
            wout = al("wout", [128, 8, D], BF16)
            B_wp = Buf()
            k.dma("sp", wsbo[:], wb_sb_o[l].rearrange("(c p) n -> p c n", p=128), reads=(B_w["in"],), writes=(B_wp,))
            k.dma("sp", wreto[:], wb_ret_o[l].rearrange("(c p) n -> p c n", p=128), reads=(B_w["in"],), writes=(B_wp,))
            k.dma("sp", wout[:], wb_out[l].rearrange("(c p) n -> p c n", p=128), reads=(B_w["in"],), writes=(B_wp,))
            sbt = al("sbt", [128, 4, 512], BF16)
            rgt = al("rgt", [128, 8, 512], BF16)
            grt = al("grt", [128, 8, 512], BF16)
            gst = al("gst", [128, 8, 512], BF16)
            B_ld = Buf()
            t1 = [al("pt1%d" % i, [128, 512], F32) for i in range(2)]
            t2 = [al("pt2%d" % i, [128, 512], F32) for i in range(2)]
            B_t1 = [Buf(), Buf()]
            B_t2 = [Buf(), Buf()]
            mt = al("mt", [128, 8, 512], BF16)
            B_mt = Buf()
            pc = [0]
            for tg in range(NTG):
                cols = slice(tg * 512, (tg + 1) * 512)
                k.dma("sp", sbt[:], s_sbt[:, :, cols].rearrange("c p n -> p c n"),
                      reads=B_scr["sbt"][0:4], writes=(B_ld,))
                k.dma("sp", rgt[:], s_retgt[:, :, cols].rearrange("c p n -> p c n"),
                      reads=B_scr["retgt"][0:8], writes=(B_ld,))
                k.dma("sp", grt[:], s_grt[:, :, cols].rearrange("c p n -> p c n"),
                      reads=B_scr["grt"][0:8], writes=(B_ld,))
                k.dma("sp", gst[:], s_gst[:, :, cols].rearrange("c p n -> p c n"),
                      reads=B_scr["gst"][0:8], writes=(B_ld,))
                for ec in range(8):
                    i2 = ec % 2
                    p1, p2 = i2 * 2, i2 * 2 + 1
                    es = slice(ec * 128, (ec + 1) * 128)
                    k.mm(ps[p1][:], B_ps[p1], [(wsbo[:, c, es], sbt[:, c, :]) for c in range(4)], reads=(B_wp, B_ld))
                    k.mm(ps[p2][:], B_ps[p2], [(wreto[:, c, es], rgt[:, c, :]) for c in range(8)], reads=(B_wp, B_ld))
                    k.op("dve", lambda t, p1=p1, i2=i2, ec=ec: t.tensor_tensor(out=t1[i2][:], in0=ps[p1][:], in1=gst[:, ec, :],
                                                                               op=ALU.mult),
                         reads=(B_ps[p1], B_ld), writes=(B_t1[i2],))
                    k.op("dve", lambda t, p2=p2, i2=i2, ec=ec: t.tensor_tensor(out=t2[i2][:], in0=ps[p2][:], in1=grt[:, ec, :],
                                                                               op=ALU.mult),
                         reads=(B_ps[p2], B_ld), writes=(B_t2[i2],))
                    k.op("pool", lambda t, i2=i2, ec=ec: t.tensor_tensor(out=mt[:, ec, :], in0=t1[i2][:], in1=t2[i2][:],
                                                                         op=ALU.add),
                         reads=(B_t1[i2], B_t2[i2]), writes=(B_mt,))
                for dc in range(8):
                    pi = 4 + dc % 2
                    k.mm(ps[pi][:], B_ps[pi], [(wout[:, c, dc * 128:(dc + 1) * 128], mt[:, c, :]) for c in range(8)],
                         reads=(B_wp, B_mt))
                    k.op("dve", lambda t, pi=pi, dc=dc: t.tensor_tensor(out=hT[:, dc, cols], in0=ps[pi][:], in1=hT[:, dc, cols],
                                                                        op=ALU.add),
                         reads=(B_ps[pi], B_h[dc][tg]), writes=(B_h[dc][tg],))
            k.barrier()

    def ffn_phase(l):
        with ExitStack() as p:
            def al(name, shape, dt):
                UID[0] += 1
                return p.enter_context(nc.sbuf_tensor("%s_%d" % (name, UID[0]), list(shape), dt))
            hn = al("hn2", [128, 8, 512], BF16)
            B_hn = Buf()
            sq = al("sq2", [128, 8, 512], BF16)
            rstd = al("rstd2", [128, 512], F32)
            B_sq, B_rstd = Buf(), Buf()
            wgu = [al("wgu%d" % i, [128, 8, 512], BF16) for i in range(2)]
            B_wgu = [Buf(), Buf()]
            wd = [al("wd%d" % i, [128, NFC, 256], BF16) for i in range(2)]
            B_wd = [Buf(), Buf()]
            actT = al("actT", [128, NFC, 512], BF16)
            B_act = Buf()
            sg = [al("sg%d" % i, [128, 512], F32) for i in range(2)]
            B_sg = [Buf(), Buf()]
            wc = [0, 0]
            for tg in range(NTG):
                cols = slice(tg * 512, (tg + 1) * 512)
                rmsnorm_tg(tg, DEPTH + l, sq, B_sq, rstd, B_rstd, lambda c: (hn[:, c, :], (B_hn,)))

                def load_gu(fg):
                    i = wc[0] % 2
                    wc[0] += 1
                    k.dma("sp", wgu[i][:, :, 0:256],
                          wb_gu[l][:, fg * 256:(fg + 1) * 256].rearrange("(c p) n -> p c n", p=128),
                          reads=(B_w["in"],), writes=(B_wgu[i],))
                    k.dma("sp", wgu[i][:, :, 256:512],
                          wb_gu[l][:, FF + fg * 256:FF + (fg + 1) * 256].rearrange("(c p) n -> p c n", p=128),
                          reads=(B_w["in"],), writes=(B_wgu[i],))
                    return i
                nfg = NFC // 2
                cur = load_gu(0)
                for fg in range(nfg):
                    nxt = load_gu(fg + 1) if fg + 1 < nfg else None
                    w, Bw = wgu[cur], B_wgu[cur]
                    for j in range(2):
                        fcn = fg * 2 + j
                        i2 = fcn % 2
                        pg, pu = 1 + i2 * 2, 2 + i2 * 2
                        k.mm(ps[pg][:], B_ps[pg], [(w[:, c, j * 128:(j + 1) * 128], hn[:, c, :]) for c in range(8)],
                             reads=(Bw, B_hn))
                        k.mm(ps[pu][:], B_ps[pu], [(w[:, c, 256 + j * 128:256 + (j + 1) * 128], hn[:, c, :]) for c in range(8)],
                             reads=(Bw, B_hn))
                        k.op("act", lambda t, i2=i2, pg=pg: t.activation(out=sg[i2][:], in_=ps[pg][:], func=AF.Silu),
                             reads=(B_ps[pg],), writes=(B_sg[i2],))
                        k.op("dve", lambda t, i2=i2, pu=pu, fcn=fcn: t.tensor_tensor(out=actT[:, fcn, :], in0=ps[pu][:],
                                                                                     in1=sg[i2][:], op=ALU.mult),
                             reads=(B_ps[pu], B_sg[i2]), writes=(B_act,))
                    cur = nxt

                def load_wd(dg):
                    i = wc[1] % 2
                    wc[1] += 1
                    k.dma("sp", wd[i][:], wb_dn[l][:, dg * 256:(dg + 1) * 256].rearrange("(c p) n -> p c n", p=128),
                          reads=(B_w["in"],), writes=(B_wd[i],))
                    return i
                cur = load_wd(0)
                for dg in range(4):
                    nxt = load_wd(dg + 1) if dg + 1 < 4 else None
                    for j in range(2):
                        dc = dg * 2 + j
                        pi = 5 + dc % 2
                        k.mm(ps[pi][:], B_ps[pi], [(wd[cur][:, f, j * 128:(j + 1) * 128], actT[:, f, :]) for f in range(NFC)],
                             reads=(B_wd[cur], B_act))
                        k.op("dve", lambda t, pi=pi, dc=dc: t.tensor_tensor(out=hT[:, dc, cols], in0=ps[pi][:],
                                                                            in1=hT[:, dc, cols], op=ALU.add),
                             reads=(B_ps[pi], B_h[dc][tg]), writes=(B_h[dc][tg],))
                    cur = nxt
            k.barrier()

    for s in range(NSEQ):
        for c in range(8):
            k.dma("sp", hT[:, c, :], xT[s, c * 128:(c + 1) * 128, :], writes=B_h[c])
        for l in range(DEPTH):
            k.new_epoch()
            layer(l)
        if debug in ("p1", "p2", "p3") or (isinstance(debug, str) and debug.startswith("g")):
            break
        with ExitStack() as p:
            sq = p.enter_context(nc.sbuf_tensor("sqf_%d" % s, [128, 8, 512], BF16))
            rstd = p.enter_context(nc.sbuf_tensor("rstdf_%d" % s, [128, 512], F32))
            of = [p.enter_context(nc.sbuf_tensor("of%d_%d" % (i, s), [128, 8, 512], F32)) for i in range(2)]
            B_sq, B_rstd = Buf(), Buf()
            B_of = [Buf(), Buf()]
            for tg in range(NTG):
                o, Bo = of[tg % 2], B_of[tg % 2]
                rmsnorm_tg(tg, 2 * DEPTH, sq, B_sq, rstd, B_rstd, lambda c, o=o, Bo=Bo: (o[:, c, :], (Bo,)))
                k.dma("pool", outT[s][:, tg * 512:(tg + 1) * 512].rearrange("(c p) n -> p c n", p=128), o[:],
                      reads=(Bo,))
            k.barrier()
    k.barrier()
    return nc, ctx, consts


_CACHE = {}


def _prep_inputs(inputs, S, DEPTH):
    consts, _ = host_consts(S)
    gl = [inputs["norm_mix"][l] for l in range(DEPTH)] + [inputs["norm_ffn"][l] for l in range(DEPTH)] + \
         [inputs["norm_final"]]
    gvec = np.stack([np.asarray(g, np.float32).reshape(8, 128).T for g in gl], 1)
    shared = {
        "w_in": np.ascontiguousarray(inputs["w_in"], np.float32),
        "w_ret_o": np.ascontiguousarray(inputs["w_ret_o"], np.float32),
        "w_sb_o": np.ascontiguousarray(inputs["w_sb_o"], np.float32),
        "w_out": np.ascontiguousarray(inputs["w_out"], np.float32),
        "w_gate_up": np.ascontiguousarray(inputs["w_gate_up"], np.float32),
        "w_down": np.ascontiguousarray(inputs["w_down"], np.float32),
        "gvec": np.ascontiguousarray(gvec.reshape(128, -1), np.float32),
    }
    for kk, v in consts.items():
        shared["c_" + kk] = np.ascontiguousarray(v, np.float32)
    return shared


def kernel(x, norm_mix, w_in, w_ret_o, w_sb_o, w_out, norm_ffn, w_gate_up, w_down, norm_final):
    x = np.asarray(x, np.float32)
    B, S, _ = x.shape
    DEPTH = np.asarray(w_in).shape[0]
    ncores = 8
    NSEQ = B // ncores
    inputs = dict(norm_mix=np.asarray(norm_mix), w_in=np.asarray(w_in), w_ret_o=np.asarray(w_ret_o),
                  w_sb_o=np.asarray(w_sb_o), w_out=np.asarray(w_out), norm_ffn=np.asarray(norm_ffn),
                  w_gate_up=np.asarray(w_gate_up), w_down=np.asarray(w_down), norm_final=np.asarray(norm_final))
    shared = _prep_inputs(inputs, S, DEPTH)
    nc, ctx, _ = build_program(NSEQ, S, DEPTH)
    in_maps = []
    for c in range(ncores):
        m = dict(shared)
        m["xT"] = np.ascontiguousarray(x[c * NSEQ:(c + 1) * NSEQ].transpose(0, 2, 1))
        in_maps.append(m)
    res = run_bass_kernel_spmd(nc, in_maps, core_ids=list(range(ncores)))
    out = np.empty((B, S, D), np.float32)
    for c in range(ncores):
        out[c * NSEQ:(c + 1) * NSEQ] = res.results[c]["outT"].transpose(0, 2, 1)
    return out
```

```python
import numpy as np
from contextlib import ExitStack
import concourse.bass as bass
import concourse.mybir as mybir
from concourse.bass_utils import run_bass_kernel_spmd

F32, BF16 = mybir.dt.float32, mybir.dt.bfloat16
AF = mybir.ActivationFunctionType
ALU = mybir.AluOpType
AX = mybir.AxisListType

D = 1024
NC8 = 8
RH, RDK, RDV = 4, 128, 256
SBH, SBD = 8, 64
FF = 2816
NFC = FF // 128
INC = 6656
EPS = 1e-6
O_RQ, O_RK, O_RV, O_RG, O_SQ, O_SK, O_SV, O_GR, O_GS = 0, 512, 1024, 2048, 3072, 3584, 4096, 4608, 5632


class Buf:
    __slots__ = ("w", "r", "excl")

    def __init__(self, excl=False):
        self.w = {}
        self.r = {}
        self.excl = excl


class KB:
    def __init__(self, nc, ctx):
        self.nc, self.ctx = nc, ctx
        self.engs = {"pe": nc.tensor, "act": nc.scalar, "dve": nc.vector, "pool": nc.gpsimd, "sp": nc.sync}
        self.sems = []
        self.esem = {}
        self.seen = {e: {} for e in self.engs}
        for e in self.engs:
            self.new_eng_sem(e)
        self.dpool = {}
        for q in ("sp", "pool"):
            self.dpool[q] = [[self._new_sem("d%s%d" % (q, i)), 0] for i in range(12)]
        self.dnext = {"sp": 0, "pool": 0}
        self.dsids = set(sl[0] for q in self.dpool for sl in self.dpool[q])

    def _new_sem(self, name):
        h = self.ctx.enter_context(self.nc.semaphore(name))
        self.sems.append(h)
        return len(self.sems) - 1

    def new_eng_sem(self, e):
        self.esem[e] = [self._new_sem("e%s%d" % (e, len(self.sems))), 0]

    def new_epoch(self):
        for e in ("pe", "act", "dve", "pool"):
            if self.esem[e][1] > 12000:
                self.new_eng_sem(e)

    def _waits(self, e, reads, writes):
        need = {}
        mysid0 = self.esem[e][0]
        for b in reads:
            for sid, v in b.w.items():
                need[sid] = max(need.get(sid, 0), v)
            if b.excl:
                for sid, v in b.r.items():
                    if sid != mysid0:
                        need[sid] = max(need.get(sid, 0), v)
        for b in writes:
            for sid, v in b.w.items():
                need[sid] = max(need.get(sid, 0), v)
            for sid, v in b.r.items():
                need[sid] = max(need.get(sid, 0), v)
        mysid = self.esem[e][0]
        for sid, v in need.items():
            if sid == mysid and e == "pe":
                continue
            if self.seen[e].get(sid, 0) >= v:
                continue
            self.engs[e].wait_ge(self.sems[sid], v)
            self.seen[e][sid] = v

    def op(self, e, fn, reads=(), writes=(), inc=True):
        self._waits(e, reads, writes)
        ins = fn(self.engs[e])
        mysid = self.esem[e][0]
        tgt = self.esem[e][1] + 1
        if inc:
            ins.then_inc(self.sems[mysid], 1)
            self.esem[e][1] = tgt
        for b in reads:
            b.r[mysid] = max(b.r.get(mysid, 0), tgt)
        for b in writes:
            b.w = {mysid: tgt}
            b.r = {}
        return ins

    def dma(self, q, out, in_, reads=(), writes=()):
        self._waits(q, reads, writes)
        pool = self.dpool[q]
        slot = pool[self.dnext[q] % len(pool)]
        self.dnext[q] += 1
        sid = slot[0]
        if slot[1] > 0 and self.seen[q].get(sid, 0) < slot[1] * 16:
            self.engs[q].wait_ge(self.sems[sid], slot[1] * 16)
            self.seen[q][sid] = slot[1] * 16
        ins = self.engs[q].dma_start(out=out, in_=in_)
        slot[1] += 1
        v = slot[1] * 16
        ins.then_inc(self.sems[sid], 16)
        for b in reads:
            b.r[sid] = max(b.r.get(sid, 0), v)
        for b in writes:
            b.w = {ks: kv for ks, kv in b.w.items() if ks in self.dsids}
            b.w[sid] = v
            b.r = {}

    def barrier(self):
        tg = {}
        for e in ("pe", "act", "dve", "pool"):
            sid, c = self.esem[e]
            if c > 0:
                tg[sid] = c
        for q in ("sp", "pool"):
            for sid, c in self.dpool[q]:
                if c > 0:
                    tg[sid] = c * 16
        for e in self.engs:
            for sid, v in tg.items():
                if self.seen[e].get(sid, 0) >= v:
                    continue
                self.engs[e].wait_ge(self.sems[sid], v)
                self.seen[e][sid] = v

    def mm(self, ps, psb, pairs, reads, first=True, last=True):
        n = len(pairs)
        for i, (l, r) in enumerate(pairs):
            st = first and i == 0
            sp = last and i == n - 1
            self.op("pe", lambda t, l=l, r=r, st=st, sp=sp: t.matmul(ps, l, r, start=st, stop=sp),
                    reads=reads if i == 0 else (), writes=(psb,), inc=(i == n - 1))


def host_consts(S):
    c = {}
    ar = np.arange(128)
    c["ident"] = np.eye(128, dtype=np.float32)
    c["negmask"] = np.where(ar[:, None] >= ar[None, :], -30000.0, 0.0).astype(np.float32)
    c["negu"] = np.where(ar[:, None] >= ar[None, :], -1.0, 0.0).astype(np.float32)
    prot = np.zeros((128, 128), np.float32)
    for m in range(64):
        prot[m + 64, m] = 1.0
        prot[m, m + 64] = 1.0
    c["prot"] = prot
    c["onesm"] = np.full((128, 128), 1.0 / 1024, np.float32)
    sel = np.zeros((128, 16, 48), np.float32)
    for kb in range(16):
        sel[:, kb, kb] = 1.0
        sel[:, kb, 32 + kb] = 1.0
    c["sel"] = sel.reshape(128, 16 * 48)
    ns = np.zeros((128, 16, 128), np.float32)
    for kb in range(16):
        for r in range(16):
            if r > kb:
                ns[r, kb, :] = -1.0
                ns[32 + r, kb, :] = -1.0
    c["negstep"] = ns.reshape(128, 16 * 128)
    half = 64
    inv = (1.0 / (np.float32(10000.0) ** (np.arange(half, dtype=np.float32) / np.float32(half)))).astype(np.float32)
    pos = np.arange(S, dtype=np.float32)
    ang = (pos[:, None] * inv[None, :]).astype(np.float32)
    cos = np.cos(ang).astype(np.float32).T
    sin = np.sin(ang).astype(np.float32).T
    cosf = np.concatenate([cos, cos], 0)
    sinf = np.concatenate([-sin, sin], 0)
    ksc = np.float32(RDK ** -0.5)
    c["rope"] = np.stack([cosf, sinf, cosf * ksc, sinf * ksc], 1).astype(np.float32).reshape(128, 4 * S)
    lg = np.log1p(-np.exp2(-5.0 - np.arange(RH, dtype=np.float32))).astype(np.float32)
    i = np.arange(128, dtype=np.float32)
    diff = i[None, :] - i[:, None]
    dect = np.where(diff[None] >= 0, np.exp(lg[:, None, None] * np.maximum(diff, 0.0)[None]), 0.0)
    c["dect"] = np.ascontiguousarray(dect.transpose(1, 0, 2)).astype(np.float32).reshape(128, RH * 128)
    c["kdec"] = np.exp(lg[None, :] * (127.0 - i)[:, None]).astype(np.float32)
    qdec = np.exp(lg[:, None] * (i + 1.0)[None, :]).astype(np.float32)
    c["qdec"] = np.broadcast_to(qdec[None], (128, RH, 128)).astype(np.float32).reshape(128, RH * 128).copy()
    cd = np.exp(lg * 128.0).astype(np.float32)
    return c, [float(x) for x in cd]


def build_program(NSEQ, S, DEPTH, debug=False):
    assert S % 512 == 0
    NTG = S // 512
    NTB = S // 128
    consts, CD = host_consts(S)
    nc = bass.Bass("TRN2", target_bir_lowering=False)
    ctx = ExitStack()

    def din(name, shape, dt=F32):
        return nc.dram_tensor(name, list(shape), dt, kind="ExternalInput").ap()

    def dscr(name, shape, dt=BF16):
        return nc.dram_tensor(name, list(shape), dt, kind=("ExternalOutput" if debug else "Internal")).ap()

    xT = din("xT", [NSEQ, D, S])
    outT = nc.dram_tensor("outT", [NSEQ, D, S], F32, kind="ExternalOutput").ap()
    w_in = din("w_in", [DEPTH, D, INC])
    w_ret_o = din("w_ret_o", [DEPTH, D, D])
    w_sb_o = din("w_sb_o", [DEPTH, 512, D])
    w_out = din("w_out", [DEPTH, D, D])
    w_gu = din("w_gate_up", [DEPTH, D, 2 * FF])
    w_dn = din("w_down", [DEPTH, FF, D])
    gvec = din("gvec", [128, (2 * DEPTH + 1) * 8])
    cin = {k: din("c_" + k, v.shape) for k, v in consts.items()}
    wb_in = dscr("wb_in", [DEPTH, D, INC])
    wb_ret_o = dscr("wb_ret_o", [DEPTH, D, D])
    wb_sb_o = dscr("wb_sb_o", [DEPTH, 512, D])
    wb_out = dscr("wb_out", [DEPTH, D, D])
    wb_gu = dscr("wb_gu", [DEPTH, D, 2 * FF])
    wb_dn = dscr("wb_dn", [DEPTH, FF, D])
    s_rqt = dscr("s_rqt", [4, 128, S])
    s_rkt = dscr("s_rkt", [4, 128, S])
    s_rv = dscr("s_rv", [S, 1024])
    s_rg = dscr("s_rg", [S, 1024])
    s_sqt = dscr("s_sqt", [4, 128, S])
    s_skt = dscr("s_skt", [4, 128, S])
    s_sv = dscr("s_sv", [S, 512])
    s_grt = dscr("s_grt", [8, 128, S])
    s_gst = dscr("s_gst", [8, 128, S])
    s_sbt = dscr("s_sbt", [4, 128, S])
    s_retgt = dscr("s_retgt", [8, 128, S])
    B_scr = {n: [Buf() for _ in range(16)] for n in
             ("rqt", "rkt", "rv", "rg", "sqt", "skt", "sv", "grt", "gst", "sbt", "retgt")}
    B_w = {n: Buf() for n in ("in", "ret_o", "sb_o", "out", "gu", "dn")}

    def sb(name, shape, dt):
        return ctx.enter_context(nc.sbuf_tensor(name, list(shape), dt))

    k = KB(nc, ctx)
    UID = [0]
    hT = sb("hT", [128, 8, S], F32)
    B_h = [[Buf() for _ in range(NTG)] for _ in range(8)]
    identb = sb("identb", [128, 128], BF16)
    negmaskb = sb("negmaskb", [128, 128], BF16)
    negub = sb("negub", [128, 128], BF16)
    protb = sb("protb", [128, 128], BF16)
    onesmb = sb("onesmb", [128, 128], BF16)
    selb = sb("selb", [128, 16, 48], BF16)
    negstepb = sb("negstepb", [128, 16, 128], BF16)
    dect = sb("dect", [128, RH, 128], F32)
    kdec = sb("kdec", [128, RH], F32)
    qdec = sb("qdec", [128, RH, 128], F32)
    gv = sb("gv", [128, 2 * DEPTH + 1, 8], F32)
    epsb = sb("epsb", [128, 1], F32)
    B_const = Buf()
    ps = [ctx.enter_context(nc.psum_tensor("ps%d" % i, [128, 512], F32)) for i in range(7)]
    pT = ctx.enter_context(nc.psum_tensor("pT", [128, 1024], BF16))
    B_ps = [Buf(excl=True) for _ in range(7)]
    B_pT = Buf(excl=True)
    rr = {"evac": 0}

    def evac_eng():
        rr["evac"] += 1
        return "act" if rr["evac"] % 2 else "dve"

    def copy_op(e, out, in_, reads, writes):
        if e == "act":
            k.op("act", lambda t: t.activation(out=out, in_=in_, func=AF.Copy), reads=reads, writes=writes)
        else:
            k.op(e, lambda t: t.tensor_copy(out=out, in_=in_), reads=reads, writes=writes)

    with ExitStack() as pctx:
        stg = [pctx.enter_context(nc.sbuf_tensor("stg%d" % i, [128, 2048], F32)) for i in range(2)]
        stb = [pctx.enter_context(nc.sbuf_tensor("stb%d" % i, [128, 2048], BF16)) for i in range(2)]
        B_stg = [Buf(), Buf()]
        B_stb = [Buf(), Buf()]
        cnt = [0]

        def conv(dst_sb_ap, src_dram, ncol, npart=128):
            i = cnt[0] % 2
            cnt[0] += 1
            k.dma("sp", stg[i][0:npart, 0:ncol], src_dram, writes=(B_stg[i],))
            copy_op("dve", dst_sb_ap, stg[i][0:npart, 0:ncol], (B_stg[i],), (B_const,))

        conv(identb[:], cin["ident"], 128)
        conv(negmaskb[:], cin["negmask"], 128)
        conv(negub[:], cin["negu"], 128)
        conv(protb[:], cin["prot"], 128)
        conv(onesmb[:], cin["onesm"], 128)
        conv(selb[:].rearrange("p a b -> p (a b)"), cin["sel"], 16 * 48)
        conv(negstepb[:].rearrange("p a b -> p (a b)"), cin["negstep"], 2048)
        k.dma("sp", dect[:].rearrange("p a b -> p (a b)"), cin["dect"], writes=(B_const,))
        k.dma("sp", kdec[:], cin["kdec"], writes=(B_const,))
        k.dma("sp", qdec[:].rearrange("p a b -> p (a b)"), cin["qdec"], writes=(B_const,))
        k.dma("sp", gv[:].rearrange("p a b -> p (a b)"), gvec, writes=(B_const,))
        k.op("dve", lambda t: t.memset(epsb[:], EPS), writes=(B_const,))

        def conv_w(dst, src, R, C):
            for r0 in range(0, R, 128):
                for c0 in range(0, C, 2048):
                    cw = min(2048, C - c0)
                    i = cnt[0] % 2
                    e = ("dve", "act", "pool")[cnt[0] % 3]
                    cnt[0] += 1
                    k.dma("sp", stg[i][:, 0:cw], src[r0:r0 + 128, c0:c0 + cw], writes=(B_stg[i],))
                    copy_op(e, stb[i][:, 0:cw], stg[i][:, 0:cw], (B_stg[i],), (B_stb[i],))
                    k.dma("pool", dst[r0:r0 + 128, c0:c0 + cw], stb[i][:, 0:cw], reads=(B_stb[i],),
                          writes=(B_w["in"],))

        for l in range(DEPTH):
            conv_w(wb_in[l], w_in[l], D, INC)
            conv_w(wb_ret_o[l], w_ret_o[l], D, D)
            conv_w(wb_sb_o[l], w_sb_o[l], 512, D)
            conv_w(wb_out[l], w_out[l], D, D)
            conv_w(wb_gu[l], w_gu[l], D, 2 * FF)
            conv_w(wb_dn[l], w_dn[l], FF, D)
        k.barrier()

    if debug == "p0":
        return nc, ctx, consts
    def rmsnorm_tg(tg, gidx, sq, B_sq, rstd, B_rstd, out_fn):
        cols = slice(tg * 512, (tg + 1) * 512)
        k.op("act", lambda t: t.activation(out=sq[:], in_=hT[:, :, cols], func=AF.Square),
             reads=[B_h[c][tg] for c in range(8)], writes=(B_sq,))
        k.mm(ps[0][:], B_ps[0], [(onesmb[:], sq[:, c, :]) for c in range(8)], reads=(B_sq, B_const))
        k.op("act", lambda t: t.activation(out=rstd[:], in_=ps[0][:], func=AF.Ln, bias=epsb[:]),
             reads=(B_ps[0], B_const), writes=(B_rstd,))
        k.op("act", lambda t: t.activation(out=rstd[:], in_=rstd[:], func=AF.Exp, scale=-0.5),
             reads=(B_rstd,), writes=(B_rstd,))
        for c in range(8):
            o, ob = out_fn(c)
            k.op("dve", lambda t, o=o, c=c: t.scalar_tensor_tensor(
                out=o, in0=hT[:, c, cols], scalar=gv[:, gidx, c:c + 1], in1=rstd[:], op0=ALU.mult, op1=ALU.mult),
                reads=(B_h[c][tg], B_rstd, B_const), writes=ob)

    def layer(l):
        with ExitStack() as p:
            def al(name, shape, dt):
                UID[0] += 1
                return p.enter_context(nc.sbuf_tensor("%s_%d" % (name, UID[0]), list(shape), dt))
            hnT = al("hnT", [128, 8, S], BF16)
            B_hn = [Buf() for _ in range(NTG)]
            sq = al("sq", [128, 8, 512], BF16)
            rstd = al("rstd", [128, 512], F32)
            B_sq, B_rstd = Buf(), Buf()
            for tg in range(NTG):
                rmsnorm_tg(tg, l, sq, B_sq, rstd, B_rstd,
                           lambda c, tg=tg: (hnT[:, c, tg * 512:(tg + 1) * 512], (B_hn[tg],)))
            wt = [al("wt%d" % i, [128, 8, 512], BF16) for i in range(2)]
            B_wt = [Buf(), Buf()]
            ost = [al("ost%d" % i, [128, max(S, 2048)], BF16) for i in range(2)]
            B_ost = [Buf(), Buf()]
            rot = al("rot", [128, 4, 512], F32)
            B_rot = Buf()
            xb = al("xb", [128, 512], BF16)
            B_xb = Buf()
            t1 = al("t1", [128, 512], F32)
            t2 = al("t2", [128, 512], F32)
            B_t1, B_t2 = Buf(), Buf()
            st = {"ps": 1, "ost": 0}

            def next_ps():
                st["ps"] = 1 + (st["ps"] % 5)
                return st["ps"]

            def load_w(gi):
                i = gi % 2
                k.dma("sp", wt[i][:], wb_in[l][:, gi * 512:(gi + 1) * 512].rearrange("(c p) n -> p c n", p=128),
                      reads=(B_w["in"],), writes=(B_wt[i],))

            NG = INC // 512
            load_w(0)
            rope_v = cin["rope"].rearrange("p (a s) -> p a s", a=4)
            for gi in range(NG):
                if isinstance(debug, str) and debug.startswith("g") and gi >= int(debug[1:2]):
                    break
                stage = debug[2:] if isinstance(debug, str) and debug.startswith("g") else ""

                if gi + 1 < NG:
                    load_w(gi + 1)
                w = wt[gi % 2]
                Bw = B_wt[gi % 2]
                col0 = gi * 512
                if col0 in (O_RQ, O_RK):
                    isk = col0 == O_RK
                    dst = s_rkt if isk else s_rqt
                    Bd = B_scr["rkt" if isk else "rqt"]
                    for tg in range(NTG):
                        k.dma("sp", rot[:], rope_v[:, :, tg * 512:(tg + 1) * 512], writes=(B_rot,))
                        for h in range(4):
                            pi = next_ps()
                            k.mm(ps[pi][:], B_ps[pi],
                                 [(w[:, c, h * 128:(h + 1) * 128], hnT[:, c, tg * 512:(tg + 1) * 512]) for c in range(8)],
                                 reads=(Bw, B_hn[tg]))
                            if stage == "a":
                                continue
                            copy_op("act", xb[:], ps[pi][:], (B_ps[pi],), (B_xb,))
                            if stage == "b":
                                continue
                            k.mm(ps[6][:], B_ps[6], [(protb[:], xb[:])], reads=(B_xb, B_const))
                            if stage == "c":
                                continue
                            tb = 2 if isk else 0
                            k.op("dve", lambda t, pi=pi, tb=tb: t.tensor_tensor(
                                out=t1[:], in0=ps[pi][:], in1=rot[:, tb, :], op=ALU.mult),
                                reads=(B_ps[pi], B_rot), writes=(B_t1,))
                            k.op("dve", lambda t, tb=tb: t.tensor_tensor(
                                out=t2[:], in0=ps[6][:], in1=rot[:, tb + 1, :], op=ALU.mult),
                                reads=(B_ps[6], B_rot), writes=(B_t2,))
                            if stage == "d":
                                continue
                            o = ost[h % 2]
                            k.op("dve", lambda t, o=o, tg=tg: t.tensor_tensor(
                                out=o[:, tg * 512:(tg + 1) * 512], in0=t1[:], in1=t2[:], op=ALU.add),
                                reads=(B_t1, B_t2), writes=(B_ost[h % 2],))
                            if stage == "e":
                                continue
                            k.dma("pool", dst[h][:, tg * 512:(tg + 1) * 512], o[:, tg * 512:(tg + 1) * 512],
                                  reads=(B_ost[h % 2],), writes=(Bd[h],))
                elif col0 in (O_SQ, O_SK, O_GR, O_GR + 512, O_GS, O_GS + 512):
                    if col0 == O_SQ:
                        dst, Bd, base, fn = s_sqt, B_scr["sqt"], 0, AF.Identity
                    elif col0 == O_SK:
                        dst, Bd, base, fn = s_skt, B_scr["skt"], 0, AF.Copy
                    elif col0 >= O_GS:
                        dst, Bd, base, fn = s_gst, B_scr["gst"], (col0 - O_GS) // 128, AF.Sigmoid
                    else:
                        dst, Bd, base, fn = s_grt, B_scr["grt"], (col0 - O_GR) // 128, AF.Sigmoid
                    for fc in range(4):
                        oi = st["ost"] % 2
                        st["ost"] += 1
                        o = ost[oi]
                        for tg in range(NTG):
                            pi = next_ps()
                            k.mm(ps[pi][:], B_ps[pi],
                                 [(w[:, c, fc * 128:(fc + 1) * 128], hnT[:, c, tg * 512:(tg + 1) * 512]) for c in range(8)],
                                 reads=(Bw, B_hn[tg]))
                            if fn == AF.Copy:
                                copy_op(evac_eng(), o[:, tg * 512:(tg + 1) * 512], ps[pi][:], (B_ps[pi],), (B_ost[oi],))
                            elif fn == AF.Identity:
                                k.op("dve", lambda t, o=o, tg=tg, pi=pi: t.tensor_scalar(
                                    out=o[:, tg * 512:(tg + 1) * 512], in0=ps[pi][:], scalar1=0.125, scalar2=0.0,
                                    op0=ALU.mult, op1=ALU.add),
                                    reads=(B_ps[pi],), writes=(B_ost[oi],))
                            else:
                                k.op("act", lambda t, o=o, tg=tg, pi=pi: t.activation(
                                    out=o[:, tg * 512:(tg + 1) * 512], in_=ps[pi][:], func=AF.Sigmoid),
                                    reads=(B_ps[pi],), writes=(B_ost[oi],))
                        k.dma("pool", dst[base + fc], o[:, 0:S], reads=(B_ost[oi],), writes=(Bd[base + fc],))
                else:
                    if col0 >= O_SV:
                        dst, Bd, cb, fn = s_sv, B_scr["sv"], 0, AF.Copy
                    elif col0 >= O_RG:
                        dst, Bd, cb, fn = s_rg, B_scr["rg"], col0 - O_RG, AF.Silu
                    else:
                        dst, Bd, cb, fn = s_rv, B_scr["rv"], col0 - O_RV, AF.Copy
                    for tq in range(NTB // 4):
                        oi = st["ost"] % 2
                        st["ost"] += 1
                        o = ost[oi]
                        for tb4 in range(4):
                            tb = tq * 4 + tb4
                            pi = next_ps()
                            k.mm(ps[pi][:], B_ps[pi],
                                 [(hnT[:, c, tb * 128:(tb + 1) * 128], w[:, c, :]) for c in range(8)],
                                 reads=(Bw, B_hn[tb // 4]))
                            if fn == AF.Copy:
                                copy_op(evac_eng(), o[:, tb4 * 512:(tb4 + 1) * 512], ps[pi][:], (B_ps[pi],), (B_ost[oi],))
                            else:
                                k.op("act", lambda t, o=o, tb4=tb4, pi=pi: t.activation(
                                    out=o[:, tb4 * 512:(tb4 + 1) * 512], in_=ps[pi][:], func=AF.Silu),
                                    reads=(B_ps[pi],), writes=(B_ost[oi],))
                        k.dma("pool",
                              dst[tq * 512:(tq + 1) * 512, cb:cb + 512].rearrange("(t p) n -> p t n", p=128),
                              o[:, 0:2048].rearrange("p (t n) -> p t n", n=512),
                              reads=(B_ost[oi],), writes=(Bd[tq],))
            k.barrier()
        if debug == "p1" or (isinstance(debug, str) and debug.startswith("g")):
            return
        sb_phase(l)
        if debug == "p2":
            return
        ret_phase(l)
        if debug == "p3":
            return
        post_phase(l)
        if debug == "p4":
            return
        ffn_phase(l)

    def sb_phase(l):
        with ExitStack() as p:
            def al(name, shape, dt):
                UID[0] += 1
                return p.enter_context(nc.sbuf_tensor("%s_%d" % (name, UID[0]), list(shape), dt))
            qa = al("qa", [128, S], BF16)
            qb = al("qb", [128, S], BF16)
            kt = al("kt", [128, S], BF16)
            sv = al("sv", [128, NTB, 128], BF16)
            B_q, B_k, B_v = Buf(), Buf(), Buf()
            spb = [al("spb%d" % i, [128, NTB, 512], BF16) for i in range(2)]
            B_sp = [Buf(), Buf()]
            et = [al("et%d" % i, [128, 512], F32) for i in range(2)]
            B_et = [Buf(), Buf()]
            at = [al("at%d" % i, [128, 512], BF16) for i in range(4)]
            B_at = [Buf() for _ in range(4)]
            hl = [al("hl%d" % i, [128, 512], BF16) for i in range(2)]
            B_hl = [Buf(), Buf()]
            osb = al("osb", [128, S], BF16)
            B_osb = Buf()
            for i in range(2):
                k.op("dve", lambda t, i=i: t.memset(hl[i][:], 0.0), writes=(B_hl[i],))
            k.op("dve", lambda t: t.memset(qa[64:128, :], 0.0), writes=(B_q,))
            k.op("dve", lambda t: t.memset(qb[0:64, :], 0.0), writes=(B_q,))
            cnt = {"z": 0, "t": 0, "e": 0, "a": 0}
            tst = {}
            for c in range(4):
                k.dma("sp", qa[0:64, :], s_sqt[c][0:64, :], reads=(B_scr["sqt"][c],), writes=(B_q,))
                k.dma("sp", qb[64:128, :], s_sqt[c][64:128, :], reads=(B_scr["sqt"][c],), writes=(B_q,))
                k.dma("sp", kt[:], s_skt[c], reads=(B_scr["skt"][c],), writes=(B_k,))
                k.dma("sp", sv[:], s_sv[:, c * 128:(c + 1) * 128].rearrange("(t p) n -> p t n", p=128),
                      reads=[B_scr["sv"][i] for i in range(NTB // 4)], writes=(B_v,))
                units = [(g, hh) for g in range(NTG) for hh in range(2)]

                def geom(g, kb):
                    lo = max(0, 128 * kb - 512 * g)
                    return lo, kb >= 4 * g

                def p1_block(u, kb):
                    g, hh = units[u]
                    q = qa if hh == 0 else qb
                    spt, Bs = spb[u % 2], B_sp[u % 2]
                    lo, diag = geom(g, kb)
                    zi = cnt["z"] % 2
                    cnt["z"] += 1
                    z, Bz = ps[zi], B_ps[zi]
                    ks = kt[:, kb * 128:(kb + 1) * 128]
                    q0 = g * 512
                    if diag:
                        k.mm(z[:, lo:lo + 128], Bz, [(identb[:], negmaskb[:]), (ks, q[:, q0 + lo:q0 + lo + 128])],
                             reads=(B_const, B_k, B_q))
                        if lo + 128 < 512:
                            k.mm(z[:, lo + 128:512], Bz, [(ks, q[:, q0 + lo + 128:q0 + 512])], reads=(B_k, B_q))
                    else:
                        k.mm(z[:, lo:512], Bz, [(ks, q[:, q0 + lo:q0 + 512])], reads=(B_k, B_q))
                    ei = cnt["e"] % 2
                    cnt["e"] += 1
                    k.op("act", lambda t: t.activation(out=et[ei][:, lo:512], in_=z[:, lo:512], func=AF.Exp),
                         reads=(Bz,), writes=(B_et[ei],))
                    tst[("p1", u, kb)] = ei

                def p1_mid(u, kb):
                    g, hh = units[u]
                    spt, Bs = spb[u % 2], B_sp[u % 2]
                    lo, diag = geom(g, kb)
                    ei = tst[("p1", u, kb)]
                    k.op("act", lambda t: t.activation(out=spt[:, kb, lo:512], in_=et[ei][:, lo:512], func=AF.Ln, bias=1.0),
                         reads=(B_et[ei],), writes=(Bs,))

                def p1_back(u, kb):
                    g, hh = units[u]
                    spt, Bs = spb[u % 2], B_sp[u % 2]
                    lo, diag = geom(g, kb)
                    nkb = 4 * g + 4
                    k.op("pe", lambda t: t.matmul(ps[4][0:48, lo:512], selb[:, kb, :], spt[:, kb, lo:512],
                                                   start=(kb == 0), stop=(kb == nkb - 1)),
                         reads=(Bs, B_const), writes=(B_ps[4],))

                def p1_fin(u):
                    h_, Bh = hl[u % 2], B_hl[u % 2]
                    k.op("dve", lambda t: t.tensor_copy(out=h_[0:48, :], in_=ps[4][0:48, :]),
                         reads=(B_ps[4],), writes=(Bh,))
                    k.op("dve", lambda t: t.tensor_tensor(out=h_[32:48, :], in0=ps[4][32:48, :], in1=h_[32:48, :],
                                                          op=ALU.subtract),
                         reads=(B_ps[4], Bh), writes=(Bh,))

                def p2_block(u, kb):
                    g, hh = units[u]
                    q = qa if hh == 0 else qb
                    spt, Bs = spb[u % 2], B_sp[u % 2]
                    h_, Bh = hl[u % 2], B_hl[u % 2]
                    lo, diag = geom(g, kb)
                    ti = 2 + cnt["t"] % 2
                    cnt["t"] += 1
                    T, Bt = ps[ti], B_ps[ti]
                    ks = kt[:, kb * 128:(kb + 1) * 128]
                    q0 = g * 512
                    rd = (B_const, B_k, B_q, Bs, Bh)

                    def grp(a, b, withmask):
                        prs = []
                        if withmask:
                            prs.append((identb[:], negmaskb[:]))
                        prs.append((ks, q[:, q0 + a:q0 + b]))
                        prs.append((negub[:], spt[:, kb, a:b]))
                        prs.append((negstepb[:, kb, :], h_[:, a:b]))
                        k.mm(T[:, a:b], Bt, prs, reads=rd)
                    if diag:
                        grp(lo, lo + 128, True)
                        if lo + 128 < 512:
                            grp(lo + 128, 512, False)
                    else:
                        grp(lo, 512, False)
                    ai = cnt["a"] % 4
                    cnt["a"] += 1
                    k.op("act", lambda t: t.activation(out=at[ai][:, lo:512], in_=T[:, lo:512], func=AF.Exp),
                         reads=(Bt,), writes=(B_at[ai],))
                    tst[("p2", u, kb)] = ai

                def p2_back(u, kb):
                    g, hh = units[u]
                    lo, diag = geom(g, kb)
                    ai = tst[("p2", u, kb)]
                    nkb = 4 * g + 4
                    k.op("pe", lambda t: t.matmul(ps[5][hh * 64:(hh + 1) * 64, lo:512], sv[:, kb, hh * 64:(hh + 1) * 64],
                                                   at[ai][:, lo:512], start=(kb == 0), stop=(kb == nkb - 1)),
                         reads=(B_at[ai], B_v), writes=(B_ps[5],))

                def p2_fin(u):
                    g, hh = units[u]
                    if hh == 1:
                        copy_op("dve", osb[:, g * 512:(g + 1) * 512], ps[5][:], (B_ps[5],), (B_osb,))

                nu = len(units)
                q_mid, q_back = [], []

                def do_mid(t):
                    if t[0] == "p1":
                        p1_mid(t[1], t[2])

                def do_back(t):
                    if t[0] == "p1":
                        p1_back(t[1], t[2])
                    else:
                        p2_back(t[1], t[2])

                def flush():
                    for t in q_mid:
                        do_mid(t)
                    del q_mid[:]
                    for t in q_back:
                        do_back(t)
                    del q_back[:]

                def step(t):
                    if t[0] == "p1":
                        p1_block(t[1], t[2])
                    else:
                        p2_block(t[1], t[2])
                    for x in q_mid:
                        do_mid(x)
                    del q_mid[:]
                    q_mid.append(t)
                    q_back.append(t)
                    if len(q_back) > 2:
                        do_back(q_back.pop(0))

                markers = []

                def do_markers():
                    for kind, uu in markers:
                        if kind == "p1fin":
                            p1_fin(uu)
                        else:
                            p2_fin(uu)
                    del markers[:]

                for u in range(nu + 1):
                    n1 = 4 * units[u][0] + 4 if u < nu else 0
                    n2 = 4 * units[u - 1][0] + 4 if u >= 1 else 0
                    seq = []
                    for i in range(max(n1, n2)):
                        if i < n1:
                            seq.append(("p1", u, i))
                        if i < n2:
                            seq.append(("p2", u - 1, i))
                    if n1 >= 2 and n2 >= 1:
                        seq.remove(("p1", u, 1))
                        seq.insert(1, ("p1", u, 1))
                    if n1 < 2:
                        flush()
                        do_markers()
                    for idx, t in enumerate(seq):
                        step(t)
                        if idx == 1:
                            do_markers()
                    if u < nu:
                        markers.append(("p1fin", u))
                    if u >= 1:
                        markers.append(("p2fin", u - 1))
                flush()
                do_markers()
                k.dma("pool", s_sbt[c], osb[:], reads=(B_osb,), writes=(B_scr["sbt"][c],))
            k.barrier()

    def ret_phase(l):
        with ExitStack() as p:
            def al(name, shape, dt):
                UID[0] += 1
                return p.enter_context(nc.sbuf_tensor("%s_%d" % (name, UID[0]), list(shape), dt))
            rq = al("rq", [128, NTB, 128], BF16)
            rk = al("rk", [128, NTB, 128], BF16)
            rv = al("rv", [128, NTB, 256], BF16)
            rg = al("rg", [128, NTB, 256], BF16)
            B_rq, B_rk, B_rv, B_rg = Buf(), Buf(), Buf(), Buf()
            obuf = al("obuf", [128, NTB, 256], F32)
            wk = al("wk", [128, NTB, 256], F32)
            rball = al("rball", [128, NTB, 256], BF16)
            yb = al("yb", [128, NTB, 256], BF16)
            B_ob, B_wk, B_rb, B_yb = Buf(), Buf(), Buf(), Buf()
            stt = al("stt", [128, NTB, 128], BF16)
            qd = al("qd", [128, NTB, 128], BF16)
            kd = al("kd", [128, NTB, 128], BF16)
            B_stt, B_qd, B_kd = Buf(), Buf(), Buf()
            stats = al("stats", [128, 6, NTB], F32)
            B_stats = Buf()
            ogt = al("ogt", [128, 2, S], BF16)
            B_ogt = Buf()
            for h in range(RH):
                k.dma("sp", rq[:].rearrange("p a b -> p (a b)"), s_rqt[h], reads=(B_scr["rqt"][h],), writes=(B_rq,))
                k.dma("sp", rk[:].rearrange("p a b -> p (a b)"), s_rkt[h], reads=(B_scr["rkt"][h],), writes=(B_rk,))
                k.dma("sp", rv[:], s_rv[:, h * 256:(h + 1) * 256].rearrange("(t p) n -> p t n", p=128),
                      reads=[B_scr["rv"][i] for i in range(NTB // 4)], writes=(B_rv,))
                k.dma("sp", rg[:], s_rg[:, h * 256:(h + 1) * 256].rearrange("(t p) n -> p t n", p=128),
                      reads=[B_scr["rg"][i] for i in range(NTB // 4)], writes=(B_rg,))
                for n0 in range(0, NTB, 8):
                    nn = min(8, NTB - n0)
                    for j in range(nn):
                        k.op("pe", lambda t, j=j, n0=n0: t.transpose(out=pT[:, j * 128:(j + 1) * 128], in_=rk[:, n0 + j, :],
                                                                   identity=identb[:]),
                             reads=(B_rk, B_const), writes=(B_pT,), inc=(j == nn - 1))
                    k.op("dve", lambda t, n0=n0, nn=nn: t.tensor_scalar(
                        out=kd[:, n0:n0 + nn, :].rearrange("p a b -> p (a b)"), in0=pT[:, 0:nn * 128],
                        scalar1=kdec[:, h:h + 1], scalar2=0.0, op0=ALU.mult, op1=ALU.add),
                        reads=(B_pT, B_const), writes=(B_kd,))
                for n in range(1, NTB):
                    pass
                k.op("dve", lambda t: t.tensor_tensor(out=qd[:], in0=rq[:],
                                                      in1=qdec[:, h, :].unsqueeze(1).broadcast_to([128, NTB, 128]),
                                                      op=ALU.mult),
                     reads=(B_rq, B_const), writes=(B_qd,))
                for n0 in range(0, NTB, 4):
                    pi = (n0 // 4) % 2
                    for j in range(4):
                        n = n0 + j
                        k.op("pe", lambda t, n=n, j=j, pi=pi: t.matmul(ps[pi][:, j * 128:(j + 1) * 128], rk[:, n, :], rq[:, n, :],
                                                                       start=True, stop=True),
                             reads=(B_rk, B_rq), writes=(B_ps[pi],), inc=(j == 3))
                    k.op("dve", lambda t, n0=n0, pi=pi: t.tensor_tensor(
                        out=stt[:, n0:n0 + 4, :], in0=ps[pi][:, :].rearrange("p (a b) -> p a b", b=128),
                        in1=dect[:, h, :].unsqueeze(1).broadcast_to([128, 4, 128]), op=ALU.mult),
                        reads=(B_ps[pi], B_const), writes=(B_stt,))
                for n0 in range(0, NTB - 1, 2):
                    pi = 2 + (n0 // 2) % 2
                    nn = min(2, NTB - 1 - n0)
                    for j in range(nn):
                        n = n0 + j
                        k.op("pe", lambda t, n=n, j=j, pi=pi: t.matmul(ps[pi][:, j * 256:(j + 1) * 256], kd[:, n, :], rv[:, n, :],
                                                                       start=True, stop=True),
                             reads=(B_kd, B_rv), writes=(B_ps[pi],), inc=(j == nn - 1))
                    copy_op(evac_eng(), wk[:, n0:n0 + nn, :].rearrange("p a b -> p (a b)"), ps[pi][:, 0:nn * 256],
                            (B_ps[pi],), (B_wk,))
                for n in range(1, NTB - 1):
                    k.op("dve", lambda t, n=n: t.scalar_tensor_tensor(out=wk[:, n, :], in0=wk[:, n - 1, :], scalar=CD[h],
                                                                      in1=wk[:, n, :], op0=ALU.mult, op1=ALU.add),
                         reads=(B_wk,), writes=(B_wk,))
                copy_op("act", rball[:, 0:NTB - 1, :], wk[:, 0:NTB - 1, :], (B_wk,), (B_rb,))
                for n0 in range(0, NTB, 2):
                    pi = 4 + (n0 // 2) % 2
                    for j in range(2):
                        n = n0 + j
                        prs = [(stt[:, n, :], rv[:, n, :])]
                        if n > 0:
                            prs.append((qd[:, n, :], rball[:, n - 1, :]))
                        k.mm(ps[pi][:, j * 256:(j + 1) * 256], B_ps[pi], prs, reads=(B_stt, B_rv, B_qd, B_rb))
                    copy_op("act", obuf[:, n0:n0 + 2, :].rearrange("p a b -> p (a b)"), ps[pi][:, :], (B_ps[pi],), (B_ob,))
                k.op("dve", lambda t: t.tensor_reduce(out=stats[:, 0, :], in_=obuf[:], axis=AX.X, op=ALU.add),
                     reads=(B_ob,), writes=(B_stats,))
                k.op("act", lambda t: t.activation(out=wk[:], in_=obuf[:], func=AF.Square), reads=(B_ob, B_rb), writes=(B_wk,))
                k.op("dve", lambda t: t.tensor_reduce(out=stats[:, 1, :], in_=wk[:], axis=AX.X, op=ALU.add),
                     reads=(B_wk,), writes=(B_stats,))
                k.op("dve", lambda t: t.tensor_scalar(out=stats[:, 2, :], in0=stats[:, 0, :], scalar1=1.0 / 256, scalar2=0.0,
                                                      op0=ALU.mult, op1=ALU.add), reads=(B_stats,), writes=(B_stats,))
                k.op("dve", lambda t: t.tensor_tensor(out=stats[:, 3, :], in0=stats[:, 2, :], in1=stats[:, 2, :], op=ALU.mult),
                     reads=(B_stats,), writes=(B_stats,))
                k.op("dve", lambda t: t.scalar_tensor_tensor(out=stats[:, 3, :], in0=stats[:, 1, :], scalar=1.0 / 256,
                                                             in1=stats[:, 3, :], op0=ALU.mult, op1=ALU.subtract),
                     reads=(B_stats,), writes=(B_stats,))
                k.op("act", lambda t: t.activation(out=stats[:, 4, :], in_=stats[:, 3, :], func=AF.Ln, bias=epsb[:]),
                     reads=(B_stats, B_const), writes=(B_stats,))
                k.op("act", lambda t: t.activation(out=stats[:, 4, :], in_=stats[:, 4, :], func=AF.Exp, scale=-0.5),
                     reads=(B_stats,), writes=(B_stats,))
                k.op("dve", lambda t: t.scalar_tensor_tensor(out=stats[:, 5, :], in0=stats[:, 2, :], scalar=-1.0,
                                                             in1=stats[:, 4, :], op0=ALU.mult, op1=ALU.mult),
                     reads=(B_stats,), writes=(B_stats,))
                for n in range(NTB):
                    if n % 2:
                        k.op("act", lambda t, n=n: t.activation(out=wk[:, n, :], in_=obuf[:, n, :], func=AF.Identity,
                                                                scale=stats[:, 4, n:n + 1], bias=stats[:, 5, n:n + 1]),
                             reads=(B_ob, B_stats), writes=(B_wk,))
                    else:
                        k.op("dve", lambda t, n=n: t.tensor_scalar(out=wk[:, n, :], in0=obuf[:, n, :],
                                                                   scalar1=stats[:, 4, n:n + 1], scalar2=stats[:, 5, n:n + 1],
                                                                   op0=ALU.mult, op1=ALU.add),
                             reads=(B_ob, B_stats), writes=(B_wk,))
                k.op("dve", lambda t: t.tensor_tensor(out=yb[:], in0=wk[:], in1=rg[:], op=ALU.mult),
                     reads=(B_wk, B_rg), writes=(B_yb,))
                for ec in range(2):
                    for n0 in range(0, NTB, 8):
                        nn = min(8, NTB - n0)
                        for j in range(nn):
                            k.op("pe", lambda t, j=j, n0=n0: t.transpose(out=pT[:, j * 128:(j + 1) * 128],
                                                                       in_=yb[:, n0 + j, ec * 128:(ec + 1) * 128],
                                                                       identity=identb[:]),
                                 reads=(B_yb, B_const), writes=(B_pT,), inc=(j == nn - 1))
                        copy_op(evac_eng(), ogt[:, ec, n0 * 128:(n0 + nn) * 128], pT[:, 0:nn * 128], (B_pT,), (B_ogt,))
                for ec in range(2):
                    k.dma("pool", s_retgt[h * 2 + ec], ogt[:, ec, :], reads=(B_ogt,), writes=(B_scr["retgt"][h * 2 + ec],))
            k.barrier()

    def post_phase(l):
        with ExitStack() as p:
            def al(name, shape, dt):
                UID[0] += 1
                return p.enter_context(nc.sbuf_tensor("%s_%d" % (name, UID[0]), list(shape), dt))
            wsbo = al("wsbo", [128, 4, D], BF16)
            wreto = al("wreto", [128, 8, D], BF16)
            wout = al("wout", [128, 8, D], BF16)
            B_wp = Buf()
            k.dma("sp", wsbo[:], wb_sb_o[l].rearrange("(c p) n -> p c n", p=128), reads=(B_w["in"],), writes=(B_wp,))
            k.dma("sp", wreto[:], wb_ret_o[l].rearrange("(c p) n -> p c n", p=128), reads=(B_w["in"],), writes=(B_wp,))
            k.dma("sp", wout[:], wb_out[l].rearrange("(c p) n -> p c n", p=128), reads=(B_w["in"],), writes=(B_wp,))
            sbt = al("sbt", [128, 4, 512], BF16)
            rgt = al("rgt", [128, 8, 512], BF16)
            grt = al("grt", [128, 8, 512], BF16)
            gst = al("gst", [128, 8, 512], BF16)
            B_ld = Buf()
            t1 = [al("pt1%d" % i, [128, 512], F32) for i in range(2)]
            t2 = [al("pt2%d" % i, [128, 512], F32) for i in range(2)]
            B_t1 = [Buf(), Buf()]
            B_t2 = [Buf(), Buf()]
            mt = al("mt", [128, 8, 512], BF16)
            B_mt = Buf()
            pc = [0]
            for tg in range(NTG):
                cols = slice(tg * 512, (tg + 1) * 512)
                k.dma("sp", sbt[:], s_sbt[:, :, cols].rearrange("c p n -> p c n"),
                      reads=B_scr["sbt"][0:4], writes=(B_ld,))
                k.dma("sp", rgt[:], s_retgt[:, :, cols].rearrange("c p n -> p c n"),
                      reads=B_scr["retgt"][0:8], writes=(B_ld,))
                k.dma("sp", grt[:], s_grt[:, :, cols].rearrange("c p n -> p c n"),
                      reads=B_scr["grt"][0:8], writes=(B_ld,))
                k.dma("sp", gst[:], s_gst[:, :, cols].rearrange("c p n -> p c n"),
                      reads=B_scr["gst"][0:8], writes=(B_ld,))
                for ec in range(8):
                    i2 = ec % 2
                    p1, p2 = i2 * 2, i2 * 2 + 1
                    es = slice(ec * 128, (ec + 1) * 128)
                    k.mm(ps[p1][:], B_ps[p1], [(wsbo[:, c, es], sbt[:, c, :]) for c in range(4)], reads=(B_wp, B_ld))
                    k.mm(ps[p2][:], B_ps[p2], [(wreto[:, c, es], rgt[:, c, :]) for c in range(8)], reads=(B_wp, B_ld))
                    k.op("dve", lambda t, p1=p1, i2=i2, ec=ec: t.tensor_tensor(out=t1[i2][:], in0=ps[p1][:], in1=gst[:, ec, :],
                                                                               op=ALU.mult),
                         reads=(B_ps[p1], B_ld), writes=(B_t1[i2],))
                    k.op("dve", lambda t, p2=p2, i2=i2, ec=ec: t.tensor_tensor(out=t2[i2][:], in0=ps[p2][:], in1=grt[:, ec, :],
                                                                               op=ALU.mult),
                         reads=(B_ps[p2], B_ld), writes=(B_t2[i2],))
                    k.op("pool", lambda t, i2=i2, ec=ec: t.tensor_tensor(out=mt[:, ec, :], in0=t1[i2][:], in1=t2[i2][:],
                                                                         op=ALU.add),
                         reads=(B_t1[i2], B_t2[i2]), writes=(B_mt,))
                for dc in range(8):
                    pi = 4 + dc % 2
                    k.mm(ps[pi][:], B_ps[pi], [(wout[:, c, dc * 128:(dc + 1) * 128], mt[:, c, :]) for c in range(8)],
                         reads=(B_wp, B_mt))
                    k.op("dve", lambda t, pi=pi, dc=dc: t.tensor_tensor(out=hT[:, dc, cols], in0=ps[pi][:], in1=hT[:, dc, cols],
                                                                        op=ALU.add),
                         reads=(B_ps[pi], B_h[dc][tg]), writes=(B_h[dc][tg],))
            k.barrier()

    def ffn_phase(l):
        with ExitStack() as p:
            def al(name, shape, dt):
                UID[0] += 1
                return p.enter_context(nc.sbuf_tensor("%s_%d" % (name, UID[0]), list(shape), dt))
            hn = al("hn2", [128, 8, 512], BF16)
            B_hn = Buf()
            sq = al("sq2", [128, 8, 512], BF16)
            rstd = al("rstd2", [128, 512], F32)
            B_sq, B_rstd = Buf(), Buf()
            wgu = [al("wgu%d" % i, [128, 8, 512], BF16) for i in range(2)]
            B_wgu = [Buf(), Buf()]
            wd = [al("wd%d" % i, [128, NFC, 256], BF16) for i in range(2)]
            B_wd = [Buf(), Buf()]
            actT = al("actT", [128, NFC, 512], BF16)
            B_act = Buf()
            sg = [al("sg%d" % i, [128, 512], F32) for i in range(2)]
            B_sg = [Buf(), Buf()]
            wc = [0, 0]
            for tg in range(NTG):
                cols = slice(tg * 512, (tg + 1) * 512)
                rmsnorm_tg(tg, DEPTH + l, sq, B_sq, rstd, B_rstd, lambda c: (hn[:, c, :], (B_hn,)))

                def load_gu(fg):
                    i = wc[0] % 2
                    wc[0] += 1
                    k.dma("sp", wgu[i][:, :, 0:256],
                          wb_gu[l][:, fg * 256:(fg + 1) * 256].rearrange("(c p) n -> p c n", p=128),
                          reads=(B_w["in"],), writes=(B_wgu[i],))
                    k.dma("sp", wgu[i][:, :, 256:512],
                          wb_gu[l][:, FF + fg * 256:FF + (fg + 1) * 256].rearrange("(c p) n -> p c n", p=128),
                          reads=(B_w["in"],), writes=(B_wgu[i],))
                    return i
                nfg = NFC // 2
                cur = load_gu(0)
                for fg in range(nfg):
                    nxt = load_gu(fg + 1) if fg + 1 < nfg else None
                    w, Bw = wgu[cur], B_wgu[cur]
                    for j in range(2):
                        fcn = fg * 2 + j
                        i2 = fcn % 2
                        pg, pu = 1 + i2 * 2, 2 + i2 * 2
                        k.mm(ps[pg][:], B_ps[pg], [(w[:, c, j * 128:(j + 1) * 128], hn[:, c, :]) for c in range(8)],
                             reads=(Bw, B_hn))
                        k.mm(ps[pu][:], B_ps[pu], [(w[:, c, 256 + j * 128:256 + (j + 1) * 128], hn[:, c, :]) for c in range(8)],
                             reads=(Bw, B_hn))
                        k.op("act", lambda t, i2=i2, pg=pg: t.activation(out=sg[i2][:], in_=ps[pg][:], func=AF.Silu),
                             reads=(B_ps[pg],), writes=(B_sg[i2],))
                        k.op("dve", lambda t, i2=i2, pu=pu, fcn=fcn: t.tensor_tensor(out=actT[:, fcn, :], in0=ps[pu][:],
                                                                                     in1=sg[i2][:], op=ALU.mult),
                             reads=(B_ps[pu], B_sg[i2]), writes=(B_act,))
                    cur = nxt

                def load_wd(dg):
                    i = wc[1] % 2
                    wc[1] += 1
                    k.dma("sp", wd[i][:], wb_dn[l][:, dg * 256:(dg + 1) * 256].rearrange("(c p) n -> p c n", p=128),
                          reads=(B_w["in"],), writes=(B_wd[i],))
                    return i
                cur = load_wd(0)
                for dg in range(4):
                    nxt = load_wd(dg + 1) if dg + 1 < 4 else None
                    for j in range(2):
                        dc = dg * 2 + j
                        pi = 5 + dc % 2
                        k.mm(ps[pi][:], B_ps[pi], [(wd[cur][:, f, j * 128:(j + 1) * 128], actT[:, f, :]) for f in range(NFC)],
                             reads=(B_wd[cur], B_act))
                        k.op("dve", lambda t, pi=pi, dc=dc: t.tensor_tensor(out=hT[:, dc, cols], in0=ps[pi][:],
                                                                            in1=hT[:, dc, cols], op=ALU.add),
                             reads=(B_ps[pi], B_h[dc][tg]), writes=(B_h[dc][tg],))
                    cur = nxt
            k.barrier()

    for s in range(NSEQ):
        for c in range(8):
            k.dma("sp", hT[:, c, :], xT[s, c * 128:(c + 1) * 128, :], writes=B_h[c])
        for l in range(DEPTH):
            k.new_epoch()
            layer(l)
        if debug in ("p1", "p2", "p3") or (isinstance(debug, str) and debug.startswith("g")):
            break
        with ExitStack() as p:
            sq = p.enter_context(nc.sbuf_tensor("sqf_%d" % s, [128, 8, 512], BF16))
            rstd = p.enter_context(nc.sbuf_tensor("rstdf_%d" % s, [128, 512], F32))
            of = [p.enter_context(nc.sbuf_tensor("of%d_%d" % (i, s), [128, 8, 512], F32)) for i in range(2)]
            B_sq, B_rstd = Buf(), Buf()
            B_of = [Buf(), Buf()]
            for tg in range(NTG):
                o, Bo = of[tg % 2], B_of[tg % 2]
                rmsnorm_tg(tg, 2 * DEPTH, sq, B_sq, rstd, B_rstd, lambda c, o=o, Bo=Bo: (o[:, c, :], (Bo,)))
                k.dma("pool", outT[s][:, tg * 512:(tg + 1) * 512].rearrange("(c p) n -> p c n", p=128), o[:],
                      reads=(Bo,))
            k.barrier()
    k.barrier()
    return nc, ctx, consts


_CACHE = {}


def _prep_inputs(inputs, S, DEPTH):
    consts, _ = host_consts(S)
    gl = [inputs["norm_mix"][l] for l in range(DEPTH)] + [inputs["norm_ffn"][l] for l in range(DEPTH)] + \
         [inputs["norm_final"]]
    gvec = np.stack([np.asarray(g, np.float32).reshape(8, 128).T for g in gl], 1)
    shared = {
        "w_in": np.ascontiguousarray(inputs["w_in"], np.float32),
        "w_ret_o": np.ascontiguousarray(inputs["w_ret_o"], np.float32),
        "w_sb_o": np.ascontiguousarray(inputs["w_sb_o"], np.float32),
        "w_out": np.ascontiguousarray(inputs["w_out"], np.float32),
        "w_gate_up": np.ascontiguousarray(inputs["w_gate_up"], np.float32),
        "w_down": np.ascontiguousarray(inputs["w_down"], np.float32),
        "gvec": np.ascontiguousarray(gvec.reshape(128, -1), np.float32),
    }
    for kk, v in consts.items():
        shared["c_" + kk] = np.ascontiguousarray(v, np.float32)
    return shared


def kernel(x, norm_mix, w_in, w_ret_o, w_sb_o, w_out, norm_ffn, w_gate_up, w_down, norm_final):
    x = np.asarray(x, np.float32)
    B, S, _ = x.shape
    DEPTH = np.asarray(w_in).shape[0]
    ncores = 8
    NSEQ = B // ncores
    inputs = dict(norm_mix=np.asarray(norm_mix), w_in=np.asarray(w_in), w_ret_o=np.asarray(w_ret_o),
                  w_sb_o=np.asarray(w_sb_o), w_out=np.asarray(w_out), norm_ffn=np.asarray(norm_ffn),
                  w_gate_up=np.asarray(w_gate_up), w_down=np.asarray(w_down), norm_final=np.asarray(norm_final))
    shared = _prep_inputs(inputs, S, DEPTH)
    nc, ctx, _ = build_program(NSEQ, S, DEPTH)
    in_maps = []
    for c in range(ncores):
        m = dict(shared)
        m["xT"] = np.ascontiguousarray(x[c * NSEQ:(c + 1) * NSEQ].transpose(0, 2, 1))
        in_maps.append(m)
    res = run_bass_kernel_spmd(nc, in_maps, core_ids=list(range(ncores)))
    out = np.empty((B, S, D), np.float32)
    for c in range(ncores):
        out[c * NSEQ:(c + 1) * NSEQ] = res.results[c]["outT"].transpose(0, 2, 1)
    return out
```

```python
import numpy as np
from contextlib import ExitStack
import concourse.bass as bass
import concourse.mybir as mybir
from concourse.bass_utils import run_bass_kernel_spmd

F32, BF16 = mybir.dt.float32, mybir.dt.bfloat16
AF = mybir.ActivationFunctionType
ALU = mybir.AluOpType
AX = mybir.AxisListType

D = 1024
NC8 = 8
RH, RDK, RDV = 4, 128, 256
SBH, SBD = 8, 64
FF = 2816
NFC = FF // 128
INC = 6656
EPS = 1e-6
O_RQ, O_RK, O_RV, O_RG, O_SQ, O_SK, O_SV, O_GR, O_GS = 0, 512, 1024, 2048, 3072, 3584, 4096, 4608, 5632


class Buf:
    __slots__ = ("w", "r", "excl")

    def __init__(self, excl=False):
        self.w = {}
        self.r = {}
        self.excl = excl


class KB:
    def __init__(self, nc, ctx):
        self.nc, self.ctx = nc, ctx
        self.engs = {"pe": nc.tensor, "act": nc.scalar, "dve": nc.vector, "pool": nc.gpsimd, "sp": nc.sync}
        self.sems = []
        self.esem = {}
        self.seen = {e: {} for e in self.engs}
        for e in self.engs:
            self.new_eng_sem(e)
        self.dpool = {}
        for q in ("sp", "pool"):
            self.dpool[q] = [[self._new_sem("d%s%d" % (q, i)), 0] for i in range(12)]
        self.dnext = {"sp": 0, "pool": 0}
        self.dsids = set(sl[0] for q in self.dpool for sl in self.dpool[q])

    def _new_sem(self, name):
        h = self.ctx.enter_context(self.nc.semaphore(name))
        self.sems.append(h)
        return len(self.sems) - 1

    def new_eng_sem(self, e):
        self.esem[e] = [self._new_sem("e%s%d" % (e, len(self.sems))), 0]

    def new_epoch(self):
        for e in ("pe", "act", "dve", "pool"):
            if self.esem[e][1] > 12000:
                self.new_eng_sem(e)

    def _waits(self, e, reads, writes):
        need = {}
        mysid0 = self.esem[e][0]
        for b in reads:
            for sid, v in b.w.items():
                need[sid] = max(need.get(sid, 0), v)
            if b.excl:
                for sid, v in b.r.items():
                    if sid != mysid0:
                        need[sid] = max(need.get(sid, 0), v)
        for b in writes:
            for sid, v in b.w.items():
                need[sid] = max(need.get(sid, 0), v)
            for sid, v in b.r.items():
                need[sid] = max(need.get(sid, 0), v)
        mysid = self.esem[e][0]
        for sid, v in need.items():
            if sid == mysid and e == "pe":
                continue
            if self.seen[e].get(sid, 0) >= v:
                continue
            self.engs[e].wait_ge(self.sems[sid], v)
            self.seen[e][sid] = v

    def op(self, e, fn, reads=(), writes=(), inc=True):
        self._waits(e, reads, writes)
        ins = fn(self.engs[e])
        mysid = self.esem[e][0]
        tgt = self.esem[e][1] + 1
        if inc:
            ins.then_inc(self.sems[mysid], 1)
            self.esem[e][1] = tgt
        for b in reads:
            b.r[mysid] = max(b.r.get(mysid, 0), tgt)
        for b in writes:
            b.w = {mysid: tgt}
            b.r = {}
        return ins

    def dma(self, q, out, in_, reads=(), writes=()):
        self._waits(q, reads, writes)
        pool = self.dpool[q]
        slot = pool[self.dnext[q] % len(pool)]
        self.dnext[q] += 1
        sid = slot[0]
        if slot[1] > 0 and self.seen[q].get(sid, 0) < slot[1] * 16:
            self.engs[q].wait_ge(self.sems[sid], slot[1] * 16)
            self.seen[q][sid] = slot[1] * 16
        ins = self.engs[q].dma_start(out=out, in_=in_)
        slot[1] += 1
        v = slot[1] * 16
        ins.then_inc(self.sems[sid], 16)
        for b in reads:
            b.r[sid] = max(b.r.get(sid, 0), v)
        for b in writes:
            b.w = {ks: kv for ks, kv in b.w.items() if ks in self.dsids}
            b.w[sid] = v
            b.r = {}

    def barrier(self):
        tg = {}
        for e in ("pe", "act", "dve", "pool"):
            sid, c = self.esem[e]
            if c > 0:
                tg[sid] = c
        for q in ("sp", "pool"):
            for sid, c in self.dpool[q]:
                if c > 0:
                    tg[sid] = c * 16
        for e in self.engs:
            for sid, v in tg.items():
                if self.seen[e].get(sid, 0) >= v:
                    continue
                self.engs[e].wait_ge(self.sems[sid], v)
                self.seen[e][sid] = v

    def mm(self, ps, psb, pairs, reads, first=True, last=True):
        n = len(pairs)
        for i, (l, r) in enumerate(pairs):
            st = first and i == 0
            sp = last and i == n - 1
            self.op("pe", lambda t, l=l, r=r, st=st, sp=sp: t.matmul(ps, l, r, start=st, stop=sp),
                    reads=reads if i == 0 else (), writes=(psb,), inc=(i == n - 1))


def host_consts(S):
    c = {}
    ar = np.arange(128)
    c["ident"] = np.eye(128, dtype=np.float32)
    c["negmask"] = np.where(ar[:, None] >= ar[None, :], -30000.0, 0.0).astype(np.float32)
    c["negu"] = np.where(ar[:, None] >= ar[None, :], -1.0, 0.0).astype(np.float32)
    prot = np.zeros((128, 128), np.float32)
    for m in range(64):
        prot[m + 64, m] = 1.0
        prot[m, m + 64] = 1.0
    c["prot"] = prot
    c["onesm"] = np.full((128, 128), 1.0 / 1024, np.float32)
    sel = np.zeros((128, 16, 48), np.float32)
    for kb in range(16):
        sel[:, kb, kb] = 1.0
        sel[:, kb, 32 + kb] = 1.0
    c["sel"] = sel.reshape(128, 16 * 48)
    ns = np.zeros((128, 16, 128), np.float32)
    for kb in range(16):
        for r in range(16):
            if r > kb:
                ns[r, kb, :] = -1.0
                ns[32 + r, kb, :] = -1.0
    c["negstep"] = ns.reshape(128, 16 * 128)
    half = 64
    inv = (1.0 / (np.float32(10000.0) ** (np.arange(half, dtype=np.float32) / np.float32(half)))).astype(np.float32)
    pos = np.arange(S, dtype=np.float32)
    ang = (pos[:, None] * inv[None, :]).astype(np.float32)
    cos = np.cos(ang).astype(np.float32).T
    sin = np.sin(ang).astype(np.float32).T
    cosf = np.concatenate([cos, cos], 0)
    sinf = np.concatenate([-sin, sin], 0)
    ksc = np.float32(RDK ** -0.5)
    c["rope"] = np.stack([cosf, sinf, cosf * ksc, sinf * ksc], 1).astype(np.float32).reshape(128, 4 * S)
    lg = np.log1p(-np.exp2(-5.0 - np.arange(RH, dtype=np.float32))).astype(np.float32)
    i = np.arange(128, dtype=np.float32)
    diff = i[None, :] - i[:, None]
    dect = np.where(diff[None] >= 0, np.exp(lg[:, None, None] * np.maximum(diff, 0.0)[None]), 0.0)
    c["dect"] = np.ascontiguousarray(dect.transpose(1, 0, 2)).astype(np.float32).reshape(128, RH * 128)
    c["kdec"] = np.exp(lg[None, :] * (127.0 - i)[:, None]).astype(np.float32)
    qdec = np.exp(lg[:, None] * (i + 1.0)[None, :]).astype(np.float32)
    c["qdec"] = np.broadcast_to(qdec[None], (128, RH, 128)).astype(np.float32).reshape(128, RH * 128).copy()
    cd = np.exp(lg * 128.0).astype(np.float32)
    return c, [float(x) for x in cd]


def build_program(NSEQ, S, DEPTH, debug=False):
    assert S % 512 == 0
    NTG = S // 512
    NTB = S // 128
    consts, CD = host_consts(S)
    nc = bass.Bass("TRN2", target_bir_lowering=False)
    ctx = ExitStack()

    def din(name, shape, dt=F32):
        return nc.dram_tensor(name, list(shape), dt, kind="ExternalInput").ap()

    def dscr(name, shape, dt=BF16):
        return nc.dram_tensor(name, list(shape), dt, kind=("ExternalOutput" if debug else "Internal")).ap()

    xT = din("xT", [NSEQ, D, S])
    outT = nc.dram_tensor("outT", [NSEQ, D, S], F32, kind="ExternalOutput").ap()
    w_in = din("w_in", [DEPTH, D, INC])
    w_ret_o = din("w_ret_o", [DEPTH, D, D])
    w_sb_o = din("w_sb_o", [DEPTH, 512, D])
    w_out = din("w_out", [DEPTH, D, D])
    w_gu = din("w_gate_up", [DEPTH, D, 2 * FF])
    w_dn = din("w_down", [DEPTH, FF, D])
    gvec = din("gvec", [128, (2 * DEPTH + 1) * 8])
    cin = {k: din("c_" + k, v.shape) for k, v in consts.items()}
    wb_in = dscr("wb_in", [DEPTH, D, INC])
    wb_ret_o = dscr("wb_ret_o", [DEPTH, D, D])
    wb_sb_o = dscr("wb_sb_o", [DEPTH, 512, D])
    wb_out = dscr("wb_out", [DEPTH, D, D])
    wb_gu = dscr("wb_gu", [DEPTH, D, 2 * FF])
    wb_dn = dscr("wb_dn", [DEPTH, FF, D])
    s_rqt = dscr("s_rqt", [4, 128, S])
    s_rkt = dscr("s_rkt", [4, 128, S])
    s_rv = dscr("s_rv", [S, 1024])
    s_rg = dscr("s_rg", [S, 1024])
    s_sqt = dscr("s_sqt", [4, 128, S])
    s_skt = dscr("s_skt", [4, 128, S])
    s_sv = dscr("s_sv", [S, 512])
    s_grt = dscr("s_grt", [8, 128, S])
    s_gst = dscr("s_gst", [8, 128, S])
    s_sbt = dscr("s_sbt", [4, 128, S])
    s_retgt = dscr("s_retgt", [8, 128, S])
    B_scr = {n: [Buf() for _ in range(16)] for n in
             ("rqt", "rkt", "rv", "rg", "sqt", "skt", "sv", "grt", "gst", "sbt", "retgt")}
    B_w = {n: Buf() for n in ("in", "ret_o", "sb_o", "out", "gu", "dn")}

    def sb(name, shape, dt):
        return ctx.enter_context(nc.sbuf_tensor(name, list(shape), dt))

    k = KB(nc, ctx)
    UID = [0]
    hT = sb("hT", [128, 8, S], F32)
    B_h = [[Buf() for _ in range(NTG)] for _ in range(8)]
    identb = sb("identb", [128, 128], BF16)
    negmaskb = sb("negmaskb", [128, 128], BF16)
    negub = sb("negub", [128, 128], BF16)
    protb = sb("protb", [128, 128], BF16)
    onesmb = sb("onesmb", [128, 128], BF16)
    selb = sb("selb", [128, 16, 48], BF16)
    negstepb = sb("negstepb", [128, 16, 128], BF16)
    dect = sb("dect", [128, RH, 128], F32)
    kdec = sb("kdec", [128, RH], F32)
    qdec = sb("qdec", [128, RH, 128], F32)
    gv = sb("gv", [128, 2 * DEPTH + 1, 8], F32)
    epsb = sb("epsb", [128, 1], F32)
    B_const = Buf()
    ps = [ctx.enter_context(nc.psum_tensor("ps%d" % i, [128, 512], F32)) for i in range(7)]
    pT = ctx.enter_context(nc.psum_tensor("pT", [128, 1024], BF16))
    B_ps = [Buf(excl=True) for _ in range(7)]
    B_pT = Buf(excl=True)
    rr = {"evac": 0}

    def evac_eng():
        rr["evac"] += 1
        return "act" if rr["evac"] % 2 else "dve"

    def copy_op(e, out, in_, reads, writes):
        if e == "act":
            k.op("act", lambda t: t.activation(out=out, in_=in_, func=AF.Copy), reads=reads, writes=writes)
        else:
            k.op(e, lambda t: t.tensor_copy(out=out, in_=in_), reads=reads, writes=writes)

    with ExitStack() as pctx:
        NSTG = 4
        stg = [pctx.enter_context(nc.sbuf_tensor("stg%d" % i, [128, 2048], F32)) for i in range(NSTG)]
        stb = [pctx.enter_context(nc.sbuf_tensor("stb%d" % i, [128, 2048], BF16)) for i in range(NSTG)]
        B_stg = [Buf() for _ in range(NSTG)]
        B_stb = [Buf() for _ in range(NSTG)]
        cnt = [0]

        def conv(dst_sb_ap, src_dram, ncol, npart=128):
            i = cnt[0] % NSTG
            cnt[0] += 1
            k.dma("sp", stg[i][0:npart, 0:ncol], src_dram, writes=(B_stg[i],))
            copy_op("dve", dst_sb_ap, stg[i][0:npart, 0:ncol], (B_stg[i],), (B_const,))

        conv(identb[:], cin["ident"], 128)
        conv(negmaskb[:], cin["negmask"], 128)
        conv(negub[:], cin["negu"], 128)
        conv(protb[:], cin["prot"], 128)
        conv(onesmb[:], cin["onesm"], 128)
        conv(selb[:].rearrange("p a b -> p (a b)"), cin["sel"], 16 * 48)
        conv(negstepb[:].rearrange("p a b -> p (a b)"), cin["negstep"], 2048)
        k.dma("sp", dect[:].rearrange("p a b -> p (a b)"), cin["dect"], writes=(B_const,))
        k.dma("sp", kdec[:], cin["kdec"], writes=(B_const,))
        k.dma("sp", qdec[:].rearrange("p a b -> p (a b)"), cin["qdec"], writes=(B_const,))
        k.dma("sp", gv[:].rearrange("p a b -> p (a b)"), gvec, writes=(B_const,))
        k.op("dve", lambda t: t.memset(epsb[:], EPS), writes=(B_const,))

        def conv_w(dst, src, R, C):
            for r0 in range(0, R, 128):
                for c0 in range(0, C, 2048):
                    cw = min(2048, C - c0)
                    i = cnt[0] % NSTG
                    e = ("dve", "act", "dve", "act", "pool")[cnt[0] % 5]
                    cnt[0] += 1
                    k.dma("sp", stg[i][:, 0:cw], src[r0:r0 + 128, c0:c0 + cw], writes=(B_stg[i],))
                    copy_op(e, stb[i][:, 0:cw], stg[i][:, 0:cw], (B_stg[i],), (B_stb[i],))
                    k.dma("pool", dst[r0:r0 + 128, c0:c0 + cw], stb[i][:, 0:cw], reads=(B_stb[i],),
                          writes=(B_w["in"],))

        for l in range(DEPTH):
            conv_w(wb_in[l], w_in[l], D, INC)
            conv_w(wb_ret_o[l], w_ret_o[l], D, D)
            conv_w(wb_sb_o[l], w_sb_o[l], 512, D)
            conv_w(wb_out[l], w_out[l], D, D)
            conv_w(wb_gu[l], w_gu[l], D, 2 * FF)
            conv_w(wb_dn[l], w_dn[l], FF, D)
        k.barrier()

    if debug == "p0":
        return nc, ctx, consts
    def rmsnorm_tg(tg, gidx, sq, B_sq, rstd, B_rstd, out_fn):
        cols = slice(tg * 512, (tg + 1) * 512)
        k.op("act", lambda t: t.activation(out=sq[:], in_=hT[:, :, cols], func=AF.Square),
             reads=[B_h[c][tg] for c in range(8)], writes=(B_sq,))
        k.mm(ps[0][:], B_ps[0], [(onesmb[:], sq[:, c, :]) for c in range(8)], reads=(B_sq, B_const))
        k.op("act", lambda t: t.activation(out=rstd[:], in_=ps[0][:], func=AF.Ln, bias=epsb[:]),
             reads=(B_ps[0], B_const), writes=(B_rstd,))
        k.op("act", lambda t: t.activation(out=rstd[:], in_=rstd[:], func=AF.Exp, scale=-0.5),
             reads=(B_rstd,), writes=(B_rstd,))
        for c in range(8):
            o, ob = out_fn(c)
            k.op("dve", lambda t, o=o, c=c: t.scalar_tensor_tensor(
                out=o, in0=hT[:, c, cols], scalar=gv[:, gidx, c:c + 1], in1=rstd[:], op0=ALU.mult, op1=ALU.mult),
                reads=(B_h[c][tg], B_rstd, B_const), writes=ob)

    def layer(l):
        with ExitStack() as p:
            def al(name, shape, dt):
                UID[0] += 1
                return p.enter_context(nc.sbuf_tensor("%s_%d" % (name, UID[0]), list(shape), dt))
            hnT = al("hnT", [128, 8, S], BF16)
            B_hn = [Buf() for _ in range(NTG)]
            sq = al("sq", [128, 8, 512], BF16)
            rstd = al("rstd", [128, 512], F32)
            B_sq, B_rstd = Buf(), Buf()
            def norm1(tg):
                rmsnorm_tg(tg, l, sq, B_sq, rstd, B_rstd,
                           lambda c, tg=tg: (hnT[:, c, tg * 512:(tg + 1) * 512], (B_hn[tg],)))
            norm1(0)
            wt = [al("wt%d" % i, [128, 8, 512], BF16) for i in range(2)]
            B_wt = [Buf(), Buf()]
            ost = [al("ost%d" % i, [128, max(S, 2048)], BF16) for i in range(2)]
            B_ost = [Buf(), Buf()]
            rot = al("rot", [128, 4, 512], F32)
            B_rot = Buf()
            xb = al("xb", [128, 512], BF16)
            B_xb = Buf()
            t1 = al("t1", [128, 512], F32)
            t2 = al("t2", [128, 512], F32)
            B_t1, B_t2 = Buf(), Buf()
            st = {"ps": 1, "ost": 0}

            def next_ps():
                st["ps"] = 1 + (st["ps"] % 5)
                return st["ps"]

            def load_w(gi):
                i = gi % 2
                k.dma("sp", wt[i][:], wb_in[l][:, gi * 512:(gi + 1) * 512].rearrange("(c p) n -> p c n", p=128),
                      reads=(B_w["in"],), writes=(B_wt[i],))

            NG = INC // 512
            load_w(0)
            rope_v = cin["rope"].rearrange("p (a s) -> p a s", a=4)
            for gi in range(NG):
                if isinstance(debug, str) and debug.startswith("g") and gi >= int(debug[1:2]):
                    break
                stage = debug[2:] if isinstance(debug, str) and debug.startswith("g") else ""

                if gi + 1 < NG:
                    load_w(gi + 1)
                w = wt[gi % 2]
                Bw = B_wt[gi % 2]
                col0 = gi * 512
                if col0 in (O_RQ, O_RK):
                    isk = col0 == O_RK
                    dst = s_rkt if isk else s_rqt
                    Bd = B_scr["rkt" if isk else "rqt"]
                    for tg in range(NTG):
                        if gi == 0 and tg + 1 < NTG:
                            norm1(tg + 1)
                        k.dma("sp", rot[:], rope_v[:, :, tg * 512:(tg + 1) * 512], writes=(B_rot,))
                        for h in range(4):
                            pi = next_ps()
                            k.mm(ps[pi][:], B_ps[pi],
                                 [(w[:, c, h * 128:(h + 1) * 128], hnT[:, c, tg * 512:(tg + 1) * 512]) for c in range(8)],
                                 reads=(Bw, B_hn[tg]))
                            if stage == "a":
                                continue
                            copy_op("act", xb[:], ps[pi][:], (B_ps[pi],), (B_xb,))
                            if stage == "b":
                                continue
                            k.mm(ps[6][:], B_ps[6], [(protb[:], xb[:])], reads=(B_xb, B_const))
                            if stage == "c":
                                continue
                            tb = 2 if isk else 0
                            k.op("dve", lambda t, pi=pi, tb=tb: t.tensor_tensor(
                                out=t1[:], in0=ps[pi][:], in1=rot[:, tb, :], op=ALU.mult),
                                reads=(B_ps[pi], B_rot), writes=(B_t1,))
                            k.op("dve", lambda t, tb=tb: t.tensor_tensor(
                                out=t2[:], in0=ps[6][:], in1=rot[:, tb + 1, :], op=ALU.mult),
                                reads=(B_ps[6], B_rot), writes=(B_t2,))
                            if stage == "d":
                                continue
                            o = ost[h % 2]
                            k.op("dve", lambda t, o=o, tg=tg: t.tensor_tensor(
                                out=o[:, tg * 512:(tg + 1) * 512], in0=t1[:], in1=t2[:], op=ALU.add),
                                reads=(B_t1, B_t2), writes=(B_ost[h % 2],))
                            if stage == "e":
                                continue
                            k.dma("pool", dst[h][:, tg * 512:(tg + 1) * 512], o[:, tg * 512:(tg + 1) * 512],
                                  reads=(B_ost[h % 2],), writes=(Bd[h],))
                elif col0 in (O_SQ, O_SK, O_GR, O_GR + 512, O_GS, O_GS + 512):
                    if col0 == O_SQ:
                        dst, Bd, base, fn = s_sqt, B_scr["sqt"], 0, AF.Identity
                    elif col0 == O_SK:
                        dst, Bd, base, fn = s_skt, B_scr["skt"], 0, AF.Copy
                    elif col0 >= O_GS:
                        dst, Bd, base, fn = s_gst, B_scr["gst"], (col0 - O_GS) // 128, AF.Sigmoid
                    else:
                        dst, Bd, base, fn = s_grt, B_scr["grt"], (col0 - O_GR) // 128, AF.Sigmoid
                    for fc in range(4):
                        oi = st["ost"] % 2
                        st["ost"] += 1
                        o = ost[oi]
                        for tg in range(NTG):
                            pi = next_ps()
                            k.mm(ps[pi][:], B_ps[pi],
                                 [(w[:, c, fc * 128:(fc + 1) * 128], hnT[:, c, tg * 512:(tg + 1) * 512]) for c in range(8)],
                                 reads=(Bw, B_hn[tg]))
                            if fn == AF.Copy:
                                copy_op(evac_eng(), o[:, tg * 512:(tg + 1) * 512], ps[pi][:], (B_ps[pi],), (B_ost[oi],))
                            elif fn == AF.Identity:
                                k.op("dve", lambda t, o=o, tg=tg, pi=pi: t.tensor_scalar(
                                    out=o[:, tg * 512:(tg + 1) * 512], in0=ps[pi][:], scalar1=0.125, scalar2=0.0,
                                    op0=ALU.mult, op1=ALU.add),
                                    reads=(B_ps[pi],), writes=(B_ost[oi],))
                            else:
                                k.op("act", lambda t, o=o, tg=tg, pi=pi: t.activation(
                                    out=o[:, tg * 512:(tg + 1) * 512], in_=ps[pi][:], func=AF.Sigmoid),
                                    reads=(B_ps[pi],), writes=(B_ost[oi],))
                        k.dma("pool", dst[base + fc], o[:, 0:S], reads=(B_ost[oi],), writes=(Bd[base + fc],))
                else:
                    if col0 >= O_SV:
                        dst, Bd, cb, fn = s_sv, B_scr["sv"], 0, AF.Copy
                    elif col0 >= O_RG:
                        dst, Bd, cb, fn = s_rg, B_scr["rg"], col0 - O_RG, AF.Silu
                    else:
                        dst, Bd, cb, fn = s_rv, B_scr["rv"], col0 - O_RV, AF.Copy
                    for tq in range(NTB // 4):
                        oi = st["ost"] % 2
                        st["ost"] += 1
                        o = ost[oi]
                        for tb4 in range(4):
                            tb = tq * 4 + tb4
                            pi = next_ps()
                            k.mm(ps[pi][:], B_ps[pi],
                                 [(hnT[:, c, tb * 128:(tb + 1) * 128], w[:, c, :]) for c in range(8)],
                                 reads=(Bw, B_hn[tb // 4]))
                            if fn == AF.Copy:
                                copy_op(evac_eng(), o[:, tb4 * 512:(tb4 + 1) * 512], ps[pi][:], (B_ps[pi],), (B_ost[oi],))
                            else:
                                k.op("act", lambda t, o=o, tb4=tb4, pi=pi: t.activation(
                                    out=o[:, tb4 * 512:(tb4 + 1) * 512], in_=ps[pi][:], func=AF.Silu),
                                    reads=(B_ps[pi],), writes=(B_ost[oi],))
                        k.dma("pool",
                              dst[tq * 512:(tq + 1) * 512, cb:cb + 512].rearrange("(t p) n -> p t n", p=128),
                              o[:, 0:2048].rearrange("p (t n) -> p t n", n=512),
                              reads=(B_ost[oi],), writes=(Bd[tq],))
            k.barrier()
        if debug == "p1" or (isinstance(debug, str) and debug.startswith("g")):
            return
        sb_phase(l)
        if debug == "p2":
            return
        ret_phase(l)
        if debug == "p3":
            return
        post_phase(l)
        if debug == "p4":
            return
        ffn_phase(l)

    def sb_phase(l):
        with ExitStack() as p:
            def al(name, shape, dt):
                UID[0] += 1
                return p.enter_context(nc.sbuf_tensor("%s_%d" % (name, UID[0]), list(shape), dt))
            qa = al("qa", [128, S], BF16)
            qb = al("qb", [128, S], BF16)
            kt = al("kt", [128, S], BF16)
            sv = al("sv", [128, NTB, 128], BF16)
            B_q, B_k, B_v = Buf(), Buf(), Buf()
            spb = [al("spb%d" % i, [128, NTB, 512], BF16) for i in range(2)]
            B_sp = [Buf(), Buf()]
            et = [al("et%d" % i, [128, 512], F32) for i in range(2)]
            B_et = [Buf(), Buf()]
            at = [al("at%d" % i, [128, 512], BF16) for i in range(4)]
            B_at = [Buf() for _ in range(4)]
            hl = [al("hl%d" % i, [128, 512], BF16) for i in range(2)]
            B_hl = [Buf(), Buf()]
            osb = al("osb", [128, S], BF16)
            B_osb = Buf()
            for i in range(2):
                k.op("dve", lambda t, i=i: t.memset(hl[i][:], 0.0), writes=(B_hl[i],))
            k.op("dve", lambda t: t.memset(qa[64:128, :], 0.0), writes=(B_q,))
            k.op("dve", lambda t: t.memset(qb[0:64, :], 0.0), writes=(B_q,))
            cnt = {"z": 0, "t": 0, "e": 0, "a": 0}
            tst = {}
            for c in range(4):
                k.dma("sp", qa[0:64, :], s_sqt[c][0:64, :], reads=(B_scr["sqt"][c],), writes=(B_q,))
                k.dma("sp", qb[64:128, :], s_sqt[c][64:128, :], reads=(B_scr["sqt"][c],), writes=(B_q,))
                k.dma("sp", kt[:], s_skt[c], reads=(B_scr["skt"][c],), writes=(B_k,))
                k.dma("sp", sv[:], s_sv[:, c * 128:(c + 1) * 128].rearrange("(t p) n -> p t n", p=128),
                      reads=[B_scr["sv"][i] for i in range(NTB // 4)], writes=(B_v,))
                units = [(g, hh) for g in range(NTG) for hh in range(2)]

                def geom(g, kb):
                    lo = max(0, 128 * kb - 512 * g)
                    return lo, kb >= 4 * g

                def p1_block(u, kb):
                    g, hh = units[u]
                    q = qa if hh == 0 else qb
                    spt, Bs = spb[u % 2], B_sp[u % 2]
                    lo, diag = geom(g, kb)
                    zi = cnt["z"] % 2
                    cnt["z"] += 1
                    z, Bz = ps[zi], B_ps[zi]
                    ks = kt[:, kb * 128:(kb + 1) * 128]
                    q0 = g * 512
                    if diag:
                        k.mm(z[:, lo:lo + 128], Bz, [(identb[:], negmaskb[:]), (ks, q[:, q0 + lo:q0 + lo + 128])],
                             reads=(B_const, B_k, B_q))
                        if lo + 128 < 512:
                            k.mm(z[:, lo + 128:512], Bz, [(ks, q[:, q0 + lo + 128:q0 + 512])], reads=(B_k, B_q))
                    else:
                        k.mm(z[:, lo:512], Bz, [(ks, q[:, q0 + lo:q0 + 512])], reads=(B_k, B_q))
                    ei = cnt["e"] % 2
                    cnt["e"] += 1
                    k.op("act", lambda t: t.activation(out=et[ei][:, lo:512], in_=z[:, lo:512], func=AF.Exp),
                         reads=(Bz,), writes=(B_et[ei],))
                    tst[("p1", u, kb)] = ei

                def p1_mid(u, kb):
                    g, hh = units[u]
                    spt, Bs = spb[u % 2], B_sp[u % 2]
                    lo, diag = geom(g, kb)
                    ei = tst[("p1", u, kb)]
                    k.op("act", lambda t: t.activation(out=spt[:, kb, lo:512], in_=et[ei][:, lo:512], func=AF.Ln, bias=1.0),
                         reads=(B_et[ei],), writes=(Bs,))

                def p1_back(u, kb):
                    g, hh = units[u]
                    spt, Bs = spb[u % 2], B_sp[u % 2]
                    lo, diag = geom(g, kb)
                    nkb = 4 * g + 4
                    k.op("pe", lambda t: t.matmul(ps[4][0:48, lo:512], selb[:, kb, :], spt[:, kb, lo:512],
                                                   start=(kb == 0), stop=(kb == nkb - 1)),
                         reads=(Bs, B_const), writes=(B_ps[4],))

                def p1_fin(u):
                    h_, Bh = hl[u % 2], B_hl[u % 2]
                    k.op("dve", lambda t: t.tensor_copy(out=h_[0:48, :], in_=ps[4][0:48, :]),
                         reads=(B_ps[4],), writes=(Bh,))
                    k.op("dve", lambda t: t.tensor_tensor(out=h_[32:48, :], in0=ps[4][32:48, :], in1=h_[32:48, :],
                                                          op=ALU.subtract),
                         reads=(B_ps[4], Bh), writes=(Bh,))

                def p2_block(u, kb):
                    g, hh = units[u]
                    q = qa if hh == 0 else qb
                    spt, Bs = spb[u % 2], B_sp[u % 2]
                    h_, Bh = hl[u % 2], B_hl[u % 2]
                    lo, diag = geom(g, kb)
                    ti = 2 + cnt["t"] % 2
                    cnt["t"] += 1
                    T, Bt = ps[ti], B_ps[ti]
                    ks = kt[:, kb * 128:(kb + 1) * 128]
                    q0 = g * 512
                    rd = (B_const, B_k, B_q, Bs, Bh)

                    def grp(a, b, withmask):
                        prs = []
                        if withmask:
                            prs.append((identb[:], negmaskb[:]))
                        prs.append((ks, q[:, q0 + a:q0 + b]))
                        prs.append((negub[:], spt[:, kb, a:b]))
                        prs.append((negstepb[:, kb, :], h_[:, a:b]))
                        k.mm(T[:, a:b], Bt, prs, reads=rd)
                    if diag:
                        grp(lo, lo + 128, True)
                        if lo + 128 < 512:
                            grp(lo + 128, 512, False)
                    else:
                        grp(lo, 512, False)
                    ai = cnt["a"] % 4
                    cnt["a"] += 1
                    k.op("act", lambda t: t.activation(out=at[ai][:, lo:512], in_=T[:, lo:512], func=AF.Exp),
                         reads=(Bt,), writes=(B_at[ai],))
                    tst[("p2", u, kb)] = ai

                def p2_back(u, kb):
                    g, hh = units[u]
                    lo, diag = geom(g, kb)
                    ai = tst[("p2", u, kb)]
                    nkb = 4 * g + 4
                    k.op("pe", lambda t: t.matmul(ps[5][hh * 64:(hh + 1) * 64, lo:512], sv[:, kb, hh * 64:(hh + 1) * 64],
                                                   at[ai][:, lo:512], start=(kb == 0), stop=(kb == nkb - 1)),
                         reads=(B_at[ai], B_v), writes=(B_ps[5],))

                def p2_fin(u):
                    g, hh = units[u]
                    if hh == 1:
                        copy_op("dve", osb[:, g * 512:(g + 1) * 512], ps[5][:], (B_ps[5],), (B_osb,))

                nu = len(units)
                q_mid, q_back = [], []

                def do_mid(t):
                    if t[0] == "p1":
                        p1_mid(t[1], t[2])

                def do_back(t):
                    if t[0] == "p1":
                        p1_back(t[1], t[2])
                    else:
                        p2_back(t[1], t[2])

                def flush():
                    for t in q_mid:
                        do_mid(t)
                    del q_mid[:]
                    for t in q_back:
                        do_back(t)
                    del q_back[:]

                def step(t):
                    if t[0] == "p1":
                        p1_block(t[1], t[2])
                    else:
                        p2_block(t[1], t[2])
                    for x in q_mid:
                        do_mid(x)
                    del q_mid[:]
                    q_mid.append(t)
                    q_back.append(t)
                    if len(q_back) > 2:
                        do_back(q_back.pop(0))

                markers = []

                def do_markers():
                    for kind, uu in markers:
                        if kind == "p1fin":
                            p1_fin(uu)
                        else:
                            p2_fin(uu)
                    del markers[:]

                for u in range(nu + 1):
                    n1 = 4 * units[u][0] + 4 if u < nu else 0
                    n2 = 4 * units[u - 1][0] + 4 if u >= 1 else 0
                    seq = []
                    for i in range(max(n1, n2)):
                        if i < n1:
                            seq.append(("p1", u, i))
                        if i < n2:
                            seq.append(("p2", u - 1, i))
                    if n1 >= 2 and n2 >= 1:
                        seq.remove(("p1", u, 1))
                        seq.insert(1, ("p1", u, 1))
                    if n1 < 2:
                        flush()
                        do_markers()
                    for idx, t in enumerate(seq):
                        step(t)
                        if idx == 1:
                            do_markers()
                    if u < nu:
                        markers.append(("p1fin", u))
                    if u >= 1:
                        markers.append(("p2fin", u - 1))
                flush()
                do_markers()
                k.dma("pool", s_sbt[c], osb[:], reads=(B_osb,), writes=(B_scr["sbt"][c],))
            k.barrier()

    def ret_phase(l):
        with ExitStack() as p:
            def al(name, shape, dt):
                UID[0] += 1
                return p.enter_context(nc.sbuf_tensor("%s_%d" % (name, UID[0]), list(shape), dt))
            rq = al("rq", [128, NTB, 128], BF16)
            rk = al("rk", [128, NTB, 128], BF16)
            rv = al("rv", [128, NTB, 256], BF16)
            rg = al("rg", [128, NTB, 256], BF16)
            B_rq, B_rk, B_rv, B_rg = Buf(), Buf(), Buf(), Buf()
            obuf = al("obuf", [128, NTB, 256], F32)
            wk = al("wk", [128, NTB, 256], F32)
            rball = al("rball", [128, NTB, 256], BF16)
            yb = al("yb", [128, NTB, 256], BF16)
            B_ob, B_wk, B_rb, B_yb = Buf(), Buf(), Buf(), Buf()
            stt = al("stt", [128, NTB, 128], BF16)
            qd = al("qd", [128, NTB, 128], BF16)
            kd = al("kd", [128, NTB, 128], BF16)
            B_stt, B_qd, B_kd = Buf(), Buf(), Buf()
            stats = al("stats", [128, 6, NTB], F32)
            B_stats = Buf()
            ogt = al("ogt", [128, 2, S], BF16)
            B_ogt = Buf()
            for h in range(RH):
                k.dma("sp", rq[:].rearrange("p a b -> p (a b)"), s_rqt[h], reads=(B_scr["rqt"][h],), writes=(B_rq,))
                k.dma("sp", rk[:].rearrange("p a b -> p (a b)"), s_rkt[h], reads=(B_scr["rkt"][h],), writes=(B_rk,))
                k.dma("sp", rv[:], s_rv[:, h * 256:(h + 1) * 256].rearrange("(t p) n -> p t n", p=128),
                      reads=[B_scr["rv"][i] for i in range(NTB // 4)], writes=(B_rv,))
                k.dma("sp", rg[:], s_rg[:, h * 256:(h + 1) * 256].rearrange("(t p) n -> p t n", p=128),
                      reads=[B_scr["rg"][i] for i in range(NTB // 4)], writes=(B_rg,))
                for n0 in range(0, NTB, 8):
                    nn = min(8, NTB - n0)
                    for j in range(nn):
                        k.op("pe", lambda t, j=j, n0=n0: t.transpose(out=pT[:, j * 128:(j + 1) * 128], in_=rk[:, n0 + j, :],
                                                                   identity=identb[:]),
                             reads=(B_rk, B_const), writes=(B_pT,), inc=(j == nn - 1))
                    k.op("dve", lambda t, n0=n0, nn=nn: t.tensor_scalar(
                        out=kd[:, n0:n0 + nn, :].rearrange("p a b -> p (a b)"), in0=pT[:, 0:nn * 128],
                        scalar1=kdec[:, h:h + 1], scalar2=0.0, op0=ALU.mult, op1=ALU.add),
                        reads=(B_pT, B_const), writes=(B_kd,))
                for n in range(1, NTB):
                    pass
                k.op("dve", lambda t: t.tensor_tensor(out=qd[:], in0=rq[:],
                                                      in1=qdec[:, h, :].unsqueeze(1).broadcast_to([128, NTB, 128]),
                                                      op=ALU.mult),
                     reads=(B_rq, B_const), writes=(B_qd,))
                for n0 in range(0, NTB, 4):
                    pi = (n0 // 4) % 2
                    for j in range(4):
                        n = n0 + j
                        k.op("pe", lambda t, n=n, j=j, pi=pi: t.matmul(ps[pi][:, j * 128:(j + 1) * 128], rk[:, n, :], rq[:, n, :],
                                                                       start=True, stop=True),
                             reads=(B_rk, B_rq), writes=(B_ps[pi],), inc=(j == 3))
                    k.op("dve", lambda t, n0=n0, pi=pi: t.tensor_tensor(
                        out=stt[:, n0:n0 + 4, :], in0=ps[pi][:, :].rearrange("p (a b) -> p a b", b=128),
                        in1=dect[:, h, :].unsqueeze(1).broadcast_to([128, 4, 128]), op=ALU.mult),
                        reads=(B_ps[pi], B_const), writes=(B_stt,))
                for n0 in range(0, NTB - 1, 2):
                    pi = 2 + (n0 // 2) % 2
                    nn = min(2, NTB - 1 - n0)
                    for j in range(nn):
                        n = n0 + j
                        k.op("pe", lambda t, n=n, j=j, pi=pi: t.matmul(ps[pi][:, j * 256:(j + 1) * 256], kd[:, n, :], rv[:, n, :],
                                                                       start=True, stop=True),
                             reads=(B_kd, B_rv), writes=(B_ps[pi],), inc=(j == nn - 1))
                    copy_op(evac_eng(), wk[:, n0:n0 + nn, :].rearrange("p a b -> p (a b)"), ps[pi][:, 0:nn * 256],
                            (B_ps[pi],), (B_wk,))
                for n in range(1, NTB - 1):
                    k.op("dve", lambda t, n=n: t.scalar_tensor_tensor(out=wk[:, n, :], in0=wk[:, n - 1, :], scalar=CD[h],
                                                                      in1=wk[:, n, :], op0=ALU.mult, op1=ALU.add),
                         reads=(B_wk,), writes=(B_wk,))
                copy_op("act", rball[:, 0:NTB - 1, :], wk[:, 0:NTB - 1, :], (B_wk,), (B_rb,))
                for n0 in range(0, NTB, 2):
                    pi = 4 + (n0 // 2) % 2
                    for j in range(2):
                        n = n0 + j
                        prs = [(stt[:, n, :], rv[:, n, :])]
                        if n > 0:
                            prs.append((qd[:, n, :], rball[:, n - 1, :]))
                        k.mm(ps[pi][:, j * 256:(j + 1) * 256], B_ps[pi], prs, reads=(B_stt, B_rv, B_qd, B_rb))
                    copy_op("act", obuf[:, n0:n0 + 2, :].rearrange("p a b -> p (a b)"), ps[pi][:, :], (B_ps[pi],), (B_ob,))
                k.op("dve", lambda t: t.tensor_reduce(out=stats[:, 0, :], in_=obuf[:], axis=AX.X, op=ALU.add),
                     reads=(B_ob,), writes=(B_stats,))
                k.op("act", lambda t: t.activation(out=wk[:], in_=obuf[:], func=AF.Square), reads=(B_ob, B_rb), writes=(B_wk,))
                k.op("dve", lambda t: t.tensor_reduce(out=stats[:, 1, :], in_=wk[:], axis=AX.X, op=ALU.add),
                     reads=(B_wk,), writes=(B_stats,))
                k.op("dve", lambda t: t.tensor_scalar(out=stats[:, 2, :], in0=stats[:, 0, :], scalar1=1.0 / 256, scalar2=0.0,
                                                      op0=ALU.mult, op1=ALU.add), reads=(B_stats,), writes=(B_stats,))
                k.op("dve", lambda t: t.tensor_tensor(out=stats[:, 3, :], in0=stats[:, 2, :], in1=stats[:, 2, :], op=ALU.mult),
                     reads=(B_stats,), writes=(B_stats,))
                k.op("dve", lambda t: t.scalar_tensor_tensor(out=stats[:, 3, :], in0=stats[:, 1, :], scalar=1.0 / 256,
                                                             in1=stats[:, 3, :], op0=ALU.mult, op1=ALU.subtract),
                     reads=(B_stats,), writes=(B_stats,))
                k.op("act", lambda t: t.activation(out=stats[:, 4, :], in_=stats[:, 3, :], func=AF.Ln, bias=epsb[:]),
                     reads=(B_stats, B_const), writes=(B_stats,))
                k.op("act", lambda t: t.activation(out=stats[:, 4, :], in_=stats[:, 4, :], func=AF.Exp, scale=-0.5),
                     reads=(B_stats,), writes=(B_stats,))
                k.op("dve", lambda t: t.scalar_tensor_tensor(out=stats[:, 5, :], in0=stats[:, 2, :], scalar=-1.0,
                                                             in1=stats[:, 4, :], op0=ALU.mult, op1=ALU.mult),
                     reads=(B_stats,), writes=(B_stats,))
                for n in range(NTB):
                    if n % 2:
                        k.op("act", lambda t, n=n: t.activation(out=wk[:, n, :], in_=obuf[:, n, :], func=AF.Identity,
                                                                scale=stats[:, 4, n:n + 1], bias=stats[:, 5, n:n + 1]),
                             reads=(B_ob, B_stats), writes=(B_wk,))
                    else:
                        k.op("dve", lambda t, n=n: t.tensor_scalar(out=wk[:, n, :], in0=obuf[:, n, :],
                                                                   scalar1=stats[:, 4, n:n + 1], scalar2=stats[:, 5, n:n + 1],
                                                                   op0=ALU.mult, op1=ALU.add),
                             reads=(B_ob, B_stats), writes=(B_wk,))
                k.op("dve", lambda t: t.tensor_tensor(out=yb[:], in0=wk[:], in1=rg[:], op=ALU.mult),
                     reads=(B_wk, B_rg), writes=(B_yb,))
                for ec in range(2):
                    for n0 in range(0, NTB, 8):
                        nn = min(8, NTB - n0)
                        for j in range(nn):
                            k.op("pe", lambda t, j=j, n0=n0: t.transpose(out=pT[:, j * 128:(j + 1) * 128],
                                                                       in_=yb[:, n0 + j, ec * 128:(ec + 1) * 128],
                                                                       identity=identb[:]),
                                 reads=(B_yb, B_const), writes=(B_pT,), inc=(j == nn - 1))
                        copy_op(evac_eng(), ogt[:, ec, n0 * 128:(n0 + nn) * 128], pT[:, 0:nn * 128], (B_pT,), (B_ogt,))
                for ec in range(2):
                    k.dma("pool", s_retgt[h * 2 + ec], ogt[:, ec, :], reads=(B_ogt,), writes=(B_scr["retgt"][h * 2 + ec],))
            k.barrier()

    def post_phase(l):
        with ExitStack() as p:
            def al(name, shape, dt):
                UID[0] += 1
                return p.enter_context(nc.sbuf_tensor("%s_%d" % (name, UID[0]), list(shape), dt))
            wsbo = al("wsbo", [128, 4, D], BF16)
            wreto = al("wreto", [128, 8, D], BF16)
            wout = al("wout", [128, 8, D], BF16)
            B_wp = Buf()
            k.dma("sp", wsbo[:], wb_sb_o[l].rearrange("(c p) n -> p c n", p=128), reads=(B_w["in"],), writes=(B_wp,))
            k.dma("sp", wreto[:], wb_ret_o[l].rearrange("(c p) n -> p c n", p=128), reads=(B_w["in"],), writes=(B_wp,))
            k.dma("sp", wout[:], wb_out[l].rearrange("(c p) n -> p c n", p=128), reads=(B_w["in"],), writes=(B_wp,))
            sbt = al("sbt", [128, 4, 512], BF16)
            rgt = al("rgt", [128, 8, 512], BF16)
            grt = al("grt", [128, 8, 512], BF16)
            gst = al("gst", [128, 8, 512], BF16)
            B_ld = Buf()
            t1 = [al("pt1%d" % i, [128, 512], F32) for i in range(2)]
            t2 = [al("pt2%d" % i, [128, 512], F32) for i in range(2)]
            B_t1 = [Buf(), Buf()]
            B_t2 = [Buf(), Buf()]
            mt = al("mt", [128, 8, 512], BF16)
            B_mt = Buf()
            pc = [0]
            for tg in range(NTG):
                cols = slice(tg * 512, (tg + 1) * 512)
                k.dma("sp", sbt[:], s_sbt[:, :, cols].rearrange("c p n -> p c n"),
                      reads=B_scr["sbt"][0:4], writes=(B_ld,))
                k.dma("sp", rgt[:], s_retgt[:, :, cols].rearrange("c p n -> p c n"),
                      reads=B_scr["retgt"][0:8], writes=(B_ld,))
                k.dma("sp", grt[:], s_grt[:, :, cols].rearrange("c p n -> p c n"),
                      reads=B_scr["grt"][0:8], writes=(B_ld,))
                k.dma("sp", gst[:], s_gst[:, :, cols].rearrange("c p n -> p c n"),
                      reads=B_scr["gst"][0:8], writes=(B_ld,))
                for ec in range(8):
                    i2 = ec % 2
                    p1, p2 = i2 * 2, i2 * 2 + 1
                    es = slice(ec * 128, (ec + 1) * 128)
                    k.mm(ps[p1][:], B_ps[p1], [(wsbo[:, c, es], sbt[:, c, :]) for c in range(4)], reads=(B_wp, B_ld))
                    k.mm(ps[p2][:], B_ps[p2], [(wreto[:, c, es], rgt[:, c, :]) for c in range(8)], reads=(B_wp, B_ld))
                    k.op("dve", lambda t, p1=p1, i2=i2, ec=ec: t.tensor_tensor(out=t1[i2][:], in0=ps[p1][:], in1=gst[:, ec, :],
                                                                               op=ALU.mult),
                         reads=(B_ps[p1], B_ld), writes=(B_t1[i2],))
                    k.op("dve", lambda t, p2=p2, i2=i2, ec=ec: t.tensor_tensor(out=t2[i2][:], in0=ps[p2][:], in1=grt[:, ec, :],
                                                                               op=ALU.mult),
                         reads=(B_ps[p2], B_ld), writes=(B_t2[i2],))
                    k.op("pool", lambda t, i2=i2, ec=ec: t.tensor_tensor(out=mt[:, ec, :], in0=t1[i2][:], in1=t2[i2][:],
                                                                         op=ALU.add),
                         reads=(B_t1[i2], B_t2[i2]), writes=(B_mt,))
                for dc in range(8):
                    pi = 4 + dc % 2
                    k.mm(ps[pi][:], B_ps[pi], [(wout[:, c, dc * 128:(dc + 1) * 128], mt[:, c, :]) for c in range(8)],
                         reads=(B_wp, B_mt))
                    k.op("dve", lambda t, pi=pi, dc=dc: t.tensor_tensor(out=hT[:, dc, cols], in0=ps[pi][:], in1=hT[:, dc, cols],
                                                                        op=ALU.add),
                         reads=(B_ps[pi], B_h[dc][tg]), writes=(B_h[dc][tg],))
            k.barrier()

    def ffn_phase(l):
        with ExitStack() as p:
            def al(name, shape, dt):
                UID[0] += 1
                return p.enter_context(nc.sbuf_tensor("%s_%d" % (name, UID[0]), list(shape), dt))
            hn = al("hn2", [128, 8, 512], BF16)
            B_hn = Buf()
            sq = al("sq2", [128, 8, 512], BF16)
            rstd = al("rstd2", [128, 512], F32)
            B_sq, B_rstd = Buf(), Buf()
            wgu = [al("wgu%d" % i, [128, 8, 512], BF16) for i in range(2)]
            B_wgu = [Buf(), Buf()]
            wd = [al("wd%d" % i, [128, NFC, 256], BF16) for i in range(2)]
            B_wd = [Buf(), Buf()]
            actT = al("actT", [128, NFC, 512], BF16)
            B_act = Buf()
            sg = [al("sg%d" % i, [128, 512], F32) for i in range(2)]
            B_sg = [Buf(), Buf()]
            wc = [0, 0]
            for tg in range(NTG):
                cols = slice(tg * 512, (tg + 1) * 512)
                rmsnorm_tg(tg, DEPTH + l, sq, B_sq, rstd, B_rstd, lambda c: (hn[:, c, :], (B_hn,)))

                def load_gu(fg):
                    i = wc[0] % 2
                    wc[0] += 1
                    k.dma("sp", wgu[i][:, :, 0:256],
                          wb_gu[l][:, fg * 256:(fg + 1) * 256].rearrange("(c p) n -> p c n", p=128),
                          reads=(B_w["in"],), writes=(B_wgu[i],))
                    k.dma("sp", wgu[i][:, :, 256:512],
                          wb_gu[l][:, FF + fg * 256:FF + (fg + 1) * 256].rearrange("(c p) n -> p c n", p=128),
                          reads=(B_w["in"],), writes=(B_wgu[i],))
                    return i
                nfg = NFC // 2
                cur = load_gu(0)
                for fg in range(nfg):
                    nxt = load_gu(fg + 1) if fg + 1 < nfg else None
                    w, Bw = wgu[cur], B_wgu[cur]
                    for j in range(2):
                        fcn = fg * 2 + j
                        i2 = fcn % 2
                        pg, pu = 1 + i2 * 2, 2 + i2 * 2
                        k.mm(ps[pg][:], B_ps[pg], [(w[:, c, j * 128:(j + 1) * 128], hn[:, c, :]) for c in range(8)],
                             reads=(Bw, B_hn))
                        k.mm(ps[pu][:], B_ps[pu], [(w[:, c, 256 + j * 128:256 + (j + 1) * 128], hn[:, c, :]) for c in range(8)],
                             reads=(Bw, B_hn))
                        k.op("act", lambda t, i2=i2, pg=pg: t.activation(out=sg[i2][:], in_=ps[pg][:], func=AF.Silu),
                             reads=(B_ps[pg],), writes=(B_sg[i2],))
                        k.op("dve", lambda t, i2=i2, pu=pu, fcn=fcn: t.tensor_tensor(out=actT[:, fcn, :], in0=ps[pu][:],
                                                                                     in1=sg[i2][:], op=ALU.mult),
                             reads=(B_ps[pu], B_sg[i2]), writes=(B_act,))
                    cur = nxt

                def load_wd(dg):
                    i = wc[1] % 2
                    wc[1] += 1
                    k.dma("sp", wd[i][:], wb_dn[l][:, dg * 256:(dg + 1) * 256].rearrange("(c p) n -> p c n", p=128),
                          reads=(B_w["in"],), writes=(B_wd[i],))
                    return i
                cur = load_wd(0)
                for dg in range(4):
                    nxt = load_wd(dg + 1) if dg + 1 < 4 else None
                    for j in range(2):
                        dc = dg * 2 + j
                        pi = 5 + dc % 2
                        k.mm(ps[pi][:], B_ps[pi], [(wd[cur][:, f, j * 128:(j + 1) * 128], actT[:, f, :]) for f in range(NFC)],
                             reads=(B_wd[cur], B_act))
                        k.op("dve", lambda t, pi=pi, dc=dc: t.tensor_tensor(out=hT[:, dc, cols], in0=ps[pi][:],
                                                                            in1=hT[:, dc, cols], op=ALU.add),
                             reads=(B_ps[pi], B_h[dc][tg]), writes=(B_h[dc][tg],))
                    cur = nxt
            k.barrier()

    for s in range(NSEQ):
        for c in range(8):
            k.dma("sp", hT[:, c, :], xT[s, c * 128:(c + 1) * 128, :], writes=B_h[c])
        for l in range(DEPTH):
            k.new_epoch()
            layer(l)
        if debug in ("p1", "p2", "p3") or (isinstance(debug, str) and debug.startswith("g")):
            break
        with ExitStack() as p:
            sq = p.enter_context(nc.sbuf_tensor("sqf_%d" % s, [128, 8, 512], BF16))
            rstd = p.enter_context(nc.sbuf_tensor("rstdf_%d" % s, [128, 512], F32))
            of = [p.enter_context(nc.sbuf_tensor("of%d_%d" % (i, s), [128, 8, 512], F32)) for i in range(2)]
            B_sq, B_rstd = Buf(), Buf()
            B_of = [Buf(), Buf()]
            for tg in range(NTG):
                o, Bo = of[tg % 2], B_of[tg % 2]
                rmsnorm_tg(tg, 2 * DEPTH, sq, B_sq, rstd, B_rstd, lambda c, o=o, Bo=Bo: (o[:, c, :], (Bo,)))
                k.dma("pool", outT[s][:, tg * 512:(tg + 1) * 512].rearrange("(c p) n -> p c n", p=128), o[:],
                      reads=(Bo,))
            k.barrier()
    k.barrier()
    return nc, ctx, consts


_CACHE = {}


def _prep_inputs(inputs, S, DEPTH):
    consts, _ = host_consts(S)
    gl = [inputs["norm_mix"][l] for l in range(DEPTH)] + [inputs["norm_ffn"][l] for l in range(DEPTH)] + \
         [inputs["norm_final"]]
    gvec = np.stack([np.asarray(g, np.float32).reshape(8, 128).T for g in gl], 1)
    shared = {
        "w_in": np.ascontiguousarray(inputs["w_in"], np.float32),
        "w_ret_o": np.ascontiguousarray(inputs["w_ret_o"], np.float32),
        "w_sb_o": np.ascontiguousarray(inputs["w_sb_o"], np.float32),
        "w_out": np.ascontiguousarray(inputs["w_out"], np.float32),
        "w_gate_up": np.ascontiguousarray(inputs["w_gate_up"], np.float32),
        "w_down": np.ascontiguousarray(inputs["w_down"], np.float32),
        "gvec": np.ascontiguousarray(gvec.reshape(128, -1), np.float32),
    }
    for kk, v in consts.items():
        shared["c_" + kk] = np.ascontiguousarray(v, np.float32)
    return shared


def kernel(x, norm_mix, w_in, w_ret_o, w_sb_o, w_out, norm_ffn, w_gate_up, w_down, norm_final):
    x = np.asarray(x, np.float32)
    B, S, _ = x.shape
    DEPTH = np.asarray(w_in).shape[0]
    ncores = 8
    NSEQ = B // ncores
    inputs = dict(norm_mix=np.asarray(norm_mix), w_in=np.asarray(w_in), w_ret_o=np.asarray(w_ret_o),
                  w_sb_o=np.asarray(w_sb_o), w_out=np.asarray(w_out), norm_ffn=np.asarray(norm_ffn),
                  w_gate_up=np.asarray(w_gate_up), w_down=np.asarray(w_down), norm_final=np.asarray(norm_final))
    shared = _prep_inputs(inputs, S, DEPTH)
    nc, ctx, _ = build_program(NSEQ, S, DEPTH)
    in_maps = []
    for c in range(ncores):
        m = dict(shared)
        m["xT"] = np.ascontiguousarray(x[c * NSEQ:(c + 1) * NSEQ].transpose(0, 2, 1))
        in_maps.append(m)
    res = run_bass_kernel_spmd(nc, in_maps, core_ids=list(range(ncores)))
    out = np.empty((B, S, D), np.float32)
    for c in range(ncores):
        out[c * NSEQ:(c + 1) * NSEQ] = res.results[c]["outT"].transpose(0, 2, 1)
    return out
```

```python
import numpy as np
from contextlib import ExitStack
import concourse.bass as bass
import concourse.mybir as mybir
from concourse.bass_utils import run_bass_kernel_spmd

F32, BF16 = mybir.dt.float32, mybir.dt.bfloat16
AF = mybir.ActivationFunctionType
ALU = mybir.AluOpType
AX = mybir.AxisListType

D = 1024
NC8 = 8
RH, RDK, RDV = 4, 128, 256
SBH, SBD = 8, 64
FF = 2816
NFC = FF // 128
INC = 6656
EPS = 1e-6
O_RQ, O_RK, O_RV, O_RG, O_SQ, O_SK, O_SV, O_GR, O_GS = 0, 512, 1024, 2048, 3072, 3584, 4096, 4608, 5632


class Buf:
    __slots__ = ("w", "r", "excl")

    def __init__(self, excl=False):
        self.w = {}
        self.r = {}
        self.excl = excl


class KB:
    def __init__(self, nc, ctx):
        self.nc, self.ctx = nc, ctx
        self.engs = {"pe": nc.tensor, "act": nc.scalar, "dve": nc.vector, "pool": nc.gpsimd, "sp": nc.sync}
        self.sems = []
        self.esem = {}
        self.seen = {e: {} for e in self.engs}
        for e in self.engs:
            self.new_eng_sem(e)
        self.dpool = {}
        for q in ("sp", "pool"):
            self.dpool[q] = [[self._new_sem("d%s%d" % (q, i)), 0] for i in range(12)]
        self.dnext = {"sp": 0, "pool": 0}
        self.dsids = set(sl[0] for q in self.dpool for sl in self.dpool[q])

    def _new_sem(self, name):
        h = self.ctx.enter_context(self.nc.semaphore(name))
        self.sems.append(h)
        return len(self.sems) - 1

    def new_eng_sem(self, e):
        self.esem[e] = [self._new_sem("e%s%d" % (e, len(self.sems))), 0]

    def new_epoch(self):
        for e in ("pe", "act", "dve", "pool"):
            if self.esem[e][1] > 12000:
                self.new_eng_sem(e)

    def _waits(self, e, reads, writes):
        need = {}
        mysid0 = self.esem[e][0]
        for b in reads:
            for sid, v in b.w.items():
                need[sid] = max(need.get(sid, 0), v)
            if b.excl:
                for sid, v in b.r.items():
                    if sid != mysid0:
                        need[sid] = max(need.get(sid, 0), v)
        for b in writes:
            for sid, v in b.w.items():
                need[sid] = max(need.get(sid, 0), v)
            for sid, v in b.r.items():
                need[sid] = max(need.get(sid, 0), v)
        mysid = self.esem[e][0]
        for sid, v in need.items():
            if sid == mysid and e == "pe":
                continue
            if self.seen[e].get(sid, 0) >= v:
                continue
            self.engs[e].wait_ge(self.sems[sid], v)
            self.seen[e][sid] = v

    def op(self, e, fn, reads=(), writes=(), inc=True):
        self._waits(e, reads, writes)
        ins = fn(self.engs[e])
        mysid = self.esem[e][0]
        tgt = self.esem[e][1] + 1
        if inc:
            ins.then_inc(self.sems[mysid], 1)
            self.esem[e][1] = tgt
        for b in reads:
            b.r[mysid] = max(b.r.get(mysid, 0), tgt)
        for b in writes:
            b.w = {mysid: tgt}
            b.r = {}
        return ins

    def dma(self, q, out, in_, reads=(), writes=()):
        self._waits(q, reads, writes)
        pool = self.dpool[q]
        slot = pool[self.dnext[q] % len(pool)]
        self.dnext[q] += 1
        sid = slot[0]
        if slot[1] > 0 and self.seen[q].get(sid, 0) < slot[1] * 16:
            self.engs[q].wait_ge(self.sems[sid], slot[1] * 16)
            self.seen[q][sid] = slot[1] * 16
        ins = self.engs[q].dma_start(out=out, in_=in_)
        slot[1] += 1
        v = slot[1] * 16
        ins.then_inc(self.sems[sid], 16)
        for b in reads:
            b.r[sid] = max(b.r.get(sid, 0), v)
        for b in writes:
            b.w = {ks: kv for ks, kv in b.w.items() if ks in self.dsids}
            b.w[sid] = v
            b.r = {}

    def barrier(self):
        tg = {}
        for e in ("pe", "act", "dve", "pool"):
            sid, c = self.esem[e]
            if c > 0:
                tg[sid] = c
        for q in ("sp", "pool"):
            for sid, c in self.dpool[q]:
                if c > 0:
                    tg[sid] = c * 16
        for e in self.engs:
            for sid, v in tg.items():
                if self.seen[e].get(sid, 0) >= v:
                    continue
                self.engs[e].wait_ge(self.sems[sid], v)
                self.seen[e][sid] = v

    def mm(self, ps, psb, pairs, reads, first=True, last=True):
        n = len(pairs)
        for i, (l, r) in enumerate(pairs):
            st = first and i == 0
            sp = last and i == n - 1
            self.op("pe", lambda t, l=l, r=r, st=st, sp=sp: t.matmul(ps, l, r, start=st, stop=sp),
                    reads=reads if i == 0 else (), writes=(psb,), inc=(i == n - 1))


def host_consts(S):
    c = {}
    ar = np.arange(128)
    c["ident"] = np.eye(128, dtype=np.float32)
    c["negmask"] = np.where(ar[:, None] >= ar[None, :], -30000.0, 0.0).astype(np.float32)
    c["negu"] = np.where(ar[:, None] >= ar[None, :], -1.0, 0.0).astype(np.float32)
    prot = np.zeros((128, 128), np.float32)
    for m in range(64):
        prot[m + 64, m] = 1.0
        prot[m, m + 64] = 1.0
    c["prot"] = prot
    c["onesm"] = np.full((128, 128), 1.0 / 1024, np.float32)
    sel = np.zeros((128, 16, 48), np.float32)
    for kb in range(16):
        sel[:, kb, kb] = 1.0
        sel[:, kb, 32 + kb] = 1.0
    c["sel"] = sel.reshape(128, 16 * 48)
    ns = np.zeros((128, 16, 128), np.float32)
    for kb in range(16):
        for r in range(16):
            if r > kb:
                ns[r, kb, :] = -1.0
                ns[32 + r, kb, :] = -1.0
    c["negstep"] = ns.reshape(128, 16 * 128)
    half = 64
    inv = (1.0 / (np.float32(10000.0) ** (np.arange(half, dtype=np.float32) / np.float32(half)))).astype(np.float32)
    pos = np.arange(S, dtype=np.float32)
    ang = (pos[:, None] * inv[None, :]).astype(np.float32)
    cos = np.cos(ang).astype(np.float32).T
    sin = np.sin(ang).astype(np.float32).T
    cosf = np.concatenate([cos, cos], 0)
    sinf = np.concatenate([-sin, sin], 0)
    ksc = np.float32(RDK ** -0.5)
    c["rope"] = np.stack([cosf, sinf, cosf * ksc, sinf * ksc], 1).astype(np.float32).reshape(128, 4 * S)
    lg = np.log1p(-np.exp2(-5.0 - np.arange(RH, dtype=np.float32))).astype(np.float32)
    i = np.arange(128, dtype=np.float32)
    diff = i[None, :] - i[:, None]
    dect = np.where(diff[None] >= 0, np.exp(lg[:, None, None] * np.maximum(diff, 0.0)[None]), 0.0)
    c["dect"] = np.ascontiguousarray(dect.transpose(1, 0, 2)).astype(np.float32).reshape(128, RH * 128)
    c["kdec"] = np.exp(lg[None, :] * (127.0 - i)[:, None]).astype(np.float32)
    qdec = np.exp(lg[:, None] * (i + 1.0)[None, :]).astype(np.float32)
    c["qdec"] = np.broadcast_to(qdec[None], (128, RH, 128)).astype(np.float32).reshape(128, RH * 128).copy()
    cd = np.exp(lg * 128.0).astype(np.float32)
    return c, [float(x) for x in cd]


def build_program(NSEQ, S, DEPTH, debug=False):
    assert S % 512 == 0
    NTG = S // 512
    NTB = S // 128
    consts, CD = host_consts(S)
    nc = bass.Bass("TRN2", target_bir_lowering=False)
    ctx = ExitStack()

    def din(name, shape, dt=F32):
        return nc.dram_tensor(name, list(shape), dt, kind="ExternalInput").ap()

    def dscr(name, shape, dt=BF16):
        return nc.dram_tensor(name, list(shape), dt, kind=("ExternalOutput" if debug else "Internal")).ap()

    xT = din("xT", [NSEQ, D, S])
    outT = nc.dram_tensor("outT", [NSEQ, D, S], F32, kind="ExternalOutput").ap()
    w_in = din("w_in", [DEPTH, D, INC])
    w_ret_o = din("w_ret_o", [DEPTH, D, D])
    w_sb_o = din("w_sb_o", [DEPTH, 512, D])
    w_out = din("w_out", [DEPTH, D, D])
    w_gu = din("w_gate_up", [DEPTH, D, 2 * FF])
    w_dn = din("w_down", [DEPTH, FF, D])
    gvec = din("gvec", [128, (2 * DEPTH + 1) * 8])
    cin = {k: din("c_" + k, v.shape) for k, v in consts.items()}
    wb_in = dscr("wb_in", [DEPTH, D, INC])
    wb_ret_o = dscr("wb_ret_o", [DEPTH, D, D])
    wb_sb_o = dscr("wb_sb_o", [DEPTH, 512, D])
    wb_out = dscr("wb_out", [DEPTH, D, D])
    wb_gu = dscr("wb_gu", [DEPTH, D, 2 * FF])
    wb_dn = dscr("wb_dn", [DEPTH, FF, D])
    s_rqt = dscr("s_rqt", [4, 128, S])
    s_rkt = dscr("s_rkt", [4, 128, S])
    s_rv = dscr("s_rv", [S, 1024])
    s_rg = dscr("s_rg", [S, 1024])
    s_sqt = dscr("s_sqt", [4, 128, S])
    s_skt = dscr("s_skt", [4, 128, S])
    s_sv = dscr("s_sv", [S, 512])
    s_grt = dscr("s_grt", [8, 128, S])
    s_gst = dscr("s_gst", [8, 128, S])
    s_sbt = dscr("s_sbt", [4, 128, S])
    s_retgt = dscr("s_retgt", [8, 128, S])
    B_scr = {n: [Buf() for _ in range(16)] for n in
             ("rqt", "rkt", "rv", "rg", "sqt", "skt", "sv", "grt", "gst", "sbt", "retgt")}
    B_w = {n: Buf() for n in ("in", "ret_o", "sb_o", "out", "gu", "dn")}

    def sb(name, shape, dt):
        return ctx.enter_context(nc.sbuf_tensor(name, list(shape), dt))

    k = KB(nc, ctx)
    UID = [0]
    hT = sb("hT", [128, 8, S], F32)
    B_h = [[Buf() for _ in range(NTG)] for _ in range(8)]
    identb = sb("identb", [128, 128], BF16)
    negmaskb = sb("negmaskb", [128, 128], BF16)
    negub = sb("negub", [128, 128], BF16)
    protb = sb("protb", [128, 128], BF16)
    onesmb = sb("onesmb", [128, 128], BF16)
    selb = sb("selb", [128, 16, 48], BF16)
    negstepb = sb("negstepb", [128, 16, 128], BF16)
    dect = sb("dect", [128, RH, 128], F32)
    kdec = sb("kdec", [128, RH], F32)
    qdec = sb("qdec", [128, RH, 128], F32)
    gv = sb("gv", [128, 2 * DEPTH + 1, 8], F32)
    epsb = sb("epsb", [128, 1], F32)
    B_const = Buf()
    ps = [ctx.enter_context(nc.psum_tensor("ps%d" % i, [128, 512], F32)) for i in range(7)]
    pT = ctx.enter_context(nc.psum_tensor("pT", [128, 1024], BF16))
    B_ps = [Buf(excl=True) for _ in range(7)]
    B_pT = Buf(excl=True)
    rr = {"evac": 0}

    def evac_eng():
        rr["evac"] += 1
        return "act" if rr["evac"] % 2 else "dve"

    def copy_op(e, out, in_, reads, writes):
        if e == "act":
            k.op("act", lambda t: t.activation(out=out, in_=in_, func=AF.Copy), reads=reads, writes=writes)
        else:
            k.op(e, lambda t: t.tensor_copy(out=out, in_=in_), reads=reads, writes=writes)

    with ExitStack() as pctx:
        NSTG = 4
        stg = [pctx.enter_context(nc.sbuf_tensor("stg%d" % i, [128, 2048], F32)) for i in range(NSTG)]
        stb = [pctx.enter_context(nc.sbuf_tensor("stb%d" % i, [128, 2048], BF16)) for i in range(NSTG)]
        B_stg = [Buf() for _ in range(NSTG)]
        B_stb = [Buf() for _ in range(NSTG)]
        cnt = [0]

        def conv(dst_sb_ap, src_dram, ncol, npart=128):
            i = cnt[0] % NSTG
            cnt[0] += 1
            k.dma("sp", stg[i][0:npart, 0:ncol], src_dram, writes=(B_stg[i],))
            copy_op("dve", dst_sb_ap, stg[i][0:npart, 0:ncol], (B_stg[i],), (B_const,))

        conv(identb[:], cin["ident"], 128)
        conv(negmaskb[:], cin["negmask"], 128)
        conv(negub[:], cin["negu"], 128)
        conv(protb[:], cin["prot"], 128)
        conv(onesmb[:], cin["onesm"], 128)
        conv(selb[:].rearrange("p a b -> p (a b)"), cin["sel"], 16 * 48)
        conv(negstepb[:].rearrange("p a b -> p (a b)"), cin["negstep"], 2048)
        k.dma("sp", dect[:].rearrange("p a b -> p (a b)"), cin["dect"], writes=(B_const,))
        k.dma("sp", kdec[:], cin["kdec"], writes=(B_const,))
        k.dma("sp", qdec[:].rearrange("p a b -> p (a b)"), cin["qdec"], writes=(B_const,))
        k.dma("sp", gv[:].rearrange("p a b -> p (a b)"), gvec, writes=(B_const,))
        k.op("dve", lambda t: t.memset(epsb[:], EPS), writes=(B_const,))

        def conv_w(dst, src, R, C):
            for r0 in range(0, R, 128):
                for c0 in range(0, C, 2048):
                    cw = min(2048, C - c0)
                    i = cnt[0] % NSTG
                    e = ("dve", "act", "dve", "act", "pool")[cnt[0] % 5]
                    cnt[0] += 1
                    k.dma("sp", stg[i][:, 0:cw], src[r0:r0 + 128, c0:c0 + cw], writes=(B_stg[i],))
                    copy_op(e, stb[i][:, 0:cw], stg[i][:, 0:cw], (B_stg[i],), (B_stb[i],))
                    k.dma("pool", dst[r0:r0 + 128, c0:c0 + cw], stb[i][:, 0:cw], reads=(B_stb[i],),
                          writes=(B_w["in"],))

        for l in range(DEPTH):
            conv_w(wb_in[l], w_in[l], D, INC)
            conv_w(wb_ret_o[l], w_ret_o[l], D, D)
            conv_w(wb_sb_o[l], w_sb_o[l], 512, D)
            conv_w(wb_out[l], w_out[l], D, D)
            conv_w(wb_gu[l], w_gu[l], D, 2 * FF)
            conv_w(wb_dn[l], w_dn[l], FF, D)
        k.barrier()

    if debug == "p0":
        return nc, ctx, consts
    def rmsnorm_tg(tg, gidx, sq, B_sq, rstd, B_rstd, out_fn):
        cols = slice(tg * 512, (tg + 1) * 512)
        k.op("act", lambda t: t.activation(out=sq[:], in_=hT[:, :, cols], func=AF.Square),
             reads=[B_h[c][tg] for c in range(8)], writes=(B_sq,))
        k.mm(ps[0][:], B_ps[0], [(onesmb[:], sq[:, c, :]) for c in range(8)], reads=(B_sq, B_const))
        k.op("act", lambda t: t.activation(out=rstd[:], in_=ps[0][:], func=AF.Ln, bias=epsb[:]),
             reads=(B_ps[0], B_const), writes=(B_rstd,))
        k.op("act", lambda t: t.activation(out=rstd[:], in_=rstd[:], func=AF.Exp, scale=-0.5),
             reads=(B_rstd,), writes=(B_rstd,))
        for c in range(8):
            o, ob = out_fn(c)
            k.op("dve", lambda t, o=o, c=c: t.scalar_tensor_tensor(
                out=o, in0=hT[:, c, cols], scalar=gv[:, gidx, c:c + 1], in1=rstd[:], op0=ALU.mult, op1=ALU.mult),
                reads=(B_h[c][tg], B_rstd, B_const), writes=ob)

    def layer(l):
        with ExitStack() as p:
            def al(name, shape, dt):
                UID[0] += 1
                return p.enter_context(nc.sbuf_tensor("%s_%d" % (name, UID[0]), list(shape), dt))
            hnT = al("hnT", [128, 8, S], BF16)
            B_hn = [Buf() for _ in range(NTG)]
            sq = al("sq", [128, 8, 512], BF16)
            rstd = al("rstd", [128, 512], F32)
            B_sq, B_rstd = Buf(), Buf()
            def norm1(tg):
                rmsnorm_tg(tg, l, sq, B_sq, rstd, B_rstd,
                           lambda c, tg=tg: (hnT[:, c, tg * 512:(tg + 1) * 512], (B_hn[tg],)))
            norm1(0)
            wt = [al("wt%d" % i, [128, 8, 512], BF16) for i in range(2)]
            B_wt = [Buf(), Buf()]
            ost = [al("ost%d" % i, [128, max(S, 2048)], BF16) for i in range(2)]
            B_ost = [Buf(), Buf()]
            rot = al("rot", [128, 4, 512], F32)
            B_rot = Buf()
            xb = al("xb", [128, 512], BF16)
            B_xb = Buf()
            t1 = al("t1", [128, 512], F32)
            t2 = al("t2", [128, 512], F32)
            B_t1, B_t2 = Buf(), Buf()
            st = {"ps": 1, "ost": 0}

            def next_ps():
                st["ps"] = 1 + (st["ps"] % 5)
                return st["ps"]

            def load_w(gi):
                i = gi % 2
                k.dma("sp", wt[i][:], wb_in[l][:, gi * 512:(gi + 1) * 512].rearrange("(c p) n -> p c n", p=128),
                      reads=(B_w["in"],), writes=(B_wt[i],))

            NG = INC // 512
            load_w(0)
            rope_v = cin["rope"].rearrange("p (a s) -> p a s", a=4)
            for gi in range(NG):
                if isinstance(debug, str) and debug.startswith("g") and gi >= int(debug[1:2]):
                    break
                stage = debug[2:] if isinstance(debug, str) and debug.startswith("g") else ""

                if gi + 1 < NG:
                    load_w(gi + 1)
                w = wt[gi % 2]
                Bw = B_wt[gi % 2]
                col0 = gi * 512
                if col0 in (O_RQ, O_RK):
                    isk = col0 == O_RK
                    dst = s_rkt if isk else s_rqt
                    Bd = B_scr["rkt" if isk else "rqt"]
                    for tg in range(NTG):
                        if gi == 0 and tg + 1 < NTG:
                            norm1(tg + 1)
                        k.dma("sp", rot[:], rope_v[:, :, tg * 512:(tg + 1) * 512], writes=(B_rot,))
                        for h in range(4):
                            pi = next_ps()
                            k.mm(ps[pi][:], B_ps[pi],
                                 [(w[:, c, h * 128:(h + 1) * 128], hnT[:, c, tg * 512:(tg + 1) * 512]) for c in range(8)],
                                 reads=(Bw, B_hn[tg]))
                            if stage == "a":
                                continue
                            copy_op("act", xb[:], ps[pi][:], (B_ps[pi],), (B_xb,))
                            if stage == "b":
                                continue
                            k.mm(ps[6][:], B_ps[6], [(protb[:], xb[:])], reads=(B_xb, B_const))
                            if stage == "c":
                                continue
                            tb = 2 if isk else 0
                            k.op("dve", lambda t, pi=pi, tb=tb: t.tensor_tensor(
                                out=t1[:], in0=ps[pi][:], in1=rot[:, tb, :], op=ALU.mult),
                                reads=(B_ps[pi], B_rot), writes=(B_t1,))
                            k.op("dve", lambda t, tb=tb: t.tensor_tensor(
                                out=t2[:], in0=ps[6][:], in1=rot[:, tb + 1, :], op=ALU.mult),
                                reads=(B_ps[6], B_rot), writes=(B_t2,))
                            if stage == "d":
                                continue
                            o = ost[h % 2]
                            k.op("dve", lambda t, o=o, tg=tg: t.tensor_tensor(
                                out=o[:, tg * 512:(tg + 1) * 512], in0=t1[:], in1=t2[:], op=ALU.add),
                                reads=(B_t1, B_t2), writes=(B_ost[h % 2],))
                            if stage == "e":
                                continue
                            k.dma("pool", dst[h][:, tg * 512:(tg + 1) * 512], o[:, tg * 512:(tg + 1) * 512],
                                  reads=(B_ost[h % 2],), writes=(Bd[h],))
                elif col0 in (O_SQ, O_SK, O_GR, O_GR + 512, O_GS, O_GS + 512):
                    if col0 == O_SQ:
                        dst, Bd, base, fn = s_sqt, B_scr["sqt"], 0, AF.Identity
                    elif col0 == O_SK:
                        dst, Bd, base, fn = s_skt, B_scr["skt"], 0, AF.Copy
                    elif col0 >= O_GS:
                        dst, Bd, base, fn = s_gst, B_scr["gst"], (col0 - O_GS) // 128, AF.Sigmoid
                    else:
                        dst, Bd, base, fn = s_grt, B_scr["grt"], (col0 - O_GR) // 128, AF.Sigmoid
                    for fc in range(4):
                        oi = st["ost"] % 2
                        st["ost"] += 1
                        o = ost[oi]
                        for tg in range(NTG):
                            pi = next_ps()
                            k.mm(ps[pi][:], B_ps[pi],
                                 [(w[:, c, fc * 128:(fc + 1) * 128], hnT[:, c, tg * 512:(tg + 1) * 512]) for c in range(8)],
                                 reads=(Bw, B_hn[tg]))
                            if fn == AF.Copy:
                                copy_op(evac_eng(), o[:, tg * 512:(tg + 1) * 512], ps[pi][:], (B_ps[pi],), (B_ost[oi],))
                            elif fn == AF.Identity:
                                k.op("dve", lambda t, o=o, tg=tg, pi=pi: t.tensor_scalar(
                                    out=o[:, tg * 512:(tg + 1) * 512], in0=ps[pi][:], scalar1=0.125, scalar2=0.0,
                                    op0=ALU.mult, op1=ALU.add),
                                    reads=(B_ps[pi],), writes=(B_ost[oi],))
                            else:
                                k.op("act", lambda t, o=o, tg=tg, pi=pi: t.activation(
                                    out=o[:, tg * 512:(tg + 1) * 512], in_=ps[pi][:], func=AF.Sigmoid),
                                    reads=(B_ps[pi],), writes=(B_ost[oi],))
                        k.dma("pool", dst[base + fc], o[:, 0:S], reads=(B_ost[oi],), writes=(Bd[base + fc],))
                else:
                    if col0 >= O_SV:
                        dst, Bd, cb, fn = s_sv, B_scr["sv"], 0, AF.Copy
                    elif col0 >= O_RG:
                        dst, Bd, cb, fn = s_rg, B_scr["rg"], col0 - O_RG, AF.Silu
                    else:
                        dst, Bd, cb, fn = s_rv, B_scr["rv"], col0 - O_RV, AF.Copy
                    for tq in range(NTB // 4):
                        oi = st["ost"] % 2
                        st["ost"] += 1
                        o = ost[oi]
                        for tb4 in range(4):
                            tb = tq * 4 + tb4
                            pi = next_ps()
                            k.mm(ps[pi][:], B_ps[pi],
                                 [(hnT[:, c, tb * 128:(tb + 1) * 128], w[:, c, :]) for c in range(8)],
                                 reads=(Bw, B_hn[tb // 4]))
                            if fn == AF.Copy:
                                copy_op(evac_eng(), o[:, tb4 * 512:(tb4 + 1) * 512], ps[pi][:], (B_ps[pi],), (B_ost[oi],))
                            else:
                                k.op("act", lambda t, o=o, tb4=tb4, pi=pi: t.activation(
                                    out=o[:, tb4 * 512:(tb4 + 1) * 512], in_=ps[pi][:], func=AF.Silu),
                                    reads=(B_ps[pi],), writes=(B_ost[oi],))
                        k.dma("pool",
                              dst[tq * 512:(tq + 1) * 512, cb:cb + 512].rearrange("(t p) n -> p t n", p=128),
                              o[:, 0:2048].rearrange("p (t n) -> p t n", n=512),
                              reads=(B_ost[oi],), writes=(Bd[tq],))
            k.barrier()
        if debug == "p1" or (isinstance(debug, str) and debug.startswith("g")):
            return
        sb_phase(l)
        if debug == "p2":
            return
        ret_phase(l)
        if debug == "p3":
            return
        post_phase(l)
        if debug == "p4":
            return
        ffn_phase(l)

    def sb_phase(l):
        with ExitStack() as p:
            def al(name, shape, dt):
                UID[0] += 1
                return p.enter_context(nc.sbuf_tensor("%s_%d" % (name, UID[0]), list(shape), dt))
            qas = [al("qa%d" % i, [128, S], BF16) for i in range(2)]
            qbs = [al("qb%d" % i, [128, S], BF16) for i in range(2)]
            kts = [al("kt%d" % i, [128, S], BF16) for i in range(2)]
            svs = [al("sv%d" % i, [128, NTB, 128], BF16) for i in range(2)]
            B_qs, B_ks, B_vs = [Buf(), Buf()], [Buf(), Buf()], [Buf(), Buf()]
            spb = [al("spb%d" % i, [128, NTB, 512], BF16) for i in range(2)]
            B_sp = [Buf(), Buf()]
            et = [al("et%d" % i, [128, 512], F32) for i in range(2)]
            B_et = [Buf(), Buf()]
            at = [al("at%d" % i, [128, 512], BF16) for i in range(4)]
            B_at = [Buf() for _ in range(4)]
            hl = [al("hl%d" % i, [128, 512], BF16) for i in range(2)]
            B_hl = [Buf(), Buf()]
            osb = al("osb", [128, S], BF16)
            B_osb = Buf()
            for i in range(2):
                k.op("dve", lambda t, i=i: t.memset(hl[i][:], 0.0), writes=(B_hl[i],))
            for i in range(2):
                k.op("dve", lambda t, i=i: t.memset(qas[i][64:128, :], 0.0), writes=(B_qs[i],))
                k.op("dve", lambda t, i=i: t.memset(qbs[i][0:64, :], 0.0), writes=(B_qs[i],))

            def load_pair(c):
                i = c % 2
                k.dma("sp", qas[i][0:64, :], s_sqt[c][0:64, :], reads=(B_scr["sqt"][c],), writes=(B_qs[i],))
                k.dma("sp", qbs[i][64:128, :], s_sqt[c][64:128, :], reads=(B_scr["sqt"][c],), writes=(B_qs[i],))
                k.dma("sp", kts[i][:], s_skt[c], reads=(B_scr["skt"][c],), writes=(B_ks[i],))
                k.dma("sp", svs[i][:], s_sv[:, c * 128:(c + 1) * 128].rearrange("(t p) n -> p t n", p=128),
                      reads=[B_scr["sv"][j] for j in range(NTB // 4)], writes=(B_vs[i],))
            load_pair(0)
            cnt = {"z": 0, "t": 0, "e": 0, "a": 0}
            tst = {}
            for c in range(4):
                if c + 1 < 4:
                    load_pair(c + 1)
                qa, qb, kt, sv = qas[c % 2], qbs[c % 2], kts[c % 2], svs[c % 2]
                B_q, B_k, B_v = B_qs[c % 2], B_ks[c % 2], B_vs[c % 2]
                units = [(g, hh) for g in range(NTG) for hh in range(2)]

                def geom(g, kb):
                    lo = max(0, 128 * kb - 512 * g)
                    return lo, kb >= 4 * g

                def p1_block(u, kb):
                    g, hh = units[u]
                    q = qa if hh == 0 else qb
                    spt, Bs = spb[u % 2], B_sp[u % 2]
                    lo, diag = geom(g, kb)
                    zi = cnt["z"] % 2
                    cnt["z"] += 1
                    z, Bz = ps[zi], B_ps[zi]
                    ks = kt[:, kb * 128:(kb + 1) * 128]
                    q0 = g * 512
                    if diag:
                        k.mm(z[:, lo:lo + 128], Bz, [(identb[:], negmaskb[:]), (ks, q[:, q0 + lo:q0 + lo + 128])],
                             reads=(B_const, B_k, B_q))
                        if lo + 128 < 512:
                            k.mm(z[:, lo + 128:512], Bz, [(ks, q[:, q0 + lo + 128:q0 + 512])], reads=(B_k, B_q))
                    else:
                        k.mm(z[:, lo:512], Bz, [(ks, q[:, q0 + lo:q0 + 512])], reads=(B_k, B_q))
                    ei = cnt["e"] % 2
                    cnt["e"] += 1
                    k.op("act", lambda t: t.activation(out=et[ei][:, lo:512], in_=z[:, lo:512], func=AF.Exp),
                         reads=(Bz,), writes=(B_et[ei],))
                    tst[("p1", u, kb)] = ei

                def p1_mid(u, kb):
                    g, hh = units[u]
                    spt, Bs = spb[u % 2], B_sp[u % 2]
                    lo, diag = geom(g, kb)
                    ei = tst[("p1", u, kb)]
                    k.op("act", lambda t: t.activation(out=spt[:, kb, lo:512], in_=et[ei][:, lo:512], func=AF.Ln, bias=1.0),
                         reads=(B_et[ei],), writes=(Bs,))

                def p1_back(u, kb):
                    g, hh = units[u]
                    spt, Bs = spb[u % 2], B_sp[u % 2]
                    lo, diag = geom(g, kb)
                    nkb = 4 * g + 4
                    k.op("pe", lambda t: t.matmul(ps[4][0:48, lo:512], selb[:, kb, :], spt[:, kb, lo:512],
                                                   start=(kb == 0), stop=(kb == nkb - 1)),
                         reads=(Bs, B_const), writes=(B_ps[4],))

                def p1_fin(u):
                    h_, Bh = hl[u % 2], B_hl[u % 2]
                    k.op("dve", lambda t: t.tensor_copy(out=h_[0:48, :], in_=ps[4][0:48, :]),
                         reads=(B_ps[4],), writes=(Bh,))
                    k.op("dve", lambda t: t.tensor_tensor(out=h_[32:48, :], in0=ps[4][32:48, :], in1=h_[32:48, :],
                                                          op=ALU.subtract),
                         reads=(B_ps[4], Bh), writes=(Bh,))

                def p2_block(u, kb):
                    g, hh = units[u]
                    q = qa if hh == 0 else qb
                    spt, Bs = spb[u % 2], B_sp[u % 2]
                    h_, Bh = hl[u % 2], B_hl[u % 2]
                    lo, diag = geom(g, kb)
                    ti = 2 + cnt["t"] % 2
                    cnt["t"] += 1
                    T, Bt = ps[ti], B_ps[ti]
                    ks = kt[:, kb * 128:(kb + 1) * 128]
                    q0 = g * 512
                    rd = (B_const, B_k, B_q, Bs, Bh)

                    def grp(a, b, withmask):
                        prs = []
                        if withmask:
                            prs.append((identb[:], negmaskb[:]))
                        prs.append((ks, q[:, q0 + a:q0 + b]))
                        prs.append((negub[:], spt[:, kb, a:b]))
                        prs.append((negstepb[:, kb, :], h_[:, a:b]))
                        k.mm(T[:, a:b], Bt, prs, reads=rd)
                    if diag:
                        grp(lo, lo + 128, True)
                        if lo + 128 < 512:
                            grp(lo + 128, 512, False)
                    else:
                        grp(lo, 512, False)
                    ai = cnt["a"] % 4
                    cnt["a"] += 1
                    k.op("act", lambda t: t.activation(out=at[ai][:, lo:512], in_=T[:, lo:512], func=AF.Exp),
                         reads=(Bt,), writes=(B_at[ai],))
                    tst[("p2", u, kb)] = ai

                def p2_back(u, kb):
                    g, hh = units[u]
                    lo, diag = geom(g, kb)
                    ai = tst[("p2", u, kb)]
                    nkb = 4 * g + 4
                    k.op("pe", lambda t: t.matmul(ps[5][hh * 64:(hh + 1) * 64, lo:512], sv[:, kb, hh * 64:(hh + 1) * 64],
                                                   at[ai][:, lo:512], start=(kb == 0), stop=(kb == nkb - 1)),
                         reads=(B_at[ai], B_v), writes=(B_ps[5],))

                def p2_fin(u):
                    g, hh = units[u]
                    if hh == 1:
                        copy_op("dve", osb[:, g * 512:(g + 1) * 512], ps[5][:], (B_ps[5],), (B_osb,))

                nu = len(units)
                q_mid, q_back = [], []

                def do_mid(t):
                    if t[0] == "p1":
                        p1_mid(t[1], t[2])

                def do_back(t):
                    if t[0] == "p1":
                        p1_back(t[1], t[2])
                    else:
                        p2_back(t[1], t[2])

                def flush():
                    for t in q_mid:
                        do_mid(t)
                    del q_mid[:]
                    for t in q_back:
                        do_back(t)
                    del q_back[:]

                def step(t):
                    if t[0] == "p1":
                        p1_block(t[1], t[2])
                    else:
                        p2_block(t[1], t[2])
                    for x in q_mid:
                        do_mid(x)
                    del q_mid[:]
                    q_mid.append(t)
                    q_back.append(t)
                    if len(q_back) > 2:
                        do_back(q_back.pop(0))

                markers = []

                def do_markers():
                    for kind, uu in markers:
                        if kind == "p1fin":
                            p1_fin(uu)
                        else:
                            p2_fin(uu)
                    del markers[:]

                for u in range(nu + 1):
                    n1 = 4 * units[u][0] + 4 if u < nu else 0
                    n2 = 4 * units[u - 1][0] + 4 if u >= 1 else 0
                    seq = []
                    for i in range(max(n1, n2)):
                        if i < n1:
                            seq.append(("p1", u, i))
                        if i < n2:
                            seq.append(("p2", u - 1, i))
                    if n1 >= 2 and n2 >= 1:
                        seq.remove(("p1", u, 1))
                        seq.insert(1, ("p1", u, 1))
                    if n1 < 2:
                        flush()
                        do_markers()
                    for idx, t in enumerate(seq):
                        step(t)
                        if idx == 1:
                            do_markers()
                    if u < nu:
                        markers.append(("p1fin", u))
                    if u >= 1:
                        markers.append(("p2fin", u - 1))
                flush()
                do_markers()
                k.dma("pool", s_sbt[c], osb[:], reads=(B_osb,), writes=(B_scr["sbt"][c],))
            k.barrier()

    def ret_phase(l):
        with ExitStack() as p:
            def al(name, shape, dt):
                UID[0] += 1
                return p.enter_context(nc.sbuf_tensor("%s_%d" % (name, UID[0]), list(shape), dt))
            rq = al("rq", [128, NTB, 128], BF16)
            rk = al("rk", [128, NTB, 128], BF16)
            rv = al("rv", [128, NTB, 256], BF16)
            rg = al("rg", [128, NTB, 256], BF16)
            B_rq, B_rk, B_rv, B_rg = Buf(), Buf(), Buf(), Buf()
            obuf = al("obuf", [128, NTB, 256], F32)
            wk = al("wk", [128, NTB, 256], F32)
            rball = al("rball", [128, NTB, 256], BF16)
            yb = al("yb", [128, NTB, 256], BF16)
            B_ob, B_wk, B_rb, B_yb = Buf(), Buf(), Buf(), Buf()
            stt = al("stt", [128, NTB, 128], BF16)
            qd = al("qd", [128, NTB, 128], BF16)
            kd = al("kd", [128, NTB, 128], BF16)
            B_stt, B_qd, B_kd = Buf(), Buf(), Buf()
            stats = al("stats", [128, 6, NTB], F32)
            B_stats = Buf()
            ogt = al("ogt", [128, 2, S], BF16)
            B_ogt = Buf()
            for h in range(RH):
                k.dma("sp", rq[:].rearrange("p a b -> p (a b)"), s_rqt[h], reads=(B_scr["rqt"][h],), writes=(B_rq,))
                k.dma("sp", rk[:].rearrange("p a b -> p (a b)"), s_rkt[h], reads=(B_scr["rkt"][h],), writes=(B_rk,))
                k.dma("sp", rv[:], s_rv[:, h * 256:(h + 1) * 256].rearrange("(t p) n -> p t n", p=128),
                      reads=[B_scr["rv"][i] for i in range(NTB // 4)], writes=(B_rv,))
                k.dma("sp", rg[:], s_rg[:, h * 256:(h + 1) * 256].rearrange("(t p) n -> p t n", p=128),
                      reads=[B_scr["rg"][i] for i in range(NTB // 4)], writes=(B_rg,))
                for n0 in range(0, NTB, 8):
                    nn = min(8, NTB - n0)
                    for j in range(nn):
                        k.op("pe", lambda t, j=j, n0=n0: t.transpose(out=pT[:, j * 128:(j + 1) * 128], in_=rk[:, n0 + j, :],
                                                                   identity=identb[:]),
                             reads=(B_rk, B_const), writes=(B_pT,), inc=(j == nn - 1))
                    k.op("dve", lambda t, n0=n0, nn=nn: t.tensor_scalar(
                        out=kd[:, n0:n0 + nn, :].rearrange("p a b -> p (a b)"), in0=pT[:, 0:nn * 128],
                        scalar1=kdec[:, h:h + 1], scalar2=0.0, op0=ALU.mult, op1=ALU.add),
                        reads=(B_pT, B_const), writes=(B_kd,))
                for n in range(1, NTB):
                    pass
                k.op("dve", lambda t: t.tensor_tensor(out=qd[:], in0=rq[:],
                                                      in1=qdec[:, h, :].unsqueeze(1).broadcast_to([128, NTB, 128]),
                                                      op=ALU.mult),
                     reads=(B_rq, B_const), writes=(B_qd,))
                for n0 in range(0, NTB, 4):
                    pi = (n0 // 4) % 2
                    for j in range(4):
                        n = n0 + j
                        k.op("pe", lambda t, n=n, j=j, pi=pi: t.matmul(ps[pi][:, j * 128:(j + 1) * 128], rk[:, n, :], rq[:, n, :],
                                                                       start=True, stop=True),
                             reads=(B_rk, B_rq), writes=(B_ps[pi],), inc=(j == 3))
                    k.op("dve", lambda t, n0=n0, pi=pi: t.tensor_tensor(
                        out=stt[:, n0:n0 + 4, :], in0=ps[pi][:, :].rearrange("p (a b) -> p a b", b=128),
                        in1=dect[:, h, :].unsqueeze(1).broadcast_to([128, 4, 128]), op=ALU.mult),
                        reads=(B_ps[pi], B_const), writes=(B_stt,))
                for n0 in range(0, NTB - 1, 2):
                    pi = 2 + (n0 // 2) % 2
                    nn = min(2, NTB - 1 - n0)
                    for j in range(nn):
                        n = n0 + j
                        k.op("pe", lambda t, n=n, j=j, pi=pi: t.matmul(ps[pi][:, j * 256:(j + 1) * 256], kd[:, n, :], rv[:, n, :],
                                                                       start=True, stop=True),
                             reads=(B_kd, B_rv), writes=(B_ps[pi],), inc=(j == nn - 1))
                    copy_op(evac_eng(), wk[:, n0:n0 + nn, :].rearrange("p a b -> p (a b)"), ps[pi][:, 0:nn * 256],
                            (B_ps[pi],), (B_wk,))
                for n in range(1, NTB - 1):
                    k.op("dve", lambda t, n=n: t.scalar_tensor_tensor(out=wk[:, n, :], in0=wk[:, n - 1, :], scalar=CD[h],
                                                                      in1=wk[:, n, :], op0=ALU.mult, op1=ALU.add),
                         reads=(B_wk,), writes=(B_wk,))
                copy_op("act", rball[:, 0:NTB - 1, :], wk[:, 0:NTB - 1, :], (B_wk,), (B_rb,))
                for n0 in range(0, NTB, 2):
                    pi = 4 + (n0 // 2) % 2
                    for j in range(2):
                        n = n0 + j
                        prs = [(stt[:, n, :], rv[:, n, :])]
                        if n > 0:
                            prs.append((qd[:, n, :], rball[:, n - 1, :]))
                        k.mm(ps[pi][:, j * 256:(j + 1) * 256], B_ps[pi], prs, reads=(B_stt, B_rv, B_qd, B_rb))
                    copy_op("act", obuf[:, n0:n0 + 2, :].rearrange("p a b -> p (a b)"), ps[pi][:, :], (B_ps[pi],), (B_ob,))
                k.op("dve", lambda t: t.tensor_reduce(out=stats[:, 0, :], in_=obuf[:], axis=AX.X, op=ALU.add),
                     reads=(B_ob,), writes=(B_stats,))
                k.op("act", lambda t: t.activation(out=wk[:], in_=obuf[:], func=AF.Square), reads=(B_ob, B_rb), writes=(B_wk,))
                k.op("dve", lambda t: t.tensor_reduce(out=stats[:, 1, :], in_=wk[:], axis=AX.X, op=ALU.add),
                     reads=(B_wk,), writes=(B_stats,))
                k.op("dve", lambda t: t.tensor_scalar(out=stats[:, 2, :], in0=stats[:, 0, :], scalar1=1.0 / 256, scalar2=0.0,
                                                      op0=ALU.mult, op1=ALU.add), reads=(B_stats,), writes=(B_stats,))
                k.op("dve", lambda t: t.tensor_tensor(out=stats[:, 3, :], in0=stats[:, 2, :], in1=stats[:, 2, :], op=ALU.mult),
                     reads=(B_stats,), writes=(B_stats,))
                k.op("dve", lambda t: t.scalar_tensor_tensor(out=stats[:, 3, :], in0=stats[:, 1, :], scalar=1.0 / 256,
                                                             in1=stats[:, 3, :], op0=ALU.mult, op1=ALU.subtract),
                     reads=(B_stats,), writes=(B_stats,))
                k.op("act", lambda t: t.activation(out=stats[:, 4, :], in_=stats[:, 3, :], func=AF.Ln, bias=epsb[:]),
                     reads=(B_stats, B_const), writes=(B_stats,))
                k.op("act", lambda t: t.activation(out=stats[:, 4, :], in_=stats[:, 4, :], func=AF.Exp, scale=-0.5),
                     reads=(B_stats,), writes=(B_stats,))
                k.op("dve", lambda t: t.scalar_tensor_tensor(out=stats[:, 5, :], in0=stats[:, 2, :], scalar=-1.0,
                                                             in1=stats[:, 4, :], op0=ALU.mult, op1=ALU.mult),
                     reads=(B_stats,), writes=(B_stats,))
                for n in range(NTB):
                    if n % 2:
                        k.op("act", lambda t, n=n: t.activation(out=wk[:, n, :], in_=obuf[:, n, :], func=AF.Identity,
                                                                scale=stats[:, 4, n:n + 1], bias=stats[:, 5, n:n + 1]),
                             reads=(B_ob, B_stats), writes=(B_wk,))
                    else:
                        k.op("dve", lambda t, n=n: t.tensor_scalar(out=wk[:, n, :], in0=obuf[:, n, :],
                                                                   scalar1=stats[:, 4, n:n + 1], scalar2=stats[:, 5, n:n + 1],
                                                                   op0=ALU.mult, op1=ALU.add),
                             reads=(B_ob, B_stats), writes=(B_wk,))
                k.op("dve", lambda t: t.tensor_tensor(out=yb[:], in0=wk[:], in1=rg[:], op=ALU.mult),
                     reads=(B_wk, B_rg), writes=(B_yb,))
                for ec in range(2):
                    for n0 in range(0, NTB, 8):
                        nn = min(8, NTB - n0)
                        for j in range(nn):
                            k.op("pe", lambda t, j=j, n0=n0: t.transpose(out=pT[:, j * 128:(j + 1) * 128],
                                                                       in_=yb[:, n0 + j, ec * 128:(ec + 1) * 128],
                                                                       identity=identb[:]),
                                 reads=(B_yb, B_const), writes=(B_pT,), inc=(j == nn - 1))
                        copy_op(evac_eng(), ogt[:, ec, n0 * 128:(n0 + nn) * 128], pT[:, 0:nn * 128], (B_pT,), (B_ogt,))
                for ec in range(2):
                    k.dma("pool", s_retgt[h * 2 + ec], ogt[:, ec, :], reads=(B_ogt,), writes=(B_scr["retgt"][h * 2 + ec],))
            k.barrier()

    def post_phase(l):
        with ExitStack() as p:
            def al(name, shape, dt):
                UID[0] += 1
                return p.enter_context(nc.sbuf_tensor("%s_%d" % (name, UID[0]), list(shape), dt))
            wsbo = al("wsbo", [128, 4, D], BF16)
            wreto = al("wreto", [128, 8, D], BF16)
            wout = al("wout", [128, 8, D], BF16)
            B_wp = Buf()
            k.dma("sp", wsbo[:], wb_sb_o[l].rearrange("(c p) n -> p c n", p=128), reads=(B_w["in"],), writes=(B_wp,))
            k.dma("sp", wreto[:], wb_ret_o[l].rearrange("(c p) n -> p c n", p=128), reads=(B_w["in"],), writes=(B_wp,))
            k.dma("sp", wout[:], wb_out[l].rearrange("(c p) n -> p c n", p=128), reads=(B_w["in"],), writes=(B_wp,))
            sbt = al("sbt", [128, 4, 512], BF16)
            rgt = al("rgt", [128, 8, 512], BF16)
            grt = al("grt", [128, 8, 512], BF16)
            gst = al("gst", [128, 8, 512], BF16)
            B_ld = Buf()
            t1 = [al("pt1%d" % i, [128, 512], F32) for i in range(2)]
            t2 = [al("pt2%d" % i, [128, 512], F32) for i in range(2)]
            B_t1 = [Buf(), Buf()]
            B_t2 = [Buf(), Buf()]
            mt = al("mt", [128, 8, 512], BF16)
            B_mt = Buf()
            pc = [0]
            for tg in range(NTG):
                cols = slice(tg * 512, (tg + 1) * 512)
                k.dma("sp", sbt[:], s_sbt[:, :, cols].rearrange("c p n -> p c n"),
                      reads=B_scr["sbt"][0:4], writes=(B_ld,))
                k.dma("sp", rgt[:], s_retgt[:, :, cols].rearrange("c p n -> p c n"),
                      reads=B_scr["retgt"][0:8], writes=(B_ld,))
                k.dma("sp", grt[:], s_grt[:, :, cols].rearrange("c p n -> p c n"),
                      reads=B_scr["grt"][0:8], writes=(B_ld,))
                k.dma("sp", gst[:], s_gst[:, :, cols].rearrange("c p n -> p c n"),
                      reads=B_scr["gst"][0:8], writes=(B_ld,))
                for ec in range(8):
                    i2 = ec % 2
                    p1, p2 = i2 * 2, i2 * 2 + 1
                    es = slice(ec * 128, (ec + 1) * 128)
                    k.mm(ps[p1][:], B_ps[p1], [(wsbo[:, c, es], sbt[:, c, :]) for c in range(4)], reads=(B_wp, B_ld))
                    k.mm(ps[p2][:], B_ps[p2], [(wreto[:, c, es], rgt[:, c, :]) for c in range(8)], reads=(B_wp, B_ld))
                    k.op("dve", lambda t, p1=p1, i2=i2, ec=ec: t.tensor_tensor(out=t1[i2][:], in0=ps[p1][:], in1=gst[:, ec, :],
                                                                               op=ALU.mult),
                         reads=(B_ps[p1], B_ld), writes=(B_t1[i2],))
                    k.op("dve", lambda t, p2=p2, i2=i2, ec=ec: t.tensor_tensor(out=t2[i2][:], in0=ps[p2][:], in1=grt[:, ec, :],
                                                                               op=ALU.mult),
                         reads=(B_ps[p2], B_ld), writes=(B_t2[i2],))
                    k.op("pool", lambda t, i2=i2, ec=ec: t.tensor_tensor(out=mt[:, ec, :], in0=t1[i2][:], in1=t2[i2][:],
                                                                         op=ALU.add),
                         reads=(B_t1[i2], B_t2[i2]), writes=(B_mt,))
                for dc in range(8):
                    pi = 4 + dc % 2
                    k.mm(ps[pi][:], B_ps[pi], [(wout[:, c, dc * 128:(dc + 1) * 128], mt[:, c, :]) for c in range(8)],
                         reads=(B_wp, B_mt))
                    k.op("dve", lambda t, pi=pi, dc=dc: t.tensor_tensor(out=hT[:, dc, cols], in0=ps[pi][:], in1=hT[:, dc, cols],
                                                                        op=ALU.add),
                         reads=(B_ps[pi], B_h[dc][tg]), writes=(B_h[dc][tg],))
            k.barrier()

    def ffn_phase(l):
        with ExitStack() as p:
            def al(name, shape, dt):
                UID[0] += 1
                return p.enter_context(nc.sbuf_tensor("%s_%d" % (name, UID[0]), list(shape), dt))
            hn = al("hn2", [128, 8, 512], BF16)
            B_hn = Buf()
            sq = al("sq2", [128, 8, 512], BF16)
            rstd = al("rstd2", [128, 512], F32)
            B_sq, B_rstd = Buf(), Buf()
            wgu = [al("wgu%d" % i, [128, 8, 512], BF16) for i in range(2)]
            B_wgu = [Buf(), Buf()]
            wd = [al("wd%d" % i, [128, NFC, 256], BF16) for i in range(2)]
            B_wd = [Buf(), Buf()]
            actT = al("actT", [128, NFC, 512], BF16)
            B_act = Buf()
            sg = [al("sg%d" % i, [128, 512], F32) for i in range(2)]
            B_sg = [Buf(), Buf()]
            wc = [0, 0]
            for tg in range(NTG):
                cols = slice(tg * 512, (tg + 1) * 512)
                rmsnorm_tg(tg, DEPTH + l, sq, B_sq, rstd, B_rstd, lambda c: (hn[:, c, :], (B_hn,)))

                def load_gu(fg):
                    i = wc[0] % 2
                    wc[0] += 1
                    k.dma("sp", wgu[i][:, :, 0:256],
                          wb_gu[l][:, fg * 256:(fg + 1) * 256].rearrange("(c p) n -> p c n", p=128),
                          reads=(B_w["in"],), writes=(B_wgu[i],))
                    k.dma("sp", wgu[i][:, :, 256:512],
                          wb_gu[l][:, FF + fg * 256:FF + (fg + 1) * 256].rearrange("(c p) n -> p c n", p=128),
                          reads=(B_w["in"],), writes=(B_wgu[i],))
                    return i
                nfg = NFC // 2
                cur = load_gu(0)
                for fg in range(nfg):
                    nxt = load_gu(fg + 1) if fg + 1 < nfg else None
                    w, Bw = wgu[cur], B_wgu[cur]
                    for j in range(2):
                        fcn = fg * 2 + j
                        i2 = fcn % 2
                        pg, pu = 1 + i2 * 2, 2 + i2 * 2
                        k.mm(ps[pg][:], B_ps[pg], [(w[:, c, j * 128:(j + 1) * 128], hn[:, c, :]) for c in range(8)],
                             reads=(Bw, B_hn))
                        k.mm(ps[pu][:], B_ps[pu], [(w[:, c, 256 + j * 128:256 + (j + 1) * 128], hn[:, c, :]) for c in range(8)],
                             reads=(Bw, B_hn))
                        k.op("act", lambda t, i2=i2, pg=pg: t.activation(out=sg[i2][:], in_=ps[pg][:], func=AF.Silu),
                             reads=(B_ps[pg],), writes=(B_sg[i2],))
                        k.op("dve", lambda t, i2=i2, pu=pu, fcn=fcn: t.tensor_tensor(out=actT[:, fcn, :], in0=ps[pu][:],
                                                                                     in1=sg[i2][:], op=ALU.mult),
                             reads=(B_ps[pu], B_sg[i2]), writes=(B_act,))
                    cur = nxt

                def load_wd(dg):
                    i = wc[1] % 2
                    wc[1] += 1
                    k.dma("sp", wd[i][:], wb_dn[l][:, dg * 256:(dg + 1) * 256].rearrange("(c p) n -> p c n", p=128),
                          reads=(B_w["in"],), writes=(B_wd[i],))
                    return i
                cur = load_wd(0)
                for dg in range(4):
                    nxt = load_wd(dg + 1) if dg + 1 < 4 else None
                    for j in range(2):
                        dc = dg * 2 + j
                        pi = 5 + dc % 2
                        k.mm(ps[pi][:], B_ps[pi], [(wd[cur][:, f, j * 128:(j + 1) * 128], actT[:, f, :]) for f in range(NFC)],
                             reads=(B_wd[cur], B_act))
                        k.op("dve", lambda t, pi=pi, dc=dc: t.tensor_tensor(out=hT[:, dc, cols], in0=ps[pi][:],
                                                                            in1=hT[:, dc, cols], op=ALU.add),
                             reads=(B_ps[pi], B_h[dc][tg]), writes=(B_h[dc][tg],))
                    cur = nxt
            k.barrier()

    for s in range(NSEQ):
        for c in range(8):
            k.dma("sp", hT[:, c, :], xT[s, c * 128:(c + 1) * 128, :], writes=B_h[c])
        for l in range(DEPTH):
            k.new_epoch()
            layer(l)
        if debug in ("p1", "p2", "p3") or (isinstance(debug, str) and debug.startswith("g")):
            break
        with ExitStack() as p:
            sq = p.enter_context(nc.sbuf_tensor("sqf_%d" % s, [128, 8, 512], BF16))
            rstd = p.enter_context(nc.sbuf_tensor("rstdf_%d" % s, [128, 512], F32))
            of = [p.enter_context(nc.sbuf_tensor("of%d_%d" % (i, s), [128, 8, 512], F32)) for i in range(2)]
            B_sq, B_rstd = Buf(), Buf()
            B_of = [Buf(), Buf()]
            for tg in range(NTG):
                o, Bo = of[tg % 2], B_of[tg % 2]
                rmsnorm_tg(tg, 2 * DEPTH, sq, B_sq, rstd, B_rstd, lambda c, o=o, Bo=Bo: (o[:, c, :], (Bo,)))
                k.dma("pool", outT[s][:, tg * 512:(tg + 1) * 512].rearrange("(c p) n -> p c n", p=128), o[:],
                      reads=(Bo,))
            k.barrier()
    k.barrier()
    return nc, ctx, consts


_CACHE = {}


def _prep_inputs(inputs, S, DEPTH):
    consts, _ = host_consts(S)
    gl = [inputs["norm_mix"][l] for l in range(DEPTH)] + [inputs["norm_ffn"][l] for l in range(DEPTH)] + \
         [inputs["norm_final"]]
    gvec = np.stack([np.asarray(g, np.float32).reshape(8, 128).T for g in gl], 1)
    shared = {
        "w_in": np.ascontiguousarray(inputs["w_in"], np.float32),
        "w_ret_o": np.ascontiguousarray(inputs["w_ret_o"], np.float32),
        "w_sb_o": np.ascontiguousarray(inputs["w_sb_o"], np.float32),
        "w_out": np.ascontiguousarray(inputs["w_out"], np.float32),
        "w_gate_up": np.ascontiguousarray(inputs["w_gate_up"], np.float32),
        "w_down": np.ascontiguousarray(inputs["w_down"], np.float32),
        "gvec": np.ascontiguousarray(gvec.reshape(128, -1), np.float32),
    }
    for kk, v in consts.items():
        shared["c_" + kk] = np.ascontiguousarray(v, np.float32)
    return shared


def kernel(x, norm_mix, w_in, w_ret_o, w_sb_o, w_out, norm_ffn, w_gate_up, w_down, norm_final):
    x = np.asarray(x, np.float32)
    B, S, _ = x.shape
    DEPTH = np.asarray(w_in).shape[0]
    ncores = 8
    NSEQ = B // ncores
    inputs = dict(norm_mix=np.asarray(norm_mix), w_in=np.asarray(w_in), w_ret_o=np.asarray(w_ret_o),
                  w_sb_o=np.asarray(w_sb_o), w_out=np.asarray(w_out), norm_ffn=np.asarray(norm_ffn),
                  w_gate_up=np.asarray(w_gate_up), w_down=np.asarray(w_down), norm_final=np.asarray(norm_final))
    shared = _prep_inputs(inputs, S, DEPTH)
    nc, ctx, _ = build_program(NSEQ, S, DEPTH)
    in_maps = []
    for c in range(ncores):
        m = dict(shared)
        m["xT"] = np.ascontiguousarray(x[c * NSEQ:(c + 1) * NSEQ].transpose(0, 2, 1))
        in_maps.append(m)
    res = run_bass_kernel_spmd(nc, in_maps, core_ids=list(range(ncores)))
    out = np.empty((B, S, D), np.float32)
    for c in range(ncores):
        out[c * NSEQ:(c + 1) * NSEQ] = res.results[c]["outT"].transpose(0, 2, 1)
    return out
```
